# Optimizing a Trainium2 kernel written in Bass

```python
import math
import jax
import jax.numpy as jnp
from jax import lax
import numpy as np

D_MODEL = 1024
BATCH = 2
SEQ = 8192
DEPTH = 2

GRID_W = 64
CTX_LEN = 256
CHUNK = 128
Q_BLOCK = 128
N_DIR = 2

M_HEADS = 4
M_QK_DIM = D_MODEL // 8
M_V_DIM = D_MODEL // 4
M_WIDTH = M_HEADS * M_V_DIM

S_HEADS = 16
S_HEAD_DIM = D_MODEL // 16
S_GROUPS = 2
S_HEADS_PER_GROUP = S_HEADS // S_GROUPS
S_STATE = 128
S_WIDTH = S_HEADS * S_HEAD_DIM
S_CONV_CH = S_WIDTH + 2 * S_GROUPS * S_STATE
CONV_K = 4

AB_WIDTHS = (M_HEADS * M_QK_DIM, M_HEADS * M_QK_DIM, M_WIDTH, M_WIDTH, N_DIR * M_HEADS, N_DIR * M_HEADS, S_WIDTH, S_CONV_CH, N_DIR * S_HEADS)
AB_IN = sum(AB_WIDTHS)
AB_SPLITS = tuple(int(v) for v in np.cumsum(AB_WIDTHS)[:-1])
AB_MIX = M_WIDTH + S_WIDTH

A_HEADS = 8
A_KV_HEADS = 2
A_HEAD_DIM = D_MODEL // A_HEADS
A_Q_W = A_HEADS * A_HEAD_DIM
A_KV_W = A_KV_HEADS * A_HEAD_DIM
AT_IN = A_Q_W + 2 * A_KV_W
ROPE_THETA = 10000.0

N_EXPERTS = 16
N_EXPERT_GROUPS = 4
EXPERTS_PER_GROUP = N_EXPERTS // N_EXPERT_GROUPS
TOP_K = 2
D_FF = D_MODEL
MOE_BLOCK = 256

DEEPNORM_ALPHA = (2 * DEPTH) ** 0.25
DEEPNORM_BETA = (8 * DEPTH) ** -0.25
N_EVEN = (DEPTH + 1) // 2
N_ODD = DEPTH // 2
LN_EPS = 1e-5
RMS_EPS = 1e-6
F32 = jnp.float32

kernel_name = 'hybrid_mlstm_ssd_gqa_moe_diffusion_block'


def layer_norm(x, g, b):
    xf = x.astype(F32)
    mu = xf.mean(-1, keepdims=True)
    var = jnp.mean(jnp.square(xf - mu), -1, keepdims=True)
    return ((xf - mu) * lax.rsqrt(var + LN_EPS) * g + b).astype(x.dtype)


def rms_norm(x, g):
    xf = x.astype(F32)
    return (xf * lax.rsqrt(jnp.mean(xf * xf, -1, keepdims=True) + RMS_EPS) * g).astype(x.dtype)


def head_layer_norm(h, g):
    mu = h.mean(-1, keepdims=True)
    var = jnp.mean(jnp.square(h - mu), -1, keepdims=True)
    y = (h - mu) * lax.rsqrt(var + LN_EPS)
    return y.reshape(h.shape[0], h.shape[1], -1) * g


def ada_params(cvec, w, b):
    m = jax.nn.silu(cvec) @ w + b
    return jnp.split(m, 6, axis=-1)


def modulate(h, shift, scale):
    return h * (1 + scale) + shift


def axial_rope_tables(n_tokens):
    rows = n_tokens // GRID_W
    row = jnp.repeat(jnp.arange(rows), GRID_W).astype(F32)
    col = jnp.tile(jnp.arange(GRID_W), rows).astype(F32)
    n_freq = A_HEAD_DIM // 4
    inv = ROPE_THETA ** (-jnp.arange(n_freq, dtype=F32) / n_freq)
    ang = jnp.concatenate([row[:, None] * inv, col[:, None] * inv], -1)
    return jnp.cos(ang), jnp.sin(ang)


def apply_rope(x, cos, sin):
    half = x.shape[-1] // 2
    x1, x2 = x[..., :half].astype(F32), x[..., half:].astype(F32)
    c = cos[None, :, None, :]
    s = sin[None, :, None, :]
    return jnp.concatenate([x1 * c - x2 * s, x2 * c + x1 * s], -1).astype(x.dtype)


def dwconv_centred(x, w, b):
    k = w.shape[0]
    y = lax.conv_general_dilated(x, w[:, None, :].astype(x.dtype), window_strides=(1,),
                                 padding=[(k // 2, k - 1 - k // 2)],
                                 dimension_numbers=('NWC', 'WIO', 'NWC'),
                                 feature_group_count=x.shape[-1])
    return y + b


def mlstm_chunkwise(q, k, v, ig, lf, state):
    bsz, t_len, nh, dk = q.shape
    dv = v.shape[-1]
    nc, L = t_len // CHUNK, CHUNK
    q = q.astype(F32).reshape(bsz, nc, L, nh, dk)
    k = k.astype(F32).reshape(bsz, nc, L, nh, dk)
    v = v.astype(F32).reshape(bsz, nc, L, nh, dv)
    ig = ig.reshape(bsz, nc, L, nh)
    lf = lf.reshape(bsz, nc, L, nh)
    b = jnp.cumsum(lf, axis=2)
    b_end = b[:, :, -1]
    g = b_end[:, :, None] - b + ig
    g_max = g.max(axis=2)
    w = jnp.exp(g - g_max[:, :, None])
    d_c = jnp.einsum('bcshv,bcshd->bchvd', v * w[..., None], k)
    d_n = jnp.einsum('bcsh,bcshd->bchd', w, k)

    def step(carry, inp):
        c_st, n_st, m_st = carry
        dc, dn, be, gm = inp
        m_new = jnp.maximum(be + m_st, gm)
        a = jnp.exp(be + m_st - m_new)
        s = jnp.exp(gm - m_new)
        c_new = a[..., None, None] * c_st + s[..., None, None] * dc
        n_new = a[..., None] * n_st + s[..., None] * dn
        return (c_new, n_new, m_new), (c_st, n_st, m_st)

    final, (cs, ns, ms) = lax.scan(step, state, (jnp.moveaxis(d_c, 1, 0), jnp.moveaxis(d_n, 1, 0),
                                                 jnp.moveaxis(b_end, 1, 0), jnp.moveaxis(g_max, 1, 0)))
    cs, ns, ms = jnp.moveaxis(cs, 0, 1), jnp.moveaxis(ns, 0, 1), jnp.moveaxis(ms, 0, 1)
    lower = jnp.tril(jnp.ones((L, L), bool))
    dmat = jnp.where(lower[:, :, None], b[:, :, :, None, :] - b[:, :, None, :, :] + ig[:, :, None, :, :], -jnp.inf)
    inter = b + ms[:, :, None, :]
    m_t = jnp.maximum(inter, dmat.max(axis=3))
    sc = jnp.einsum('bcthd,bcshd->bctsh', q, k) * jnp.exp(dmat - m_t[:, :, :, None, :])
    a_in = jnp.exp(inter - m_t)
    num = jnp.einsum('bctsh,bcshv->bcthv', sc, v) + a_in[..., None] * jnp.einsum('bcthd,bchvd->bcthv', q, cs)
    den = sc.sum(axis=3) + a_in * jnp.einsum('bcthd,bchd->bcth', q, ns)
    h = num / jnp.maximum(jnp.abs(den), jnp.exp(-m_t))[..., None]
    return h.reshape(bsz, t_len, nh, dv), final


def ssd_chunked(xs, a, bm, cm, h0):
    bsz, t_len, ng, nj, p = xs.shape
    n = bm.shape[-1]
    nc, L = t_len // CHUNK, CHUNK
    xs = xs.astype(F32).reshape(bsz, nc, L, ng, nj, p)
    a = a.astype(F32).reshape(bsz, nc, L, ng, nj)
    bm = bm.astype(F32).reshape(bsz, nc, L, ng, n)
    cm = cm.astype(F32).reshape(bsz, nc, L, ng, n)
    a_cs = jnp.cumsum(a, axis=2)
    lower = jnp.tril(jnp.ones((L, L), bool))
    seg = a_cs[:, :, :, None] - a_cs[:, :, None]
    decay = jnp.exp(jnp.where(lower[:, :, None, None], seg, -jnp.inf))
    cb = jnp.einsum('bctgn,bcsgn->bctsg', cm, bm)
    y = jnp.einsum('bctsgj,bcsgjp->bctgjp', cb[..., None] * decay, xs)
    a_end = a_cs[:, :, -1]
    xw = xs * jnp.exp(a_end[:, :, None] - a_cs)[..., None]
    d_states = jnp.einsum('bcsgn,bcsgjp->bcgjpn', bm, xw)

    def step(h, inp):
        ds, ae = inp
        return jnp.exp(ae)[..., None, None] * h + ds, h

    h_final, hs = lax.scan(step, h0, (jnp.moveaxis(d_states, 1, 0), jnp.moveaxis(a_end, 1, 0)))
    hs = jnp.moveaxis(hs, 0, 1)
    y = y + jnp.einsum('bctgn,bcgjpn->bctgjp', cm, hs) * jnp.exp(a_cs)[..., None]
    return y.reshape(bsz, t_len, ng, nj, p), h_final


def _flip_time(t):
    return jnp.flip(t, axis=1)


def _identity(t):
    return t


def bidirectional_prefix_scan(run, ctx_dirs, lat_dirs, zero_state):
    outs_c, outs_l = [], []
    for d in range(N_DIR):
        f = _flip_time if d == 1 else _identity
        yc, st = run(*[f(t) for t in ctx_dirs[d]], zero_state)
        yl, _ = run(*[f(t) for t in lat_dirs[d]], st)
        outs_c.append(f(yc))
        outs_l.append(f(yl))
    return outs_c[0] + outs_c[1], outs_l[0] + outs_l[1]


def mixer_mlstm_ssd(hl, hc, w_in, w_out, ig_b, fg_b, m_norm_g, conv_w, conv_b, dt_b, a_log, d_skip, s_norm_g):
    a_neg = (-jnp.exp(a_log.astype(F32))).reshape(N_DIR, S_GROUPS, S_HEADS_PER_GROUP)

    def prepare(h):
        bsz, t_len, _ = h.shape
        q, k, v, o, ig, fg, z, xbc, dt = jnp.split(h @ w_in, AB_SPLITS, axis=-1)
        q = q.reshape(bsz, t_len, M_HEADS, M_QK_DIM) * (M_QK_DIM ** -0.5)
        k = k.reshape(bsz, t_len, M_HEADS, M_QK_DIM)
        v = v.reshape(bsz, t_len, M_HEADS, M_V_DIM)
        ig = ig.reshape(bsz, t_len, N_DIR, M_HEADS).astype(F32) + ig_b
        lf = jax.nn.log_sigmoid(fg.reshape(bsz, t_len, N_DIR, M_HEADS).astype(F32) + fg_b)
        xbc = jax.nn.silu(dwconv_centred(xbc, conv_w, conv_b))
        xs, bm, cm = jnp.split(xbc, [S_WIDTH, S_WIDTH + S_GROUPS * S_STATE], axis=-1)
        xs = xs.reshape(bsz, t_len, S_GROUPS, S_HEADS_PER_GROUP, S_HEAD_DIM).astype(F32)
        bm = bm.reshape(bsz, t_len, S_GROUPS, S_STATE)
        cm = cm.reshape(bsz, t_len, S_GROUPS, S_STATE)
        dt = jax.nn.softplus(dt.reshape(bsz, t_len, N_DIR, S_HEADS).astype(F32) + dt_b)
        dt = dt.reshape(bsz, t_len, N_DIR, S_GROUPS, S_HEADS_PER_GROUP)
        m_dirs = [(q, k, v, ig[:, :, d], lf[:, :, d]) for d in range(N_DIR)]
        s_dirs = [(xs * dt[:, :, d][..., None], dt[:, :, d] * a_neg[d], bm, cm) for d in range(N_DIR)]
        return m_dirs, s_dirs, o, z, xs

    mc, sc, oc, zc, xsc = prepare(hc)
    ml, sl, ol, zl, xsl = prepare(hl)
    bsz = hl.shape[0]
    m_zero = (jnp.zeros((bsz, M_HEADS, M_V_DIM, M_QK_DIM), F32), jnp.zeros((bsz, M_HEADS, M_QK_DIM), F32),
              jnp.zeros((bsz, M_HEADS), F32))
    s_zero = jnp.zeros((bsz, S_GROUPS, S_HEADS_PER_GROUP, S_HEAD_DIM, S_STATE), F32)
    hm_c, hm_l = bidirectional_prefix_scan(mlstm_chunkwise, mc, ml, m_zero)
    hs_c, hs_l = bidirectional_prefix_scan(ssd_chunked, sc, sl, s_zero)

    def finish(hm, hs, o, z, xs):
        bsz_, t_len = o.shape[:2]
        ym = head_layer_norm(hm, m_norm_g) * jax.nn.sigmoid(o.astype(F32))
        ys = (hs + d_skip.astype(F32).reshape(S_GROUPS, S_HEADS_PER_GROUP, 1) * xs).reshape(bsz_, t_len, S_WIDTH)
        ys = rms_norm(ys * jax.nn.silu(z.astype(F32)), s_norm_g)
        return jnp.concatenate([ym, ys], -1).astype(o.dtype) @ w_out

    return finish(hm_l, hs_l, ol, zl, xsl), finish(hm_c, hs_c, oc, zc, xsc)


def blocked_attention(q, k, v):
    bsz, t_len, _, dh = q.shape
    nb = t_len // Q_BLOCK
    qb = jnp.moveaxis(q.reshape(bsz, nb, Q_BLOCK, A_KV_HEADS, A_HEADS // A_KV_HEADS, dh), 1, 0)
    scale = dh ** -0.5

    def one(qblk):
        s = jnp.einsum('bqkgd,bskd->bkgqs', qblk, k).astype(F32) * scale
        p = jax.nn.softmax(s, axis=-1).astype(v.dtype)
        return jnp.einsum('bkgqs,bskd->bqkgd', p, v)

    o = lax.map(one, qb)
    return jnp.moveaxis(o, 0, 1).reshape(bsz, t_len, A_Q_W)


def mixer_gqa(hl, hc, w_in, w_out, q_g, k_g, cos, sin, need_ctx):
    def q_proj(h):
        bsz, t_len, _ = h.shape
        return rms_norm((h @ w_in[:, :A_Q_W]).reshape(bsz, t_len, A_HEADS, A_HEAD_DIM), q_g)

    def kv_proj(h):
        bsz, t_len, _ = h.shape
        k, v = jnp.split(h @ w_in[:, A_Q_W:], [A_KV_W], axis=-1)
        k = rms_norm(k.reshape(bsz, t_len, A_KV_HEADS, A_HEAD_DIM), k_g)
        return k, v.reshape(bsz, t_len, A_KV_HEADS, A_HEAD_DIM)

    kl, vl = kv_proj(hl)
    kl = apply_rope(kl, cos, sin)
    ql = apply_rope(q_proj(hl), cos, sin)
    kc, vc = kv_proj(hc)
    k_all = jnp.concatenate([kc, kl], axis=1)
    v_all = jnp.concatenate([vc, vl], axis=1)
    yl = blocked_attention(ql, k_all, v_all) @ w_out
    yc = blocked_attention(q_proj(hc), kc, vc) @ w_out if need_ctx else None
    return yl, yc


def moe_ffn(h, router_w, router_b, w_gate, w_up, w_down):
    n_tok, d = h.shape
    probs = jax.nn.softmax((h @ router_w).astype(F32), axis=-1)
    sel = (probs + router_b).reshape(n_tok, N_EXPERT_GROUPS, EXPERTS_PER_GROUP)
    g_score = lax.top_k(sel, TOP_K)[0].sum(-1)
    g_idx = jnp.argmax(g_score, axis=-1)
    in_group = jnp.take_along_axis(sel, g_idx[:, None, None], axis=1)[:, 0]
    _, local = lax.top_k(in_group, TOP_K)
    e_idx = g_idx[:, None] * EXPERTS_PER_GROUP + local
    gate = jnp.take_along_axis(probs, e_idx, axis=1)
    gate = gate / gate.sum(-1, keepdims=True)
    n_asg = n_tok * TOP_K
    flat_e = e_idx.reshape(-1)
    flat_tok = jnp.repeat(jnp.arange(n_tok), TOP_K)
    order = jnp.argsort(flat_e)
    se, stok, sgate = flat_e[order], flat_tok[order], gate.reshape(-1)[order]
    counts = jnp.bincount(flat_e, length=N_EXPERTS)
    padded = (counts + MOE_BLOCK - 1) // MOE_BLOCK * MOE_BLOCK
    start = jnp.cumsum(counts) - counts
    pend = jnp.cumsum(padded)
    pstart = pend - padded
    dest = pstart[se] + jnp.arange(n_asg) - start[se]
    n_blocks = -(-n_asg // MOE_BLOCK) + N_EXPERTS
    slot_tok = jnp.zeros((n_blocks * MOE_BLOCK,), jnp.int32).at[dest].set(stok)
    blk_exp = jnp.minimum(jnp.searchsorted(pend, jnp.arange(n_blocks) * MOE_BLOCK, side='right'), N_EXPERTS - 1)

    def expert_block(args):
        toks, e = args
        xb = h[toks]
        return (jax.nn.silu(xb @ w_gate[e]) * (xb @ w_up[e])) @ w_down[e]

    y_slots = lax.map(expert_block, (slot_tok.reshape(n_blocks, MOE_BLOCK), blk_exp)).reshape(-1, d)
    contrib = y_slots[dest] * sgate[:, None].astype(h.dtype)
    return jnp.zeros_like(h).at[stok].add(contrib)


def setup_inputs(seed: int = 0) -> dict:
    key = jax.random.key(seed)
    keys = iter(jax.random.split(key, 40))
    D = D_MODEL

    def nrm(shape, scale):
        return jax.random.normal(next(keys), shape, F32) * scale

    def uni(shape, lo, hi):
        return jax.random.uniform(next(keys), shape, F32, lo, hi)

    dt0 = jnp.exp(uni((N_EVEN, N_DIR, S_HEADS), math.log(1e-3), math.log(1e-1)))
    return {
        'x': nrm((BATCH, SEQ, D), 1.0),
        'c': nrm((BATCH, D), 1.0),
        'ctx': nrm((BATCH, CTX_LEN, D), 1.0),
        'c_ctx': nrm((D,), 1.0),
        'ada_w': nrm((DEPTH, D, 6 * D), 0.5 * D ** -0.5),
        'ada_b': nrm((DEPTH, 6 * D), 0.02),
        'ln_g': 1.0 + nrm((DEPTH, 2, D), 0.02),
        'ln_b': nrm((DEPTH, 2, D), 0.02),
        'ab_w_in': nrm((N_EVEN, D, AB_IN), D ** -0.5),
        'ab_w_out': nrm((N_EVEN, AB_MIX, D), AB_MIX ** -0.5 * DEEPNORM_BETA),
        'ml_ig_b': nrm((N_EVEN, N_DIR, M_HEADS), 0.1),
        'ml_fg_b': uni((N_EVEN, N_DIR, M_HEADS), 3.0, 6.0),
        'ml_norm_g': 1.0 + nrm((N_EVEN, M_WIDTH), 0.02),
        'ssm_conv_w': nrm((N_EVEN, CONV_K, S_CONV_CH), CONV_K ** -0.5),
        'ssm_conv_b': nrm((N_EVEN, S_CONV_CH), 0.02),
        'ssm_dt_b': dt0 + jnp.log(-jnp.expm1(-dt0)),
        'ssm_a_log': jnp.log(uni((N_EVEN, N_DIR, S_HEADS), 1.0, 16.0)),
        'ssm_d': 1.0 + nrm((N_EVEN, S_HEADS), 0.1),
        'ssm_norm_g': 1.0 + nrm((N_EVEN, S_WIDTH), 0.02),
        'at_w_in': nrm((N_ODD, D, AT_IN), D ** -0.5),
        'at_w_out': nrm((N_ODD, A_Q_W, D), A_Q_W ** -0.5 * DEEPNORM_BETA),
        'at_q_g': 1.0 + nrm((N_ODD, A_HEAD_DIM), 0.02),
        'at_k_g': 1.0 + nrm((N_ODD, A_HEAD_DIM), 0.02),
        'router_w': nrm((D, N_EXPERTS), D ** -0.5),
        'router_b': nrm((N_EXPERTS,), 0.01),
        'moe_w_gate': nrm((DEPTH, N_EXPERTS, D, D_FF), D ** -0.5),
        'moe_w_up': nrm((DEPTH, N_EXPERTS, D, D_FF), D ** -0.5),
        'moe_w_down': nrm((DEPTH, N_EXPERTS, D_FF, D), D_FF ** -0.5 * DEEPNORM_BETA),
    }


def reference(x, c, ctx, c_ctx, ada_w, ada_b, ln_g, ln_b, ab_w_in, ab_w_out, ml_ig_b, ml_fg_b, ml_norm_g,
              ssm_conv_w, ssm_conv_b, ssm_dt_b, ssm_a_log, ssm_d, ssm_norm_g, at_w_in, at_w_out, at_q_g, at_k_g,
              router_w, router_b, moe_w_gate, moe_w_up, moe_w_down):
    bsz, n_lat, d = x.shape
    cos, sin = axial_rope_tables(n_lat)
    xl, xc = x, ctx
    for i in range(DEPTH):
        last = i == DEPTH - 1
        j = i // 2
        sh1l, sc1l, gt1l, sh2l, sc2l, gt2l = ada_params(c[:, None, :], ada_w[i], ada_b[i])
        sh1c, sc1c, gt1c, sh2c, sc2c, gt2c = ada_params(c_ctx, ada_w[i], ada_b[i])
        hl = modulate(xl, sh1l, sc1l)
        hc = modulate(xc, sh1c, sc1c)
        if i % 2 == 0:
            yl, yc = mixer_mlstm_ssd(hl, hc, ab_w_in[j], ab_w_out[j], ml_ig_b[j], ml_fg_b[j], ml_norm_g[j],
                                     ssm_conv_w[j], ssm_conv_b[j], ssm_dt_b[j], ssm_a_log[j], ssm_d[j], ssm_norm_g[j])
        else:
            yl, yc = mixer_gqa(hl, hc, at_w_in[j], at_w_out[j], at_q_g[j], at_k_g[j], cos, sin, not last)
        xl = layer_norm(DEEPNORM_ALPHA * xl + gt1l * yl, ln_g[i, 0], ln_b[i, 0])
        hl = modulate(xl, sh2l, sc2l)
        if last:
            yl = moe_ffn(hl.reshape(-1, d), router_w, router_b, moe_w_gate[i], moe_w_up[i], moe_w_down[i]).reshape(xl.shape)
        else:
            xc = layer_norm(DEEPNORM_ALPHA * xc + gt1c * yc, ln_g[i, 0], ln_b[i, 0])
            hc = modulate(xc, sh2c, sc2c)
            y_tok = moe_ffn(jnp.concatenate([hl.reshape(-1, d), hc.reshape(-1, d)], axis=0),
                            router_w, router_b, moe_w_gate[i], moe_w_up[i], moe_w_down[i])
            yl = y_tok[:bsz * n_lat].reshape(xl.shape)
            yc = y_tok[bsz * n_lat:].reshape(xc.shape)
            xc = layer_norm(DEEPNORM_ALPHA * xc + gt2c * yc, ln_g[i, 1], ln_b[i, 1])
        xl = layer_norm(DEEPNORM_ALPHA * xl + gt2l * yl, ln_g[i, 1], ln_b[i, 1])
    return xl
```

```python
import numpy as np
from contextlib import ExitStack
import concourse.bass as bass
import concourse.mybir as mybir
from concourse.bass_utils import run_bass_kernel_spmd

F32 = mybir.dt.float32
F32R = mybir.dt.float32r
AF = mybir.ActivationFunctionType
ALU = mybir.AluOpType
AX = mybir.AxisListType

D = 1024
NCORES = 8
CTX = 256
SEQ = 8192
TOWN = 2048
NT = 18
TOK = NT * 128
AB_IN = 5680
ALPHA = 4 ** 0.25
LN_EPS = 1e-5
RMS_EPS = 1e-6
NE = 16


class Sched:
    SEM_CAP = 30000
    NSLOT = 12

    def __init__(self, nc, es):
        self.nc = nc
        self.es = es
        self.eng = {"pe": nc.tensor, "act": nc.scalar, "dve": nc.vector, "pool": nc.gpsimd, "sp": nc.sync}
        self.count = {k: 0 for k in self.eng}
        self.sems = {k: [] for k in self.eng}
        self.waited = {k: {} for k in self.eng}
        self.last_w = {}
        self.readers = {}
        self.n_dma = 0
        self.psum_keys = set()

    def _sem(self, e, idx):
        if e.startswith("cc_ig"):
            return self.sems[e][0], 16, 0
        if e == "cc_all":
            return self.sems[e][0], idx + 1, 0
        if e.startswith("cc_"):
            return self.sems[e][0], 1, 0
        cap = self.SEM_CAP // 16 if e.startswith("dma") else self.SEM_CAP
        mul = 16 if e.startswith("dma") else 1
        k = idx // cap
        while len(self.sems[e]) <= k:
            self.sems[e].append(self.es.enter_context(self.nc.semaphore(f"s_{e}_{len(self.sems[e])}")))
        return self.sems[e][k], ((idx % cap) + 1) * mul, k

    def _wait(self, e, dep):
        p, pidx = dep
        sem, val, k = self._sem(p, pidx)
        key = (p, k)
        if self.waited[e].get(key, 0) >= val:
            return
        self.waited[e][key] = val
        self.eng[e].wait_ge(sem, val)

    def op(self, e, fn, reads=(), writes=(), dma=False):
        deps = set()
        for r in reads:
            if r in self.last_w:
                deps.add(self.last_w[r])
            if r in self.psum_keys:
                for rd in self.readers.get(r, ()):
                    if rd[0] != e:
                        deps.add(rd)
        for w in writes:
            if w in self.last_w:
                deps.add(self.last_w[w])
            for rd in self.readers.get(w, ()):
                deps.add(rd)
        for d in sorted(deps):
            if d[0] == "pe" and e == "pe":
                continue
            self._wait(e, d)
        if dma:
            slot = self.n_dma % self.NSLOT
            self.n_dma += 1
            name = f"dma{slot}"
            if name not in self.count:
                self.count[name] = 0
                self.sems[name] = []
            idx = self.count[name]
            if idx > 0:
                self._wait(e, (name, idx - 1))
            self.count[name] += 1
            sem, val, _ = self._sem(name, idx)
            inst = fn(self.eng[e])
            inst.then_inc(sem, 16)
            me = (name, idx)
        else:
            idx = self.count[e]
            self.count[e] += 1
            sem, val, _ = self._sem(e, idx)
            inst = fn(self.eng[e])
            inst.then_inc(sem, 1)
            me = (e, idx)
        for r in reads:
            self.readers.setdefault(r, []).append(me)
        for w in writes:
            self.last_w[w] = me
            self.readers[w] = []
        return inst

    def barrier(self):
        for e in self.eng:
            for name in list(self.count):
                if self.count[name] > 0:
                    self._wait(e, (name, self.count[name] - 1))
        self.last_w = {}
        self.readers = {}

    def finish(self, e="sp"):
        for name in list(self.count):
            if self.count[name] > 0 and name != e:
                self._wait(e, (name, self.count[name] - 1))


class KB:
    def __init__(self):
        self.nc = bass.Bass("TRN2", target_bir_lowering=False)
        self.es = ExitStack()
        self.S = Sched(self.nc, self.es)
        self.rr = 0
        self.stack = [self.es]
        self.nscope = 0
        self.pfx = ""

    def din(self, name, shape):
        return self.nc.dram_tensor(name, list(shape), F32, kind="ExternalInput").ap()

    def dout(self, name, shape):
        return self.nc.dram_tensor(name, list(shape), F32, kind="ExternalOutput").ap()

    def dscr(self, name, shape):
        return self.nc.dram_tensor(name, list(shape), F32, kind="Internal").ap()

    def sb(self, name, shape, dt=F32, es=None):
        return (es or self.stack[-1]).enter_context(self.nc.sbuf_tensor("sb_" + self.pfx + name, list(shape), dt))

    def ps(self, name, shape, dt=F32, es=None):
        self.S.psum_keys.add(name)
        return (es or self.stack[-1]).enter_context(self.nc.psum_tensor("ps_" + self.pfx + name, list(shape), dt))

    def load(self, out, in_, r, w, eng="sp"):
        return self.S.op(eng, lambda e: e.dma_start(out=out, in_=in_), reads=r, writes=w, dma=True)

    def store(self, out, in_, r, w, eng="sp"):
        return self.S.op(eng, lambda e: e.dma_start(out=out, in_=in_), reads=r, writes=w, dma=True)

    def mm(self, out, lhsT, rhs, r, w, start=True, stop=True):
        return self.S.op("pe", lambda e: e.matmul(out, lhsT=lhsT, rhs=rhs, start=start, stop=stop), reads=r, writes=w)

    def tr(self, out, in_, ident, r, w):
        return self.S.op("pe", lambda e: e.transpose(out, in_, ident), reads=list(r) + ["ident"], writes=w)

    def act(self, out, in_, func, r, w, bias=None, scale=None):
        kw = {}
        if bias is not None:
            kw["bias"] = bias
        if scale is not None:
            kw["scale"] = scale
        return self.S.op("act", lambda e: e.activation(out=out, in_=in_, func=func, **kw), reads=r, writes=w)

    def tt(self, out, in0, in1, op, r, w, eng="dve"):
        return self.S.op(eng, lambda e: e.tensor_tensor(out=out, in0=in0, in1=in1, op=op), reads=r, writes=w)

    def ts(self, out, in0, s1, op0, r, w, s2=None, op1=None, eng="dve"):
        if op1 is None:
            return self.S.op(eng, lambda e: e.tensor_scalar(out=out, in0=in0, scalar1=s1, scalar2=None, op0=op0), reads=r, writes=w)
        return self.S.op(eng, lambda e: e.tensor_scalar(out=out, in0=in0, scalar1=s1, scalar2=s2, op0=op0, op1=op1), reads=r, writes=w)

    def stt(self, out, in0, scalar, in1, op0, op1, r, w):
        return self.S.op("dve", lambda e: e.scalar_tensor_tensor(out=out, in0=in0, scalar=scalar, in1=in1, op0=op0, op1=op1), reads=r, writes=w)

    def cp(self, out, in_, r, w, eng="dve"):
        if eng == "act":
            return self.act(out, in_, AF.Identity, r, w)
        return self.S.op(eng, lambda e: e.tensor_copy(out=out, in_=in_), reads=r, writes=w)

    def cp_rr(self, out, in_, r, w, engs=("dve", "act")):
        self.rr += 1
        return self.cp(out, in_, r, w, eng=engs[self.rr % len(engs)])

    def memset(self, ap, val, w, eng="dve"):
        return self.S.op(eng, lambda e: e.memset(ap, val), reads=(), writes=w)

    def recip(self, out, in_, r, w):
        return self.S.op("dve", lambda e: e.reciprocal(out=out, in_=in_), reads=r, writes=w)

    def scope(self):
        kb = self

        class _Scope:
            def __enter__(self_):
                kb.nscope += 1
                self_.old = kb.pfx
                kb.pfx = f"z{kb.nscope}_"
                self_.es = ExitStack()
                kb.stack.append(self_.es)
                return self_

            def __exit__(self_, *a):
                kb.S.barrier()
                kb.stack.pop()
                self_.es.close()
                kb.pfx = self_.old
                return False
        return _Scope()

    def collective(self, kind, src, dst, groups, rkeys, wkey):
        S = self.S
        name = "cc_all"
        if name not in S.count:
            S.count[name] = 0
            S.sems[name] = [self.es.enter_context(self.nc.semaphore(name))]
        deps = set()
        for r in rkeys:
            if r in S.last_w:
                deps.add(S.last_w[r])
        for rd in S.readers.get(wkey, ()):
            deps.add(rd)
        if wkey in S.last_w:
            deps.add(S.last_w[wkey])
        for d in sorted(deps):
            S._wait("pool", d)
        inst = self.nc.gpsimd.collective_compute(kind, ALU.bypass, replica_groups=groups, ins=[src.opt()], outs=[dst.opt()])
        inst.then_inc(S.sems[name][0], 1)
        idx = S.count[name]
        S.count[name] += 1
        S.last_w[wkey] = (name, idx)
        S.readers[wkey] = []

    def gather_rows(self, out, src, idx_ap, r, w):
        S = self.S
        self.ngather = getattr(self, "ngather", 0) + 1
        name = f"cc_ig{self.ngather}"
        sem = self.es.enter_context(self.nc.semaphore(name))
        deps = set()
        for k in r:
            if k in S.last_w:
                deps.add(S.last_w[k])
        for k in w:
            if k in S.last_w:
                deps.add(S.last_w[k])
            for rd in S.readers.get(k, ()):
                deps.add(rd)
        for d in sorted(deps):
            S._wait("pool", d)
        inst = self.nc.gpsimd.indirect_dma_start(out=out, out_offset=None, in_=src, in_offset=bass.IndirectOffsetOnAxis(ap=idx_ap, axis=0))
        inst.then_inc(sem, 16)
        S.count[name] = 1
        S.sems[name] = [sem]
        for k in r:
            S.readers.setdefault(k, []).append((name, 0))
        for k in w:
            S.last_w[k] = (name, 0)
            S.readers[k] = []

    def done(self):
        self.S.finish("sp")
        self.es.close()
        return self.nc


def emit_mod_vectors(kb, ada_w, ada_b, cpair, vec_ids, pfx, es):
    nv = len(vec_ids)
    mod = kb.sb(pfx + "mod", [128, nv, 8, 2])
    with ExitStack() as les:
        csb = kb.sb(pfx + "c", [128, 8, 2], es=les)
        sig = kb.sb(pfx + "sig", [128, 8, 2], es=les)
        bpp = kb.sb(pfx + "bpp", [128, nv, 8], es=les)
        wsb = kb.sb(pfx + "w", [128, 8, 1024], es=les)
        pm = kb.ps(pfx + "pm", [128, 8, 2], es=les)
        kb.load(csb[:], cpair.rearrange("p (k w) -> p k w", w=2), ["cpair"], [pfx + "c"])
        kb.act(sig[:], csb[:], AF.Sigmoid, [pfx + "c"], [pfx + "sig"])
        kb.tt(csb[:], csb[:], sig[:], ALU.mult, [pfx + "c", pfx + "sig"], [pfx + "c"])
        for vi, v in enumerate(vec_ids):
            kb.load(bpp[:, vi, :], ada_b[:, v * 8:(v + 1) * 8], ["ada_b"], [(pfx + "bpp", vi)])
        for vi, v in enumerate(vec_ids):
            kb.load(wsb[:], ada_w[:, v * 1024:(v + 1) * 1024].rearrange("(k p) n -> p k n", p=128), ["ada_w"], [pfx + "w"])
            for cc in range(8):
                for k in range(8):
                    kb.mm(pm[:, cc, :], wsb[:, k, cc * 128:(cc + 1) * 128], csb[:, k, :], [pfx + "w", pfx + "c"], [pfx + "pm"],
                          start=(k == 0), stop=(k == 7))
            for who in range(2):
                kb.tt(mod[:, vi, :, who], pm[:, :, who], bpp[:, vi, :], ALU.add, [pfx + "pm", (pfx + "bpp", vi)], [(pfx + "mod", vi)])
        kb.S.barrier()
    return mod


def emit_bcast_vectors(kb, ada_w, ada_b_row, cpair, vec_ids, pfx, out_tiles, es):
    with ExitStack() as les:
        csb = kb.sb(pfx + "c", [128, 8, 2], es=les)
        sig = kb.sb(pfx + "sig", [128, 8, 2], es=les)
        cb = kb.sb(pfx + "cb", [128, 2, 8, 128], es=les)
        wsb = kb.sb(pfx + "w", [128, 8, 1024], es=les)
        bb = kb.sb(pfx + "bb", [128, 1024], es=les)
        pm = kb.ps(pfx + "pm", [128, 2, 512], es=les)
        kb.load(csb[:], cpair.rearrange("p (k w) -> p k w", w=2), ["cpair"], [pfx + "c"])
        kb.act(sig[:], csb[:], AF.Sigmoid, [pfx + "c"], [pfx + "sig"])
        kb.tt(csb[:], csb[:], sig[:], ALU.mult, [pfx + "c", pfx + "sig"], [pfx + "c"])
        for who in range(2):
            for k in range(8):
                kb.cp(cb[:, who, k, :], csb[:, k, who:who + 1].to_broadcast([128, 128]), [pfx + "c"], [pfx + "cb"])
        for vi, v in enumerate(vec_ids):
            kb.load(wsb[:], ada_w[:, v * 1024:(v + 1) * 1024].rearrange("(k p) n -> p k n", p=128), ["ada_w"], [pfx + "w"])
            kb.load(bb[:], ada_b_row[:, v * 1024:(v + 1) * 1024].partition_broadcast(128), ["ada_b"], [pfx + "bb"])
            for who in range(2):
                for nb in range(2):
                    for k in range(8):
                        kb.mm(pm[:, nb, :], cb[:, who, k, :], wsb[:, k, nb * 512:(nb + 1) * 512], [pfx + "w", pfx + "cb"], [pfx + "pm"],
                              start=(k == 0), stop=(k == 7))
                ot, okey = out_tiles[vi][who]
                kb.tt(ot, pm[:].rearrange("p a b -> p (a b)"), bb[:], ALU.add, [pfx + "pm", pfx + "bb"], [okey])
        kb.S.barrier()


def emit_transpose_mod(kb, src_tile, skey, hT, hkey, t, ident, ptr, mod, vi_shift, vi_scale1p, who, ptr_key):
    for half in range(2):
        for kk in range(4):
            k = half * 4 + kk
            kb.tr(ptr[:, kk, :], src_tile[:, k * 128:(k + 1) * 128], ident, [skey], [ptr_key])
        for kk in range(4):
            k = half * 4 + kk
            kb.act(hT[:, k, t * 128:(t + 1) * 128], ptr[:, kk, :], AF.Identity, [ptr_key, "mod"], [(hkey, t, k)],
                   bias=mod[:, vi_shift, k, who:who + 1], scale=mod[:, vi_scale1p, k, who:who + 1])


def build_A():
    kb = KB()
    xin = kb.din("xin", [TOK, D])
    cpair = kb.din("cpair", [128, 16])
    ada_w = kb.din("ada_w", [D, 6 * D])
    ada_b = kb.din("ada_b", [128, 48])
    w_in = kb.din("w_in", [D, AB_IN])
    ident_d = kb.din("ident_d", [128, 128])
    proj = kb.dout("proj", [TOK, AB_IN])

    ident = kb.sb("ident", [128, 128])
    kb.load(ident[:], ident_d, ["ident_d"], ["ident"])
    mod = emit_mod_vectors(kb, ada_w, ada_b, cpair, [0, 1], "m0", kb.es)
    kb.ts(mod[:, 1], mod[:, 1], 1.0, ALU.add, [("m0mod", 1)], ["mod"])
    kb.S.barrier()

    hT = kb.sb("hT", [128, 8, TOK], F32R)
    xt = [kb.sb(f"xt{i}", [128, D]) for i in range(2)]
    ptr = [kb.ps(f"ptr{i}", [128, 4, 128]) for i in range(2)]
    for t in range(NT):
        b = t % 2
        kb.load(xt[b][:], xin[t * 128:(t + 1) * 128, :], ["xin"], [f"xt{b}"])
        who = 1 if t < 2 else 0
        emit_transpose_mod(kb, xt[b], f"xt{b}", hT, "hT", t, ident[:], ptr[b], mod, 0, 1, who, f"ptr{b}")

    wf = [kb.sb(f"wf{i}", [128, 8, 512]) for i in range(2)]
    wr = [kb.sb(f"wr{i}", [128, 8, 512], F32R) for i in range(2)]
    po = [kb.ps(f"po{i}", [128, 512]) for i in range(2)]
    ot = [kb.sb(f"ot{i}", [128, 512]) for i in range(3)]
    nblk = (AB_IN + 511) // 512
    it = 0

    def ldw(cb):
        c0 = cb * 512
        cw = min(512, AB_IN - c0)
        kb.load(wf[cb % 2][:, :, :cw], w_in[:, c0:c0 + cw].rearrange("(k p) n -> p k n", p=128), ["w_in"], [f"wf{cb % 2}"])

    ldw(0)
    for cb in range(nblk):
        c0 = cb * 512
        cw = min(512, AB_IN - c0)
        b = cb % 2
        if cb + 1 < nblk:
            ldw(cb + 1)
        for k in range(8):
            kb.cp(wr[b][:, k, :cw], wf[b][:, k, :cw], [f"wf{b}"], [(f"wr{b}", k)], eng=("pool" if k % 2 else "dve"))
        for t in range(NT):
            pb = it % 2
            ob = it % 3
            it += 1
            for k in range(8):
                kb.mm(po[pb][:, :cw], hT[:, k, t * 128:(t + 1) * 128], wr[b][:, k, :cw], [("hT", t, k), (f"wr{b}", k)], [f"po{pb}"],
                      start=(k == 0), stop=(k == 7))
            kb.cp_rr(ot[ob][:, :cw], po[pb][:, :cw], [f"po{pb}"], [f"ot{ob}"])
            kb.store(proj[t * 128:(t + 1) * 128, c0:c0 + cw], ot[ob][:, :cw], [f"ot{ob}"], ["proj"])
    return kb.done()


def consts():
    ident = np.eye(128, dtype=np.float32)
    return {"ident": ident}


def pp_layout(v):
    return np.ascontiguousarray(v.reshape(-1, 128).T)


def cpair_pp(inp, b):
    a = np.stack([inp["c"][b], inp["c_ctx"]], axis=1)
    return np.ascontiguousarray(a.reshape(8, 128, 2).transpose(1, 0, 2).reshape(128, 16))


def core_tokens(x, ctx, core):
    b, r = core // 4, core % 4
    return np.concatenate([ctx[b], x[b, r * TOWN:(r + 1) * TOWN]], axis=0)


def run_A(inp):
    nc = build_A()
    maps = []
    for core in range(NCORES):
        b = core // 4
        maps.append({
            "xin": np.ascontiguousarray(core_tokens(inp["x"], inp["ctx"], core)),
            "cpair": cpair_pp(inp, b),
            "ada_w": np.ascontiguousarray(inp["ada_w"][0]),
            "ada_b": pp_layout(inp["ada_b"][0]),
            "w_in": np.ascontiguousarray(inp["ab_w_in"][0]),
            "ident_d": consts()["ident"],
        })
    res = run_bass_kernel_spmd(nc, maps, core_ids=list(range(NCORES)))
    return [r["proj"] for r in res.results]


def build_B(nlat=64, dbg=99):
    NCH = nlat + 2
    T = NCH * 128
    kb = KB()
    aps = dict(
        qT=kb.din("qT", [128, T]), kT=kb.din("kT", [128, T]), ktok=kb.din("ktok", [T, 128]), v=kb.din("v", [T, 256]),
        gates=kb.din("gates", [128, NCH * 4]), xbcT=kb.din("xbcT", [512, T]), dtr=kb.din("dtr", [128, NCH * 8]),
        gb=kb.din("gb", [128, 4]), convw=kb.din("convw", [128, 16]), convb=kb.din("convb", [128, 4]), dtb=kb.din("dtb", [128, 8]),
        alog=kb.din("alog", [128, 8]), dsk=kb.din("dsk", [128, 4]), cst=kb.din("cst", [128, 6, 128]))
    hm_o = kb.dout("hm", [128, NCH * 256])
    ys_o = kb.dout("ys", [128, NCH * 256])
    aps["hm3"] = hm_o.rearrange("p (c f) -> p c f", f=256)
    aps["ys3"] = ys_o.rearrange("p (c f) -> p c f", f=256)
    aps["xpost"] = kb.dscr("xpost", [512, T])
    with kb.scope():
        emit_B(kb, aps, nlat)
    return kb.done()


def emit_B(kb, aps, nlat):
    NCH = nlat + 2
    T = NCH * 128
    dbg = 99
    qT_d, kT_d, kt_d, v_d, g_d, xbc_d, dtr_d = aps["qT"], aps["kT"], aps["ktok"], aps["v"], aps["gates"], aps["xbcT"], aps["dtr"]
    gb_d, cw_d, cb_d, dtb_d, alog_d, dsk_d, cst_d = aps["gb"], aps["convw"], aps["convb"], aps["dtb"], aps["alog"], aps["dsk"], aps["cst"]
    xpost = aps["xpost"]
    cst = kb.sb("cst", [128, 6, 128])
    kb.load(cst[:], cst_d, ["cst_d"], ["cst"])
    ident = cst[:, 0, :]
    tri = [cst[:, 1, :], cst[:, 2, :]]
    strict = [cst[:, 3, :], cst[:, 4, :]]
    ones = cst[:, 5, :]
    par = kb.sb("par", [128, 48])
    for nm, ap_, o, n in (("gb", gb_d, 0, 4), ("cw", cw_d, 4, 16), ("cb", cb_d, 20, 4), ("dtb", dtb_d, 24, 8), ("alog", alog_d, 32, 8), ("dsk", dsk_d, 40, 4)):
        kb.load(par[:, o:o + n], ap_, [nm], ["par"])
    gb, cw, cbias, dtb, alog, dsk = par[:, 0:4], par[:, 4:20], par[:, 20:24], par[:, 24:32], par[:, 32:40], par[:, 40:44]

    G = kb.sb("G", [128, NCH, 4])
    kb.load(G[:], g_d.rearrange("p (c g) -> p c g", g=4), ["g_d"], ["G"])
    negb = kb.sb("negb", [128, 4])
    kb.ts(negb[:], gb, -1.0, ALU.mult, ["par"], ["negb"])
    LF = [kb.sb(f"LF{d}", [128, NCH]) for d in range(2)]
    IG = [kb.sb(f"IG{d}", [128, NCH]) for d in range(2)]
    WK = [kb.sb(f"WK{d}", [128, NCH]) for d in range(2)]
    QS = [kb.sb(f"QS{d}", [128, NCH]) for d in range(2)]
    EB = [kb.sb(f"EB{d}", [128, NCH]) for d in range(2)]
    pes = ExitStack()
    pg = kb.ps("pg", [128, 512], es=pes)
    for d in range(2):
        kb.ts(IG[d][:], G[:, :, d], gb[:, d:d + 1], ALU.add, ["G", "par"], [f"IG{d}"])
        kb.act(LF[d][:], G[:, :, 2 + d], AF.Exp, ["G", "negb"], [f"LF{d}"], bias=negb[:, 2 + d:3 + d], scale=-1.0)
        kb.act(LF[d][:], LF[d][:], AF.Ln, [f"LF{d}"], [f"LF{d}"], bias=1.0)
        kb.ts(LF[d][:], LF[d][:], -1.0, ALU.mult, [f"LF{d}"], [f"LF{d}"])
        kb.mm(pg[:, :NCH], tri[d], LF[d][:], ["cst", f"LF{d}"], ["pg"])
        kb.tt(WK[d][:], IG[d][:], pg[:, :NCH], ALU.subtract, [f"IG{d}", "pg"], [f"WK{d}"])
        kb.act(WK[d][:], WK[d][:], AF.Exp, [f"WK{d}"], [f"WK{d}"])
        kb.act(QS[d][:], pg[:, :NCH], AF.Exp, ["pg"], [f"QS{d}"])
        kb.ts(QS[d][:], QS[d][:], 128 ** -0.5, ALU.mult, [f"QS{d}"], [f"QS{d}"])
        kb.mm(pg[:, :NCH], ones, LF[d][:], ["cst", f"LF{d}"], ["pg"])
        kb.act(EB[d][:], pg[:, :NCH], AF.Exp, ["pg"], [f"EB{d}"])

    DT = kb.sb("DT", [128, NCH, 8])
    kb.load(DT[:], dtr_d.rearrange("p (c g) -> p c g", g=8), ["dtr_d"], ["DT"])
    for i in range(8):
        kb.ts(DT[:, :, i], DT[:, :, i], dtb[:, i:i + 1], ALU.add, ["DT", "par"], ["DT"])
    kb.act(DT[:], DT[:], AF.Exp, ["DT"], ["DT"])
    kb.act(DT[:], DT[:], AF.Ln, ["DT"], ["DT"], bias=1.0)
    aneg = kb.sb("aneg", [128, 8])
    kb.act(aneg[:], alog, AF.Exp, ["par"], ["aneg"])
    kb.ts(aneg[:], aneg[:], -1.0, ALU.mult, ["aneg"], ["aneg"])
    A = [kb.sb(f"A{d}", [128, NCH, 4]) for d in range(2)]
    EACS = [kb.sb(f"EACS{d}", [128, NCH, 4]) for d in range(2)]
    DTW = [kb.sb(f"DTW{d}", [128, NCH, 4]) for d in range(2)]
    EAE = [kb.sb(f"EAE{d}", [128, NCH, 4]) for d in range(2)]
    pg2 = kb.ps("pg2", [128, 512], es=pes)
    for d in range(2):
        for h in range(4):
            kb.ts(A[d][:, :, h], DT[:, :, d * 4 + h], aneg[:, d * 4 + h:d * 4 + h + 1], ALU.mult, ["DT", "aneg"], [f"A{d}"])
        Af = A[d][:].rearrange("p c h -> p (c h)")
        kb.mm(pg[:, :NCH * 4], tri[d], Af, ["cst", f"A{d}"], ["pg"])
        kb.mm(pg2[:, :NCH * 4], ones, Af, ["cst", f"A{d}"], ["pg2"])
        kb.act(EACS[d][:].rearrange("p c h -> p (c h)"), pg[:, :NCH * 4], AF.Exp, ["pg"], [f"EACS{d}"])
        kb.act(EAE[d][:].rearrange("p c h -> p (c h)"), pg2[:, :NCH * 4], AF.Exp, ["pg2"], [f"EAE{d}"])
        kb.cp(DTW[d][:].rearrange("p c h -> p (c h)"), pg[:, :NCH * 4], ["pg"], [f"DTW{d}"])
        kb.tt(DTW[d][:].rearrange("p c h -> p (c h)"), pg2[:, :NCH * 4], DTW[d][:].rearrange("p c h -> p (c h)"), ALU.subtract, ["pg2", f"DTW{d}"], [f"DTW{d}"])
        DWf = DTW[d][:].rearrange("p c h -> p (c h)")
        kb.act(DWf, DWf, AF.Exp, [f"DTW{d}"], [f"DTW{d}"])
        for h in range(4):
            kb.tt(DTW[d][:, :, h], DTW[d][:, :, h], DT[:, :, d * 4 + h], ALU.mult, [f"DTW{d}", "DT"], [f"DTW{d}"])

    kb.S.barrier()
    pes.close()
    with ExitStack() as les:
        Lmax = max(256, nlat * 128)
        xp = kb.sb("xp", [128, Lmax + 3], es=les)
        acc = kb.sb("acc", [128, Lmax], es=les)
        for cc in range(4):
            for (t0, L) in ((0, 256), (256, nlat * 128)):
                kb.memset(xp[:, 0:2], 0.0, ["xp"])
                kb.memset(xp[:, L + 2:L + 3], 0.0, ["xp"])
                kb.load(xp[:, 2:2 + L], xbc_d[cc * 128:(cc + 1) * 128, t0:t0 + L], ["xbc_d"], ["xp"])
                kb.ts(acc[:, :L], xp[:, 0:L], cw[:, cc * 4:cc * 4 + 1], ALU.mult, ["xp", "par"], ["acc"], s2=cbias[:, cc:cc + 1], op1=ALU.add)
                for j in range(1, 4):
                    kb.stt(acc[:, :L], xp[:, j:j + L], cw[:, cc * 4 + j:cc * 4 + j + 1], acc[:, :L], ALU.mult, ALU.add, ["xp", "acc", "par"], ["acc"])
                kb.act(acc[:, :L], acc[:, :L], AF.Silu, ["acc"], ["acc"])
                kb.store(xpost[cc * 128:(cc + 1) * 128, t0:t0 + L], acc[:, :L], ["acc"], ["xpost"])
        kb.S.barrier()

    HM = kb.sb("HM", [128, NCH, 256])
    YS = kb.sb("YS", [128, NCH, 256])
    nb = 2

    class TS:
        pass

    TD = []
    for d in range(2):
        T_ = TS()
        sfx = f"_{d}"
        T_.sfx = sfx
        T_.qTc = [kb.sb(f"qTc{i}{sfx}", [128, 128]) for i in range(nb)]
        T_.kTc = [kb.sb(f"kTc{i}{sfx}", [128, 128]) for i in range(nb)]
        T_.ktc = [kb.sb(f"ktc{i}{sfx}", [128, 128]) for i in range(nb)]
        T_.vaug = [kb.sb(f"vaug{i}{sfx}", [128, 257]) for i in range(nb)]
        T_.xsT = [kb.sb(f"xsT{i}{sfx}", [128, 2, 128]) for i in range(nb)]
        T_.BTc = [kb.sb(f"BTc{i}{sfx}", [128, 128]) for i in range(nb)]
        T_.CTc = [kb.sb(f"CTc{i}{sfx}", [128, 128]) for i in range(nb)]
        for i in range(nb):
            kb.memset(T_.vaug[i][:, 256:257], 1.0, [(f"vaug{i}{sfx}", "one")])
        for nm, shp in (("xs_tok", [128, 256]), ("Btok", [128, 128]), ("PT", [128, 128]), ("vw", [128, 257]), ("sm", [128, 4]), ("cbm", [128, 128]),
                        ("xsdt", [128, 256]), ("xw", [128, 256]), ("tmp", [128, 256]), ("tmp2", [128, 256])):
            setattr(T_, nm, kb.sb(nm + sfx, shp))
        T_.CTst = [kb.sb(f"CTst{i}{sfx}", [128, 257]) for i in range(2)]
        T_.Hst = [kb.sb(f"Hst{i}{sfx}", [128, 256]) for i in range(2)]
        T_.Lh = [kb.sb(f"Lh{i}{sfx}", [128, 128]) for i in range(2)]
        T_.dec = [kb.sb(f"dec{i}{sfx}", [128, 128]) for i in range(2)]
        T_.MT = [kb.sb(f"MT{i}{sfx}", [128, 128]) for i in range(2)]
        T_.cur = 0
        T_.it = 0
        kb.memset(T_.CTst[0][:], 0.0, [f"CTst0{sfx}"])
        kb.memset(T_.Hst[0][:], 0.0, [f"Hst0{sfx}"])
        TD.append(T_)
    ptr = kb.ps("ptr", [128, 3, 128])
    pA = kb.ps("pA", [128, 2, 128])
    pN = kb.ps("pN", [128, 257])
    pC = kb.ps("pC", [128, 257])
    pSeg = kb.ps("pSeg", [128, 128])
    pY = kb.ps("pY", [128, 2, 256])
    pH = kb.ps("pH", [128, 256])
    written = set()

    def proc(d, c):
        T_ = TD[d]
        x_ = T_.sfx
        i = T_.it % nb
        T_.it += 1
        cur = T_.cur
        nxt = 1 - cur
        first = c not in written
        written.add(c)
        qTc, kTc, ktc, vaug, xsT, BTc, CTc = T_.qTc, T_.kTc, T_.ktc, T_.vaug, T_.xsT, T_.BTc, T_.CTc
        xs_tok, Btok, PT, vw, sm, cbm, xsdt, xw, tmp, tmp2 = T_.xs_tok, T_.Btok, T_.PT, T_.vw, T_.sm, T_.cbm, T_.xsdt, T_.xw, T_.tmp, T_.tmp2
        CTst, Hst, Lh, dec, MT = T_.CTst, T_.Hst, T_.Lh, T_.dec, T_.MT
        sl = slice(c * 128, (c + 1) * 128)
        kb.load(qTc[i][:], qT_d[:, sl], ["qT_d"], [f"qTc{i}{x_}"])
        kb.load(kTc[i][:], kT_d[:, sl], ["kT_d"], [f"kTc{i}{x_}"])
        kb.load(ktc[i][:], kt_d[sl, :], ["kt_d"], [f"ktc{i}{x_}"])
        kb.load(vaug[i][:, :256], v_d[sl, :], ["v_d"], [f"vaug{i}{x_}"])
        kb.load(xsT[i][:], xpost[0:256, sl].rearrange("(a p) t -> p a t", p=128), ["xpost"], [f"xsT{i}{x_}"])
        kb.load(BTc[i][:], xpost[256:384, sl], ["xpost"], [f"BTc{i}{x_}"])
        kb.load(CTc[i][:], xpost[384:512, sl], ["xpost"], [f"CTc{i}{x_}"])
        kb.tr(ptr[:, 0, :], xsT[i][:, 0, :], ident, [f"xsT{i}{x_}"], ["ptr"])
        kb.tr(ptr[:, 1, :], xsT[i][:, 1, :], ident, [f"xsT{i}{x_}"], ["ptr"])
        kb.tr(ptr[:, 2, :], BTc[i][:], ident, [f"BTc{i}{x_}"], ["ptr"])
        kb.cp(xs_tok[:], ptr[:, 0:2, :].rearrange("p a t -> p (a t)"), ["ptr"], ["xs_tok" + x_], eng="act")
        kb.cp(Btok[:], ptr[:, 2, :], ["ptr"], ["Btok" + x_], eng="act")
        wkc = WK[d][:, c:c + 1]
        kb.mm(pA[:, 0, :], kTc[i][:], qTc[i][:], [f"kTc{i}{x_}", f"qTc{i}{x_}"], ["pA"])
        kb.mm(pA[:, 1, :], BTc[i][:], CTc[i][:], [f"BTc{i}{x_}", f"CTc{i}{x_}"], ["pA"])
        kb.stt(PT[:], pA[:, 0, :], wkc, tri[d], ALU.mult, ALU.mult, ["pA", f"WK{d}", "cst"], ["PT" + x_])
        kb.tt(cbm[:], pA[:, 1, :], tri[d], ALU.mult, ["pA", "cst"], ["cbm" + x_])
        kb.act(vw[:], vaug[i][:], AF.Identity, [f"vaug{i}{x_}", (f"vaug{i}{x_}", "one"), f"WK{d}"], ["vw" + x_], scale=wkc)
        kb.mm(pN[:], PT[:], vaug[i][:], ["PT" + x_, f"vaug{i}{x_}", (f"vaug{i}{x_}", "one")], ["pN"], start=True, stop=False)
        kb.mm(pN[:], qTc[i][:], CTst[cur][:], [f"qTc{i}{x_}", f"CTst{cur}{x_}"], ["pN"], start=False, stop=True)
        kb.mm(pC[:], ktc[i][:], vw[:], [f"ktc{i}{x_}", "vw" + x_], ["pC"])
        ebc = EB[d][:, c:c + 1]
        kb.act(CTst[nxt][:], CTst[cur][:], AF.Identity, [f"CTst{cur}{x_}", f"EB{d}"], [f"CTst{nxt}{x_}"], scale=ebc)
        kb.stt(CTst[nxt][:], pC[:], ebc, CTst[nxt][:], ALU.mult, ALU.add, ["pC", f"EB{d}", f"CTst{nxt}{x_}"], [f"CTst{nxt}{x_}"])
        qsc = QS[d][:, c:c + 1]
        smk = "sm" + x_
        kb.ts(sm[:, 0:1], pN[:, 256:257], qsc, ALU.mult, ["pN", f"QS{d}"], [smk])
        kb.act(sm[:, 1:2], sm[:, 0:1], AF.Abs, [smk], [smk])
        kb.ts(sm[:, 1:2], sm[:, 1:2], 1.0, ALU.max, [smk], [smk])
        kb.recip(sm[:, 2:3], sm[:, 1:2], [smk], [smk])
        kb.tt(sm[:, 3:4], sm[:, 2:3], qsc, ALU.mult, [smk, f"QS{d}"], [smk])
        if first:
            kb.ts(HM[:, c, :], pN[:, 0:256], sm[:, 3:4], ALU.mult, ["pN", smk], [("HM", c)])
        else:
            kb.stt(HM[:, c, :], pN[:, 0:256], sm[:, 3:4], HM[:, c, :], ALU.mult, ALU.add, ["pN", smk, ("HM", c)], [("HM", c)])
        for h in range(4):
            hs = slice(h * 64, (h + 1) * 64)
            kb.act(xsdt[:, hs], xs_tok[:, hs], AF.Identity, ["xs_tok" + x_, "DT"], ["xsdt" + x_], scale=DT[:, c, d * 4 + h:d * 4 + h + 1])
            kb.act(xw[:, hs], xs_tok[:, hs], AF.Identity, ["xs_tok" + x_, f"DTW{d}"], ["xw" + x_], scale=DTW[d][:, c, h:h + 1])
        for h in range(4):
            hs = slice(h * 64, (h + 1) * 64)
            j = h % 2
            kb.act(Lh[j][:], strict[d], AF.Identity, ["cst", f"A{d}"], [f"Lh{j}{x_}"], scale=A[d][:, c, h:h + 1])
            kb.mm(pSeg[:], Lh[j][:], tri[d], [f"Lh{j}{x_}", "cst"], ["pSeg"])
            kb.act(dec[j][:], pSeg[:], AF.Exp, ["pSeg"], [f"dec{j}{x_}"])
            kb.tt(MT[j][:], dec[j][:], cbm[:], ALU.mult, [f"dec{j}{x_}", "cbm" + x_], [f"MT{j}{x_}"])
            kb.mm(pY[:, 0, hs], MT[j][:], xsdt[:, hs], [f"MT{j}{x_}", "xsdt" + x_], ["pY"])
        kb.mm(pY[:, 1, :], CTc[i][:], Hst[cur][:], [f"CTc{i}{x_}", f"Hst{cur}{x_}"], ["pY"])
        for h in range(4):
            hs = slice(h * 64, (h + 1) * 64)
            kb.ts(tmp[:, hs], pY[:, 1, hs], EACS[d][:, c, h:h + 1], ALU.mult, ["pY", f"EACS{d}"], ["tmp" + x_])
        if first:
            kb.tt(YS[:, c, :], pY[:, 0, :], tmp[:], ALU.add, ["pY", "tmp" + x_], [("YS", c)])
        else:
            kb.tt(tmp2[:], pY[:, 0, :], tmp[:], ALU.add, ["pY", "tmp" + x_], ["tmp2" + x_])
            kb.tt(YS[:, c, :], YS[:, c, :], tmp2[:], ALU.add, ["tmp2" + x_, ("YS", c)], [("YS", c)])
        if d == 0:
            for h in range(4):
                hs = slice(h * 64, (h + 1) * 64)
                kb.stt(YS[:, c, hs], xs_tok[:, hs], dsk[:, h:h + 1], YS[:, c, hs], ALU.mult, ALU.add, ["xs_tok" + x_, "par", ("YS", c)], [("YS", c)])
        kb.mm(pH[:], Btok[:], xw[:], ["Btok" + x_, "xw" + x_], ["pH"])
        for h in range(4):
            hs = slice(h * 64, (h + 1) * 64)
            kb.act(Hst[nxt][:, hs], Hst[cur][:, hs], AF.Identity, [f"Hst{cur}{x_}", f"EAE{d}"], [f"Hst{nxt}{x_}"], scale=EAE[d][:, c, h:h + 1])
        kb.tt(Hst[nxt][:], Hst[nxt][:], pH[:], ALU.add, [f"Hst{nxt}{x_}", "pH"], [f"Hst{nxt}{x_}"])
        T_.cur = nxt

    orders = [list(range(NCH)), [1, 0] + list(range(NCH - 1, 1, -1))]
    for step in range(NCH):
        for d in range(2):
            proc(d, orders[d][step])
    if "E" in aps:
        E = aps["E"]
        for c in range(NCH):
            kb.store(E[c * 128:(c + 1) * 128, 0:256], HM[:, c, :], [("HM", c)], ["hm_o"])
            kb.store(E[c * 128:(c + 1) * 128, 256:512], YS[:, c, :], [("YS", c)], ["ys_o"])
        return
    hm3, ys3 = aps["hm3"], aps["ys3"]
    step = aps.get("ostep", 16)
    for c0 in range(0, NCH, step):
        c1 = min(NCH, c0 + step)
        kb.store(hm3[:, c0:c1, :], HM[:, c0:c1, :], [("HM", c) for c in range(c0, c1)], ["hm_o"])
        kb.store(ys3[:, c0:c1, :], YS[:, c0:c1, :], [("YS", c) for c in range(c0, c1)], ["ys_o"])


def consts_B():
    s = np.arange(128)[:, None]
    t = np.arange(128)[None, :]
    return np.ascontiguousarray(np.stack([np.eye(128), s <= t, s >= t, s > t, s < t, np.ones((128, 128))], axis=1).astype(np.float32))


def pmajor(a):
    C = a.shape[0] // 128
    return np.ascontiguousarray(a.reshape(C, 128, -1).transpose(1, 0, 2).reshape(128, -1))


def unpmajor(a, F):
    C = a.shape[1] // F
    return np.ascontiguousarray(a.reshape(128, C, F).transpose(1, 0, 2).reshape(C * 128, F))


def rep(v, n=128):
    return np.ascontiguousarray(np.broadcast_to(np.asarray(v, np.float32).reshape(1, -1), (n, np.asarray(v).size)))


def maps_B(inp, projb, b, j):
    g = j // 2
    q = projb[:, j * 128:(j + 1) * 128]
    k = projb[:, 512 + j * 128:512 + (j + 1) * 128]
    v = projb[:, 1024 + j * 256:1024 + (j + 1) * 256]
    ig = projb[:, 3072:3080].reshape(-1, 2, 4)[:, :, j]
    fg = projb[:, 3080:3088].reshape(-1, 2, 4)[:, :, j]
    xbc0 = 3088 + 1024
    xs = projb[:, xbc0 + j * 256:xbc0 + (j + 1) * 256]
    Bm = projb[:, xbc0 + 1024 + g * 128:xbc0 + 1024 + (g + 1) * 128]
    Cm = projb[:, xbc0 + 1280 + g * 128:xbc0 + 1280 + (g + 1) * 128]
    dt = projb[:, 5648:5680].reshape(-1, 2, 16)[:, :, 4 * j:4 * j + 4].reshape(-1, 8)
    chs = np.concatenate([np.arange(j * 256, (j + 1) * 256), 1024 + np.arange(g * 128, (g + 1) * 128), 1280 + np.arange(g * 128, (g + 1) * 128)])
    cwt = inp["ssm_conv_w"][0][:, chs]
    convw = cwt.T.reshape(4, 128, 4).transpose(1, 0, 2).reshape(128, 16)
    convb = inp["ssm_conv_b"][0][chs].reshape(4, 128).T
    return {
        "qT": np.ascontiguousarray(q.T), "kT": np.ascontiguousarray(k.T), "ktok": np.ascontiguousarray(k), "v": np.ascontiguousarray(v),
        "gates": pmajor(np.concatenate([ig, fg], axis=1)),
        "xbcT": np.ascontiguousarray(np.concatenate([xs, Bm, Cm], axis=1).T),
        "dtr": pmajor(dt),
        "gb": rep(np.concatenate([inp["ml_ig_b"][0][:, j], inp["ml_fg_b"][0][:, j]])),
        "convw": np.ascontiguousarray(convw), "convb": np.ascontiguousarray(convb),
        "dtb": rep(inp["ssm_dt_b"][0][:, 4 * j:4 * j + 4].reshape(-1)),
        "alog": rep(inp["ssm_a_log"][0][:, 4 * j:4 * j + 4].reshape(-1)),
        "dsk": rep(inp["ssm_d"][0][4 * j:4 * j + 4]),
        "cst": consts_B(),
    }


def emit_ln(kb, u, ukey, out, okey, g_bc, b_bc, st, mv, pfx):
    ukeys = list(ukey) if isinstance(ukey, list) else [ukey]
    for hh in range(2):
        kb.S.op("dve", lambda e, hh=hh: e.bn_stats(out=st[:, hh, :], in_=u[:, hh * 512:(hh + 1) * 512]), reads=ukeys, writes=[pfx + "st"])
    kb.S.op("dve", lambda e: e.bn_aggr(out=mv[:, 0:2], in_=st[:].rearrange("p a b -> p (a b)")), reads=[pfx + "st"], writes=[pfx + "mv"])
    kb.act(mv[:, 2:3], mv[:, 1:2], AF.Sqrt, [pfx + "mv"], [pfx + "mv"], bias=LN_EPS)
    kb.recip(mv[:, 3:4], mv[:, 2:3], [pfx + "mv"], [pfx + "mv"])
    kb.ts(out, u[:], mv[:, 0:1], ALU.subtract, ukeys + [pfx + "mv"], [okey], s2=mv[:, 3:4], op1=ALU.mult)
    kb.tt(out, out, g_bc, ALU.mult, [okey, "bc"], [okey])
    kb.tt(out, out, b_bc, ALU.add, [okey, "bc"], [okey], eng="pool")


def build_P(layer, nt=NT, ne=NE):
    K_ = 2048 if layer == 0 else 1024
    tok = nt * 128
    kb = KB()
    aps = dict(
        xres=kb.din("xres", [tok, D]), cpair=kb.din("cpair", [128, 16]), ada_w=kb.din("ada_w", [D, 6 * D]), ada_b=kb.din("ada_b", [128, 48]),
        ada_b_row=kb.din("ada_b_row", [1, 6 * D]), lnp=kb.din("lnp", [1, 4 * D]), w_out=kb.din("w_out", [K_, D]), cst_d=kb.din("cst_d", [128, 128]),
        router_w=kb.din("router_w", [D, 16]), router_b=kb.din("router_b", [1, 16]),
        w_gate=kb.din("w_gate", [ne, 4, 128, 2048]), w_up=kb.din("w_up", [ne, 4, 128, 2048]), w_down=kb.din("w_down", [ne, 4, 128, 2048]))
    if layer == 0:
        aps.update(hm=kb.din("hm", [tok, D]), ysd=kb.din("ysd", [tok, D]), o=kb.din("o", [tok, D]), z=kb.din("z", [tok, D]), ng=kb.din("ng", [1, 2 * D]))
    else:
        aps.update(attn=kb.din("attn", [tok, D]))
    xout = kb.dout("xout", [tok, D])
    aps["xout_fn"] = lambda t: xout[t * 128:(t + 1) * 128, :]
    aps["x1_d"] = kb.dscr("x1_d", [tok, D])
    with kb.scope():
        emit_P(kb, layer, aps, nt, ne)
    return kb.done()


def emit_P(kb, layer, aps, nt=NT, ne=NE, nctx=2, TB=384):
    K_ = 2048 if layer == 0 else 1024
    KC = K_ // 128
    tok = nt * 128
    TPP = nt // 2
    NTB = TPP * 128 // TB
    assert NTB * TB == TPP * 128
    xres, cpair, ada_w, ada_b, ada_b_row, lnp, w_out, cst_d = (aps[k] for k in ("xres", "cpair", "ada_w", "ada_b", "ada_b_row", "lnp", "w_out", "cst_d"))
    rw_d, rb_d, wg_d, wu_d, wd_d = (aps[k] for k in ("router_w", "router_b", "w_gate", "w_up", "w_down"))
    x1_d = aps["x1_d"]
    gathered = "EG" in aps
    if layer == 0:
        ng_d = aps["ng"]
        if not gathered:
            hm_d, ys_d, o_d, z_d = aps["hm"], aps["ysd"], aps["o"], aps["z"]
    else:
        at_d = aps["attn"]

    ident = kb.sb("ident", [128, 128])
    kb.load(ident[:], cst_d, ["cst_d"], ["ident"])
    mod = emit_mod_vectors(kb, ada_w, ada_b, cpair, [3, 4], "m", kb.es)
    kb.ts(mod[:, 1], mod[:, 1], 1.0, ALU.add, [("mmod", 1)], ["mod"])
    bc = kb.sb("bc", [128, 4, D])

    def fill_bc(stage):
        emit_bcast_vectors(kb, ada_w, ada_b_row, cpair, [2 if stage == 0 else 5], f"g{stage}",
                           [[(bc[:, 0, :], "bc"), (bc[:, 1, :], "bc")]], kb.es)
        for i in range(2):
            kb.load(bc[:, 2 + i, :], lnp[:, (2 * stage + i) * D:(2 * stage + i + 1) * D].partition_broadcast(128), ["lnp"], ["bc"])
        kb.S.barrier()

    fill_bc(0)

    with ExitStack() as les:
        wo = kb.sb("wo", [128, KC, D], F32R, es=les)
        wtmp = [kb.sb(f"wtmp{i}", [128, D], es=les) for i in range(2)]
        for kc in range(KC):
            kb.load(wtmp[kc % 2][:], w_out[kc * 128:(kc + 1) * 128, :], ["w_out"], [f"wtmp{kc % 2}"])
            kb.cp(wo[:, kc, :], wtmp[kc % 2][:], [f"wtmp{kc % 2}"], [("wo", kc)], eng=("pool" if kc % 2 else "dve"))
        if layer == 0:
            ngb = kb.sb("ngb", [128, 2 * D], es=les)
            kb.load(ngb[:], ng_d.partition_broadcast(128), ["ng"], ["ngb"])
            if gathered:
                tinG = [kb.sb(f"tinG{i}", [128, 4, D], es=les) for i in range(2)]
                cands = [kb.sb(f"cand{q_}", [128, 4, D], es=les) for q_ in range(2)]
                selt = kb.sb("selt", [128, 4], es=les)
                kb.load(selt[:], aps["sel"], ["sel_d"], ["selt"])
            else:
                tin = [[kb.sb(f"tin{i}_{j}", [128, D], es=les) for j in range(4)] for i in range(2)]
        mo = [kb.sb(f"mo{i}", [128, K_], es=les) for i in range(2)]
        xr = [kb.sb(f"xr{i}", [128, D], es=les) for i in range(2)]
        yT = kb.sb("yT", [128, KC, 128], F32R, es=les)
        tmpa = kb.sb("tmpa", [128, D], es=les)
        u = kb.sb("u", [128, D], es=les)
        x1t = [kb.sb("x1t0", [128, D], es=les)] * 2
        st = kb.sb("st", [128, 4, 6], es=les)
        mv = kb.sb("mv", [128, 4, 4], es=les)
        lst = kb.sb("lst", [128, 2, 6], es=les)
        lmv = kb.sb("lmv", [128, 4], es=les)
        ss = kb.sb("ss", [128, 4], es=les)
        ptr = [kb.ps(f"ptr{i}", [128, 4, 128], es=les) for i in range(2)]
        pO = [kb.ps(f"pO{i}", [128, 512], es=les) for i in range(2)]
        def load_in(t):
            i = t % 2
            rows = slice(t * 128, (t + 1) * 128)
            kb.load(xr[i][:], xres[rows, :], ["xres"], [f"xr{i}"])
            if layer == 0:
                if gathered:
                    tk = [f"tin{i}_hm", f"tin{i}_ysd", f"tin{i}_o", f"tin{i}_z"]
                    own_rows = (nt - 2) * 128

                    def cand_ap(token0):
                        piece, off = token0 // 256, token0 % 256
                        return aps["EG"][piece * 1024:(piece + 1) * 1024, :].rearrange("(j r) f -> r j f", j=4)[off:off + 128]

                    if t < 2:
                        kb.load(tinG[i][:], cand_ap(t * 128), ["EG"], tk)
                    else:
                        for rho in range(4):
                            cand, ck = cands[rho % 2], f"cand{rho % 2}"
                            kb.load(cand[:], cand_ap(256 + rho * own_rows + (t - 2) * 128), ["EG"], [ck])
                            if rho == 0:
                                kb.ts(tinG[i][:], cand[:], selt[:, 0:1], ALU.mult, [ck, "selt"], tk)
                            else:
                                kb.stt(tinG[i][:].rearrange("p j f -> p (j f)"), cand[:].rearrange("p j f -> p (j f)"), selt[:, rho:rho + 1],
                                       tinG[i][:].rearrange("p j f -> p (j f)"), ALU.mult, ALU.add, [ck, "selt"] + tk, tk)
                else:
                    for tl, src, nm in zip(tin[i], (hm_d, ys_d, o_d, z_d), ("hm", "ysd", "o", "z")):
                        kb.load(tl[:], src[rows, :], [nm], [f"tin{i}_{nm}"])
            else:
                kb.load(mo[i][:], at_d[rows, :], ["attn"], [f"mo{i}a"])

        load_in(0)
        for t in range(nt):
            i = t % 2
            who = 1 if t < nctx else 0
            rows = slice(t * 128, (t + 1) * 128)
            if t + 1 < nt:
                load_in(t + 1)
            if layer == 0:
                if gathered:
                    hm3, ys3, o3, z3 = (tinG[i][:, :, q * 256:(q + 1) * 256] for q in range(4))
                else:
                    hm_t, ys_t, o_t, z_t = tin[i]
                    hm3, ys3, o3, z3 = (tl[:].rearrange("p (h d) -> p h d", d=256) for tl in (hm_t, ys_t, o_t, z_t))
                moA = mo[i][:, :D].rearrange("p (h d) -> p h d", d=256)
                moB = mo[i][:, D:].rearrange("p (h d) -> p h d", d=256)
                for h in range(4):
                    kb.S.op("dve", lambda e, h=h: e.bn_stats(out=st[:, h, :], in_=hm3[:, h, :]), reads=[f"tin{i}_hm"], writes=["st"])
                    kb.S.op("dve", lambda e, h=h: e.bn_aggr(out=mv[:, h, 0:2], in_=st[:, h, :]), reads=["st"], writes=["mv"])
                kb.act(mv[:, :, 2], mv[:, :, 1], AF.Sqrt, ["mv"], ["mv"], bias=LN_EPS)
                kb.recip(mv[:, :, 3], mv[:, :, 2], ["mv"], ["mv"])
                for h in range(4):
                    kb.ts(moA[:, h, :], hm3[:, h, :], mv[:, h, 0:1], ALU.subtract, [f"tin{i}_hm", "mv"], [f"mo{i}a"], s2=mv[:, h, 3:4], op1=ALU.mult)
                kb.tt(mo[i][:, :D], mo[i][:, :D], ngb[:, :D], ALU.mult, [f"mo{i}a", "ngb"], [f"mo{i}a"], eng="pool")
                kb.act(o3, o3, AF.Sigmoid, [f"tin{i}_o"], [f"tin{i}_o"])
                kb.tt(moA, moA, o3, ALU.mult, [f"mo{i}a", f"tin{i}_o"], [f"mo{i}a"])
                kb.act(z3, z3, AF.Silu, [f"tin{i}_z"], [f"tin{i}_z"])
                kb.tt(ys3, ys3, z3, ALU.mult, [f"tin{i}_ysd", f"tin{i}_z"], [f"tin{i}_ysd"])
                kb.S.op("act", lambda e: e.activation(out=z3, in_=ys3, func=AF.Square, accum_out=ss[:, 0:1]),
                        reads=[f"tin{i}_ysd"], writes=[f"tin{i}_z", "ss"])
                kb.act(ss[:, 1:2], ss[:, 0:1], AF.Sqrt, ["ss"], ["ss"], bias=RMS_EPS, scale=1.0 / D)
                kb.recip(ss[:, 2:3], ss[:, 1:2], ["ss"], ["ss"])
                kb.stt(moB, ys3, ss[:, 2:3], ngb[:, D:].rearrange("p (h d) -> p h d", d=256), ALU.mult, ALU.mult, [f"tin{i}_ysd", "ss", "ngb"], [f"mo{i}b"])
                mokeys = [f"mo{i}a", f"mo{i}b"]
            else:
                mokeys = [f"mo{i}a"]
            for g4 in range(KC // 4):
                pb = g4 % 2
                for kk in range(4):
                    kc = g4 * 4 + kk
                    kb.tr(ptr[pb][:, kk, :], mo[i][:, kc * 128:(kc + 1) * 128], ident[:], mokeys, [f"ptr{pb}"])
                kb.cp_rr(yT[:, g4 * 4:(g4 + 1) * 4, :], ptr[pb][:], [f"ptr{pb}"], [("yT", g4)])
            for nb in range(2):
                for kc in range(KC):
                    kb.mm(pO[nb][:], yT[:, kc, :], wo[:, kc, nb * 512:(nb + 1) * 512], [("yT", kc // 4), ("wo", kc)], [f"pO{nb}"],
                          start=(kc == 0), stop=(kc == KC - 1))
            for nb in range(2):
                cs = slice(nb * 512, (nb + 1) * 512)
                kb.tt(tmpa[:, cs], pO[nb][:], bc[:, who, cs], ALU.mult, [f"pO{nb}", "bc"], [("tmpa", nb)])
                kb.stt(u[:, cs], xr[i][:, cs], ALPHA, tmpa[:, cs], ALU.mult, ALU.add, [f"xr{i}", ("tmpa", nb)], ["u"])
            emit_ln(kb, u, "u", x1t[i][:], "x1t", bc[:, 2, :], bc[:, 3, :], lst, lmv, "l1")
            kb.store(x1_d[rows, :], x1t[i][:], ["x1t"], ["x1_d"])
        kb.S.barrier()

    fill_bc(1)
    ptok = TPP * 128
    rwf = kb.sb("rwf", [128, 8, 16])
    rwr = kb.sb("rwr", [128, 8, 16], F32R)
    kb.load(rwf[:], rw_d.rearrange("(k p) e -> p k e", p=128), ["rw_d"], ["rwf"])
    kb.cp(rwr[:], rwf[:], ["rwf"], ["rwr"])
    rbb = kb.sb("rbb", [128, 16])
    kb.load(rbb[:], rb_d.partition_broadcast(128), ["rb_d"], ["rbb"])
    ones16 = kb.sb("ones16", [16, 128])
    kb.memset(ones16[:], 1.0, ["ones16"])
    GTm = kb.sb("GTm", [16, ptok])
    hT = kb.sb("hT2", [128, 8, ptok], F32R)
    accT = kb.sb("accT", [128, 8, ptok])
    GT = kb.sb("GT", [16, ptok])
    gbc = kb.sb("gbc", [128, ptok])
    x1r = [kb.sb(f"x1r{i}", [128, D]) for i in range(2)]
    rt = kb.sb("rt", [128, 8, 16])
    rs = kb.sb("rs", [128, 16])
    g16 = kb.sb("g16", [128, 16])
    NWB = 3
    wgr = [kb.sb(f"wgr{i}", [128, 8, 256], F32R) for i in range(NWB)]
    wur = [kb.sb(f"wur{i}", [128, 8, 256], F32R) for i in range(NWB)]
    wdr = [kb.sb(f"wdr{i}", [128, 2, D], F32R) for i in range(NWB)]
    sg = [kb.sb(f"sg{i}", [128, TB]) for i in range(2)]
    AT = [kb.sb(f"AT{i}", [128, 2, TB], F32R) for i in range(2)]
    tmpb = kb.sb("tmpb", [128, D])
    u2 = tmpb
    xo = [kb.sb("xo0", [128, D])] * 2
    lst2 = kb.sb("lst2", [128, 2, 6])
    lmv2 = kb.sb("lmv2", [128, 4])
    ptr2 = [kb.ps(f"ptrb{i}", [128, 4, 128]) for i in range(2)]
    pG = [kb.ps(f"pG{i}", [128, 512]) for i in range(2)]
    pU = [kb.ps(f"pU{i}", [128, 512]) for i in range(2)]
    pD = [kb.ps(f"pD{i}", [128, 512]) for i in range(2)]

    units = [(e, q) for e in range(ne) for q in range(4)]

    def load_unit(ui):
        e, q = units[ui]
        b = ui % NWB
        kb.load(wgr[b][:].rearrange("p k f -> p (k f)"), wg_d[e, q], ["wg_d"], [f"wgr{b}"], eng="pool")
        kb.load(wur[b][:].rearrange("p k f -> p (k f)"), wu_d[e, q], ["wu_d"], [f"wur{b}"], eng="pool")
        kb.load(wdr[b][:].rearrange("p a n -> p (a n)"), wd_d[e, q], ["wd_d"], [f"wdr{b}"], eng="pool")

    for p in range(2):
        t0 = p * TPP
        for lt in range(TPP):
            t = t0 + lt
            i = lt % 2
            who = 1 if t < nctx else 0
            kb.load(x1r[i][:], x1_d[t * 128:(t + 1) * 128, :], ["x1_d"], [f"x1r{i}"])
            emit_transpose_mod(kb, x1r[i], f"x1r{i}", hT, "hT2", lt, ident[:], ptr2[i], mod, 0, 1, who, f"ptrb{i}")
            pR = pG[lt % 2]
            for k in range(8):
                kb.mm(pR[:, :16], hT[:, k, lt * 128:(lt + 1) * 128], rwr[:, k, :], [("hT2", lt, k), "rwr"], [f"pG{lt % 2}"],
                      start=(k == 0), stop=(k == 7))
            probs, sel, pr, gs = rt[:, 0, :], rt[:, 1, :], rt[:, 2:4, :].rearrange("p a b -> p (a b)"), rt[:, 4, :]
            kb.S.op("act", lambda e, pR=pR: e.activation(out=probs, in_=pR[:, :16], func=AF.Exp, accum_out=rs[:, 0:1]),
                    reads=[f"pG{lt % 2}"], writes=["rt0", "rs"])
            kb.recip(rs[:, 1:2], rs[:, 0:1], ["rs"], ["rs"])
            kb.ts(probs, probs, rs[:, 1:2], ALU.mult, ["rt0", "rs"], ["rt0"])
            kb.tt(sel, probs, rbb[:], ALU.add, ["rt0", "rbb"], ["rt1"])
            sel3 = sel.rearrange("p (g e) -> p g e", e=4)
            pr3 = pr[:, :24].rearrange("p (g e) -> p g e", e=6)
            for pi, (a, b_) in enumerate(((0, 1), (0, 2), (0, 3), (1, 2), (1, 3), (2, 3))):
                kb.tt(pr3[:, :, pi:pi + 1], sel3[:, :, a:a + 1], sel3[:, :, b_:b_ + 1], ALU.add, ["rt1"], ["rt2"])
            kb.S.op("dve", lambda e: e.tensor_reduce(out=gs[:, 0:4], in_=pr3, axis=AX.X, op=ALU.max), reads=["rt2"], writes=["rt4"])
            kb.S.op("dve", lambda e: e.tensor_reduce(out=rs[:, 2:3], in_=gs[:, 0:4], axis=AX.X, op=ALU.max), reads=["rt4"], writes=["rs"])
            kb.ts(gs[:, 4:8], gs[:, 0:4], rs[:, 2:3], ALU.is_ge, ["rt4", "rs"], ["rt4"])
            kb.ts(gs[:, 8:12], gs[:, 4:8], -1.0, ALU.add, ["rt4"], ["rt4"], s2=4.0, op1=ALU.mult)
            selm = rt[:, 5, :]
            selm3 = selm.rearrange("p (g e) -> p g e", e=4)
            for g in range(4):
                kb.ts(selm3[:, g, :], sel3[:, g, :], gs[:, 4 + g:5 + g], ALU.mult, ["rt1", "rt4"], ["rt5"], s2=gs[:, 8 + g:9 + g], op1=ALU.add)
            top8 = rt[:, 6, 0:8]
            kb.S.op("dve", lambda e: e.max(out=top8, in_=selm), reads=["rt5"], writes=["rt6"])
            em = rt[:, 7, :]
            kb.ts(em, selm, rt[:, 6, 1:2], ALU.is_ge, ["rt5", "rt6"], ["rt7"])
            kb.tt(em, em, probs, ALU.mult, ["rt7", "rt0"], ["rt7"])
            kb.S.op("dve", lambda e: e.tensor_reduce(out=rs[:, 3:4], in_=em, axis=AX.X, op=ALU.add), reads=["rt7"], writes=["rs"])
            kb.recip(rs[:, 4:5], rs[:, 3:4], ["rs"], ["rs"])
            kb.ts(g16[:], em, rs[:, 4:5], ALU.mult, ["rt7", "rs"], ["g16"])
            kb.tr(ptr2[i][:16, 0, :], g16[:], ident[:], ["g16"], [f"ptrb{i}"])
            kb.cp(GT[:, lt * 128:(lt + 1) * 128], ptr2[i][:16, 0, :], [f"ptrb{i}"], ["GT"])
        load_unit(0)
        load_unit(1)
        for ui, (e, q) in enumerate(units):
            b = ui % NWB
            if ui + 2 < len(units):
                load_unit(ui + 2)
            if q == 0:
                kb.ts(GTm[:], GT[:], ident[:16, e:e + 1], ALU.mult, ["GT", "ident"], ["GTm"], eng="pool")
                for tb in range(NTB):
                    ts_ = slice(tb * TB, (tb + 1) * TB)
                    kb.mm(pD[tb % 2][:, :TB], ones16[:], GTm[:, ts_], ["ones16", "GTm"], [f"pD{tb % 2}"])
                    kb.cp(gbc[:, ts_], pD[tb % 2][:, :TB], [f"pD{tb % 2}"], ["gbc"], eng="act")
            def emit_GU(tb):
                ts_ = slice(tb * TB, (tb + 1) * TB)
                ab = tb % 2
                for fc in range(2):
                    fs = slice(fc * 128, (fc + 1) * 128)
                    pb = fc
                    for k in range(8):
                        kb.mm(pG[pb][:, :TB], wgr[b][:, k, fs], hT[:, k, ts_], [f"wgr{b}", "hT2all"], [f"pG{pb}"], start=(k == 0), stop=(k == 7))
                    for k in range(8):
                        kb.mm(pU[pb][:, :TB], wur[b][:, k, fs], hT[:, k, ts_], [f"wur{b}", "hT2all"], [f"pU{pb}"], start=(k == 0), stop=(k == 7))
                    kb.act(sg[pb][:], pG[pb][:, :TB], AF.Silu, [f"pG{pb}"], [f"sg{pb}"])
                    kb.tt(sg[pb][:], sg[pb][:], pU[pb][:, :TB], ALU.mult, [f"sg{pb}", f"pU{pb}"], [f"sg{pb}"])
                    kb.tt(AT[ab][:, fc, :], sg[pb][:], gbc[:, ts_], ALU.mult, [f"sg{pb}", "gbc"], [f"AT{ab}"], eng="pool")

            def emit_Dn(tb):
                ts_ = slice(tb * TB, (tb + 1) * TB)
                ab = tb % 2
                for dc in range(8):
                    pb = dc % 2
                    for fc in range(2):
                        kb.mm(pD[pb][:, :TB], wdr[b][:, fc, dc * 128:(dc + 1) * 128], AT[ab][:, fc, :], [f"wdr{b}", f"AT{ab}"], [f"pD{pb}"],
                              start=(fc == 0), stop=(fc == 1))
                    if ui == 0:
                        kb.cp(accT[:, dc, ts_], pD[pb][:, :TB], [f"pD{pb}"], [("accT", dc, tb)])
                    else:
                        kb.tt(accT[:, dc, ts_], accT[:, dc, ts_], pD[pb][:, :TB], ALU.add, [f"pD{pb}", ("accT", dc, tb)], [("accT", dc, tb)])

            for tb in range(NTB + 1):
                if tb < NTB:
                    emit_GU(tb)
                if tb >= 1:
                    emit_Dn(tb - 1)
        for lt in range(TPP):
            t = t0 + lt
            i = lt % 2
            who = 1 if t < nctx else 0
            tb = lt * 128 // TB
            kb.load(x1r[i][:], x1_d[t * 128:(t + 1) * 128, :], ["x1_d"], [f"x1r{i}"])
            for nb in range(2):
                for kk in range(4):
                    dc = nb * 4 + kk
                    kb.tr(pD[nb][:, kk * 128:(kk + 1) * 128], accT[:, dc, lt * 128:(lt + 1) * 128], ident[:], [("accT", dc, tb)], [f"pD{nb}"])
            for nb in range(2):
                cs = slice(nb * 512, (nb + 1) * 512)
                kb.tt(tmpb[:, cs], pD[nb][:], bc[:, who, cs], ALU.mult, [f"pD{nb}", "bc"], [("tmpb", nb)])
                kb.stt(u2[:, cs], x1r[i][:, cs], ALPHA, tmpb[:, cs], ALU.mult, ALU.add, [f"x1r{i}", ("tmpb", nb)], [("tmpb", nb)])
            emit_ln(kb, u2, [("tmpb", 0), ("tmpb", 1)], xo[i][:], "xo", bc[:, 2, :], bc[:, 3, :], lst2, lmv2, "l2")
            dst = aps["xout_fn"](t)
            if dst is not None:
                kb.store(dst, xo[i][:], ["xo"], ["xout"])


def moe_layout(kind, w):
    E = w.shape[0]
    if kind in ("w_gate", "w_up"):
        return np.ascontiguousarray(w.reshape(E, 8, 128, 4, 256).transpose(0, 3, 2, 1, 4).reshape(E, 4, 128, 2048))
    return np.ascontiguousarray(w.reshape(E, 4, 2, 128, 1024).transpose(0, 1, 3, 2, 4).reshape(E, 4, 128, 2048))


def esel_const():
    m = np.zeros((16, 16, 128), np.float32)
    for e in range(16):
        m[e, e, :] = 1.0
    return np.ascontiguousarray(m.transpose(1, 0, 2).reshape(16, 16 * 128))


def maps_P_common(inp, layer, b):
    return {
        "cpair": cpair_pp(inp, b),
        "ada_w": np.ascontiguousarray(inp["ada_w"][layer]),
        "ada_b": pp_layout(inp["ada_b"][layer]),
        "ada_b_row": np.ascontiguousarray(inp["ada_b"][layer].reshape(1, -1)),
        "lnp": np.ascontiguousarray(np.concatenate([inp["ln_g"][layer, 0], inp["ln_b"][layer, 0], inp["ln_g"][layer, 1], inp["ln_b"][layer, 1]]).reshape(1, -1)),
        "cst_d": consts()["ident"],
        "router_w": np.ascontiguousarray(inp["router_w"]),
        "router_b": np.ascontiguousarray(inp["router_b"].reshape(1, -1)),
        "w_gate": np.ascontiguousarray(inp["moe_w_gate"][layer]),
        "w_up": np.ascontiguousarray(inp["moe_w_up"][layer]),
        "w_down": np.ascontiguousarray(inp["moe_w_down"][layer]),
    }


def build_Q(nt=NT):
    tok = nt * 128
    kb = KB()
    aps = dict(xin=kb.din("xin", [tok, D]), cpair=kb.din("cpair", [128, 16]), ada_w=kb.din("ada_w", [D, 6 * D]), ada_b=kb.din("ada_b", [128, 48]),
               w_in=kb.din("w_in", [D, 1536]), cst_d=kb.din("cst_d", [128, 128]), gqk=kb.din("gqk", [1, 1280]), cossin=kb.din("cossin", [tok, 2, 640]),
               qkv=kb.dout("qkv", [tok, 1536]))
    with kb.scope():
        emit_Q(kb, aps, nt)
    return kb.done()


def emit_Q(kb, aps, nt=NT):
    tok = nt * 128
    xin, cpair, ada_w, ada_b, w_in, cst_d, gq_d, cs_d, qkv = (aps[k] for k in ("xin", "cpair", "ada_w", "ada_b", "w_in", "cst_d", "gqk", "cossin", "qkv"))

    ident = kb.sb("ident", [128, 128])
    kb.load(ident[:], cst_d, ["cst_d"], ["ident"])
    mod = emit_mod_vectors(kb, ada_w, ada_b, cpair, [0, 1], "m", kb.es)
    kb.ts(mod[:, 1], mod[:, 1], 1.0, ALU.add, [("mmod", 1)], ["mod"])
    gq = kb.sb("gq", [128, 1280])
    kb.load(gq[:], gq_d.partition_broadcast(128), ["gq_d"], ["gq"])
    kb.S.barrier()
    wr = kb.sb("wr", [128, 8, 1536], F32R)
    wtmp = [kb.sb(f"wtmp{i}", [128, 1536]) for i in range(2)]
    for k in range(8):
        kb.load(wtmp[k % 2][:], w_in[k * 128:(k + 1) * 128, :], ["w_in"], [f"wtmp{k % 2}"])
        kb.cp(wr[:, k, :], wtmp[k % 2][:], [f"wtmp{k % 2}"], [("wr", k)], eng=("pool" if k % 2 else "dve"))
    hT = kb.sb("hT", [128, 8, 256], F32R)
    xt = [kb.sb(f"xt{i}", [128, D]) for i in range(2)]
    cs = [kb.sb(f"cs{i}", [128, 2, 640]) for i in range(2)]
    qk = kb.sb("qk", [128, 1536])
    sq = kb.sb("sq", [128, 1280])
    ms = kb.sb("ms", [128, 3, 10])
    ro = [kb.sb(f"ro{i}", [128, 1536]) for i in range(2)]
    t1 = kb.sb("t1", [128, 10, 64])
    t2 = kb.sb("t2", [128, 10, 64])
    ptr = [kb.ps(f"ptr{i}", [128, 4, 128]) for i in range(2)]
    po = [kb.ps(f"po{i}", [128, 512]) for i in range(3)]
    kb.load(xt[0][:], xin[0:128, :], ["xin"], ["xt0"])
    kb.load(cs[0][:], cs_d[0:128], ["cs_d"], ["cs0"])
    for t in range(nt):
        i = t % 2
        who = 1 if t < 2 else 0
        rows = slice(t * 128, (t + 1) * 128)
        if t + 1 < nt:
            kb.load(xt[1 - i][:], xin[(t + 1) * 128:(t + 2) * 128, :], ["xin"], [f"xt{1 - i}"])
            kb.load(cs[1 - i][:], cs_d[(t + 1) * 128:(t + 2) * 128], ["cs_d"], [f"cs{1 - i}"])
        emit_transpose_mod(kb, xt[i], f"xt{i}", hT, "hT", i, ident[:], ptr[i], mod, 0, 1, who, f"ptr{i}")
        for nb in range(3):
            for k in range(8):
                kb.mm(po[nb][:], hT[:, k, i * 128:(i + 1) * 128], wr[:, k, nb * 512:(nb + 1) * 512], [("hT", i, k), ("wr", k)], [f"po{nb}"],
                      start=(k == 0), stop=(k == 7))
            kb.cp_rr(qk[:, nb * 512:(nb + 1) * 512], po[nb][:], [f"po{nb}"], [("qk", nb)])
        qkk = [("qk", 0), ("qk", 1), ("qk", 2)]
        kb.tt(sq[:], qk[:, :1280], qk[:, :1280], ALU.mult, qkk, ["sq"], eng="pool")
        kb.S.op("dve", lambda e: e.tensor_reduce(out=ms[:, 0, :], in_=sq[:].rearrange("p (h d) -> p h d", d=128), axis=AX.X, op=ALU.add), reads=["sq"], writes=["ms"])
        kb.act(ms[:, 1, :], ms[:, 0, :], AF.Sqrt, ["ms"], ["ms"], bias=RMS_EPS, scale=1.0 / 128)
        kb.recip(ms[:, 2, :], ms[:, 1, :], ["ms"], ["ms"])
        for h in range(10):
            hs = slice(h * 128, (h + 1) * 128)
            kb.ts(qk[:, hs], qk[:, hs], ms[:, 2, h:h + 1], ALU.mult, qkk + ["ms"], qkk)
        kb.tt(qk[:, :1280], qk[:, :1280], gq[:], ALU.mult, qkk + ["gq"], qkk)
        q3 = qk[:, :1280].rearrange("p (h d) -> p h d", d=128)
        r3 = ro[i][:, :1280].rearrange("p (h d) -> p h d", d=128)
        C3 = cs[i][:, 0, :].rearrange("p (h d) -> p h d", d=64)
        S3 = cs[i][:, 1, :].rearrange("p (h d) -> p h d", d=64)
        x1, x2 = q3[:, :, 0:64], q3[:, :, 64:128]
        rk = [f"ro{i}"]
        kb.tt(t1[:], x1, C3, ALU.mult, qkk + [f"cs{i}"], ["t1"])
        kb.tt(t2[:], x2, S3, ALU.mult, qkk + [f"cs{i}"], ["t2"], eng="pool")
        kb.tt(r3[:, :, 0:64], t1[:], t2[:], ALU.subtract, ["t1", "t2"], rk)
        kb.tt(t1[:], x2, C3, ALU.mult, qkk + [f"cs{i}"], ["t1"])
        kb.tt(t2[:], x1, S3, ALU.mult, qkk + [f"cs{i}"], ["t2"], eng="pool")
        kb.tt(r3[:, :, 64:128], t1[:], t2[:], ALU.add, ["t1", "t2"], rk)
        kb.cp(ro[i][:, 1280:], qk[:, 1280:], qkk, rk, eng="act")
        kb.store(qkv[rows, :], ro[i][:], rk, ["qkv"])
        if "kv_own" in aps and t >= 2:
            kb.store(aps["kv_own"][(t - 2) * 128:(t - 1) * 128, :], ro[i][:, 1024:1536], rk, ["kv_own"])


def rope_tables(nt_lat_rows):
    t = np.asarray(nt_lat_rows)
    row = (t // 64).astype(np.float32)
    col = (t % 64).astype(np.float32)
    inv = (np.float32(10000.0) ** (-np.arange(32, dtype=np.float32) / np.float32(32))).astype(np.float32)
    ang = np.concatenate([row[:, None] * inv, col[:, None] * inv], -1).astype(np.float32)
    return np.cos(ang).astype(np.float32), np.sin(ang).astype(np.float32)


def cossin_for(core, nt=NT):
    r = core % 4
    lat = np.arange(r * (nt - 2) * 128, (r + 1) * (nt - 2) * 128)
    c, s = rope_tables(lat)
    c = np.concatenate([np.ones((256, 64), np.float32), c], 0)
    s = np.concatenate([np.zeros((256, 64), np.float32), s], 0)
    return np.ascontiguousarray(np.stack([np.tile(c, (1, 10)), np.tile(s, (1, 10))], axis=1))


def build_D(nq=16, nkc=66):
    NQ = nq * 128
    NK = nkc * 128
    QB = 4 if nq % 4 == 0 else nq
    kb = KB()
    qT_d = kb.din("qT", [8, 128, NQ])
    kT_d = kb.din("kT", [2, 128, NK])
    v_d = kb.din("vaug", [2, 128, nkc * 130])
    attn = kb.dout("attn", [128, nq * D])
    KTr = kb.sb("KTr", [128, NK], F32R)
    Vr = kb.sb("Vr", [128, nkc, 130], F32R)
    QTr = kb.sb("QTr", [128, NQ], F32R)
    stg = [kb.sb(f"stg{i}", [128, 1040]) for i in range(2)]
    PTt = [kb.sb(f"PTt{i}", [128, QB * 128], F32R) for i in range(2)]
    OT = kb.sb("OT", [128, nq, D])
    rc = kb.sb("rc", [128, 4])
    pS = [kb.ps(f"pS{i}", [128, 512]) for i in range(2)]
    pO = [kb.ps(f"pO{i}", [128, 130]) for i in range(4)]
    si = 0
    for g in range(2):
        for c0 in range(0, NK, 1024):
            w = min(1024, NK - c0)
            kb.load(stg[si % 2][:, :w], kT_d[g, :, c0:c0 + w], ["kT_d"], [f"stg{si % 2}"])
            kb.cp(KTr[:, c0:c0 + w], stg[si % 2][:, :w], [f"stg{si % 2}"], ["KTr"], eng=("pool" if si % 2 else "dve"))
            si += 1
        for c0 in range(0, nkc, 8):
            w = min(8, nkc - c0)
            kb.load(stg[si % 2][:, :w * 130], v_d[g, :, c0 * 130:(c0 + w) * 130], ["v_d"], [f"stg{si % 2}"])
            kb.cp(Vr[:, c0:c0 + w, :], stg[si % 2][:, :w * 130].rearrange("p (c f) -> p c f", f=130), [f"stg{si % 2}"], ["Vr"], eng=("pool" if si % 2 else "dve"))
            si += 1
        for hh in range(4):
            h = g * 4 + hh
            for c0 in range(0, NQ, 1024):
                w = min(1024, NQ - c0)
                kb.load(stg[si % 2][:, :w], qT_d[h, :, c0:c0 + w], ["qT_d"], [f"stg{si % 2}"])
                kb.cp(QTr[:, c0:c0 + w], stg[si % 2][:, :w], [f"stg{si % 2}"], ["QTr"], eng=("pool" if si % 2 else "dve"))
                si += 1
            for qb in range(nq // QB):
                qs = slice(qb * QB * 128, (qb + 1) * QB * 128)
                for kc in range(nkc):
                    pb = kc % 2
                    kb.mm(pS[pb][:, :QB * 128], KTr[:, kc * 128:(kc + 1) * 128], QTr[:, qs], ["KTr", "QTr"], [f"pS{pb}"])
                    kb.act(PTt[pb][:], pS[pb][:, :QB * 128], AF.Exp, [f"pS{pb}"], [f"PTt{pb}"], scale=128 ** -0.5)
                    for jq in range(QB):
                        kb.mm(pO[jq][:], PTt[pb][:, jq * 128:(jq + 1) * 128], Vr[:, kc, :], [f"PTt{pb}", "Vr"], [f"pO{jq}"],
                              start=(kc == 0), stop=(kc == nkc - 1))
                for jq in range(QB):
                    qt = qb * QB + jq
                    kb.recip(rc[:, jq:jq + 1], pO[jq][:, 128:129], [f"pO{jq}"], [("rc", jq)])
                    kb.ts(OT[:, qt, h * 128:(h + 1) * 128], pO[jq][:, 0:128], rc[:, jq:jq + 1], ALU.mult, [f"pO{jq}", ("rc", jq)], [("OT", qt)])
    at3 = attn.rearrange("p (c f) -> p c f", f=D)
    for c0 in range(0, nq, 8):
        c1 = min(nq, c0 + 8)
        kb.store(at3[:, c0:c1, :], OT[:, c0:c1, :], [("OT", q) for q in range(c0, c1)], ["attn"])
    return kb.done()


TM_W = 908
FM_W = 768


def emit_S1(kb, aps, nch):
    T = nch * 128
    xb, cpair, ada_w, ada_b, w_d, cst_d = (aps[k] for k in ("xb", "cpair", "ada_w", "ada_b", "w_in_j", "cst_d"))
    ident = kb.sb("ident", [128, 128])
    kb.load(ident[:], cst_d, ["cst_d"], ["ident"])
    mod = emit_mod_vectors(kb, ada_w, ada_b, cpair, [0, 1], "m", kb.es)
    kb.ts(mod[:, 1], mod[:, 1], 1.0, ALU.add, [("mmod", 1)], ["mod"])
    kb.S.barrier()
    W = TM_W + FM_W
    wr = kb.sb("wr", [128, 8, W], F32R)
    wtmp = [kb.sb(f"wtmp{i}", [128, W]) for i in range(2)]
    for k in range(8):
        kb.load(wtmp[k % 2][:], w_d[k * 128:(k + 1) * 128, :], ["w_d"], [f"wtmp{k % 2}"])
        kb.cp(wr[:, k, :], wtmp[k % 2][:], [f"wtmp{k % 2}"], [("wr", k)], eng=("pool" if k % 2 else "dve"))
    hT = kb.sb("hT", [128, 8, 256], F32R)
    xt = [kb.sb(f"xt{i}", [128, D]) for i in range(2)]
    tmo = [kb.sb(f"tmo{i}", [128, TM_W]) for i in range(2)]
    fmo = [kb.sb(f"fmo{i}", [128, 6, 128]) for i in range(2)]
    ptr = [kb.ps(f"ptr{i}", [128, 4, 128]) for i in range(2)]
    pt = [kb.ps(f"pt{i}", [128, 512]) for i in range(2)]
    pf = [kb.ps("pf0", [128, 4, 128]), kb.ps("pf1", [128, 2, 128])]
    E = aps["E"]
    kb.load(xt[0][:], xb[0:128, :], ["xb"], ["xt0"])
    for t in range(nch):
        i = t % 2
        who = 1 if t < 2 else 0
        rows = slice(t * 128, (t + 1) * 128)
        if t + 1 < nch:
            kb.load(xt[1 - i][:], xb[(t + 1) * 128:(t + 2) * 128, :], ["xb"], [f"xt{1 - i}"])
        emit_transpose_mod(kb, xt[i], f"xt{i}", hT, "hT", i, ident[:], ptr[i], mod, 0, 1, who, f"ptr{i}")
        hk = [("hT", i, k) for k in range(8)]
        for nb, (c0, cw) in enumerate(((0, 512), (512, TM_W - 512))):
            for k in range(8):
                kb.mm(pt[nb][:, :cw], hT[:, k, i * 128:(i + 1) * 128], wr[:, k, c0:c0 + cw], [("hT", i, k), ("wr", k)], [f"pt{nb}"], start=(k == 0), stop=(k == 7))
            kb.cp_rr(tmo[i][:, c0:c0 + cw], pt[nb][:, :cw], [f"pt{nb}"], [(f"tmo{i}", nb)])
        for fc in range(6):
            tl, sl_ = (pf[0], fc) if fc < 4 else (pf[1], fc - 4)
            for k in range(8):
                kb.mm(tl[:, sl_, :], wr[:, k, TM_W + fc * 128:TM_W + (fc + 1) * 128], hT[:, k, i * 128:(i + 1) * 128], [("hT", i, k), ("wr", k)],
                      ["pf0" if fc < 4 else "pf1"], start=(k == 0), stop=(k == 7))
        kb.cp_rr(fmo[i][:, 0:4, :], pf[0][:], ["pf0"], [(f"fmo{i}", 0)])
        kb.cp_rr(fmo[i][:, 4:6, :], pf[1][:], ["pf1"], [(f"fmo{i}", 1)])
        k0, k1 = [(f"tmo{i}", 0)], [(f"tmo{i}", 1)]
        kb.store(aps["v_s"][rows, :], tmo[i][:, 0:256], k0, ["v_s"])
        kb.store(E[rows, 512:768], tmo[i][:, 256:512], k0, ["E_o"])
        kb.store(E[rows, 768:1024], tmo[i][:, 512:768], k1, ["E_z"])
        kb.store(aps["ktok_s"][rows, :], tmo[i][:, 768:896], k1, ["ktok_s"])
        kb.store(aps["gates_s"][:, t * 4:(t + 1) * 4], tmo[i][:, 896:900], k1, ["gates_s"])
        kb.store(aps["dtr_s"][:, t * 8:(t + 1) * 8], tmo[i][:, 900:908], k1, ["dtr_s"])
        f0, f1 = [(f"fmo{i}", 0)], [(f"fmo{i}", 1)]
        kb.store(aps["qT_s"][:, rows], fmo[i][:, 0, :], f0, ["qT_s"])
        kb.store(aps["kT_s"][:, rows], fmo[i][:, 1, :], f0, ["kT_s"])
        kb.store(aps["xbcT_s"][0:256, rows].rearrange("(a p) t -> p a t", p=128), fmo[i][:, 2:4, :], f0, ["xbcT_s"])
        kb.store(aps["xbcT_s"][256:512, rows].rearrange("(a p) t -> p a t", p=128), fmo[i][:, 4:6, :], f1, ["xbcT_s"])


def emit_D2(kb, aps, nq, nkc):
    NQ = nq * 128
    NK = nkc * 128
    QB = 4
    qkv, kvg, attn_s, cst_d = aps["qkv"], aps["kvg"], aps["attn_s"], aps["cst_d"]
    ident = kb.sb("ident", [128, 128])
    kb.load(ident[:], cst_d, ["cst_d"], ["ident"])
    KTr = kb.sb("KTr", [128, NK], F32R)
    Vr = kb.sb("Vr", [128, nkc, 130], F32R)
    QTr = kb.sb("QTr", [128, NQ], F32R)
    stg = [kb.sb(f"stg{i}", [128, 4, 128]) for i in range(3)]
    PTt = [kb.sb(f"PTt{i}", [128, QB * 128], F32R) for i in range(2)]
    OT = kb.sb("OT", [128, nq, D])
    rc = kb.sb("rc", [128, 4])
    zt = kb.sb("zt", [128, D])
    pS = [kb.ps(f"pS{i}", [128, 512]) for i in range(2)]
    pO = [kb.ps(f"pO{i}", [128, 130]) for i in range(4)]
    pX = [kb.ps(f"pX{i}", [128, 4, 128]) for i in range(2)]
    kb.memset(zt[:], 0.0, ["zt"])
    for c in range(2):
        kb.store(attn_s[c * 128:(c + 1) * 128, :], zt[:], ["zt"], ["attn_s"])
    onez = kb.sb("onez", [128, nkc, 2])
    kb.memset(onez[:, :, 0:1], 1.0, ["onez"])
    kb.memset(onez[:, :, 1:2], 0.0, ["onez"])
    kb.cp(Vr[:, :, 128:130], onez[:], ["onez"], [("Vr", "one"), ("Vr", "zero")])
    si = 0

    def key_groups():
        yield 0, 2, None
        for c0 in range(2, nkc, 4):
            yield c0, 4, (c0 - 2) * 128

    for g in range(2):
        for (c0, n, krow) in key_groups():
            for which, col0 in (("k", g * 128), ("v", 256 + g * 128)):
                b_ = si % 3
                si += 1
                if krow is None:
                    src = qkv[0:256, 1024 + col0:1024 + col0 + 128].rearrange("(c p) f -> p c f", p=128)
                else:
                    src = kvg[krow:krow + n * 128, col0:col0 + 128].rearrange("(c p) f -> p c f", p=128)
                kb.load(stg[b_][:, :n, :], src, ["qkv", "kvg"], [f"stg{b_}"])
                if which == "v":
                    kb.cp(Vr[:, c0:c0 + n, 0:128], stg[b_][:, :n, :], [f"stg{b_}"], ["Vr"], eng=("pool" if si % 2 else "dve"))
                else:
                    px = pX[(si // 2) % 2]
                    pk = f"pX{(si // 2) % 2}"
                    for a in range(n):
                        kb.tr(px[:, a, :], stg[b_][:, a, :], ident[:], [f"stg{b_}"], [pk])
                    kb.cp_rr(KTr[:, c0 * 128:(c0 + n) * 128], px[:, :n, :].rearrange("p a t -> p (a t)"), [pk], ["KTr"])
        for hh in range(4):
            h = g * 4 + hh
            for q0 in range(0, nq, 4):
                b_ = si % 3
                si += 1
                kb.load(stg[b_][:], qkv[256 + q0 * 128:256 + (q0 + 4) * 128, h * 128:(h + 1) * 128].rearrange("(c p) f -> p c f", p=128), ["qkv"], [f"stg{b_}"])
                px = pX[si % 2]
                pk = f"pX{si % 2}"
                for a in range(4):
                    kb.tr(px[:, a, :], stg[b_][:, a, :], ident[:], [f"stg{b_}"], [pk])
                kb.cp_rr(QTr[:, q0 * 128:(q0 + 4) * 128], px[:].rearrange("p a t -> p (a t)"), [pk], ["QTr"])
            for qb in range(nq // QB):
                qs = slice(qb * QB * 128, (qb + 1) * QB * 128)
                def s_mm(kc_):
                    kb.mm(pS[kc_ % 2][:, :QB * 128], KTr[:, kc_ * 128:(kc_ + 1) * 128], QTr[:, qs], ["KTr", "QTr"], [f"pS{kc_ % 2}"])

                s_mm(0)
                for kc in range(nkc):
                    pb = kc % 2
                    if kc + 1 < nkc:
                        s_mm(kc + 1)
                    kb.act(PTt[pb][:], pS[pb][:, :QB * 128], AF.Exp, [f"pS{pb}"], [f"PTt{pb}"], scale=128 ** -0.5)
                    for jq in range(QB):
                        kb.mm(pO[jq][:], PTt[pb][:, jq * 128:(jq + 1) * 128], Vr[:, kc, :], [f"PTt{pb}", "Vr", ("Vr", "one"), ("Vr", "zero")], [f"pO{jq}"],
                              start=(kc == 0), stop=(kc == nkc - 1))
                for jq in range(QB):
                    qt = qb * QB + jq
                    kb.recip(rc[:, jq:jq + 1], pO[jq][:, 128:129], [f"pO{jq}"], [("rc", jq)])
                    kb.ts(OT[:, qt, h * 128:(h + 1) * 128], pO[jq][:, 0:128], rc[:, jq:jq + 1], ALU.mult, [f"pO{jq}", ("rc", jq)], [("OT", qt)])
    at3 = attn_s[256:256 + NQ, :].rearrange("(c p) f -> p c f", p=128)
    for c0 in range(0, nq, 4):
        kb.store(at3[:, c0:c0 + 4, :], OT[:, c0:c0 + 4, :], [("OT", q) for q in range(c0, c0 + 4)], ["attn_s"])


def build_fused(ntown=16, stop=99):
    nt = ntown + 2
    tok = nt * 128
    nlat = 4 * ntown
    NCH = nlat + 2
    T = NCH * 128
    kb = KB()
    nc = kb.nc
    W = TM_W + FM_W
    I = dict(
        xb=kb.din("xb", [T, D]), xown=kb.din("xown", [tok, D]), cpair=kb.din("cpair", [128, 16]),
        ada_w0=kb.din("ada_w0", [D, 6 * D]), ada_b0=kb.din("ada_b0", [128, 48]), ada_b0_row=kb.din("ada_b0_row", [1, 6 * D]),
        ada_w1=kb.din("ada_w1", [D, 6 * D]), ada_b1=kb.din("ada_b1", [128, 48]), ada_b1_row=kb.din("ada_b1_row", [1, 6 * D]),
        w_in_j=kb.din("w_in_j", [D, W]), cst_d=kb.din("cst_d", [128, 128]),
        gb=kb.din("gb", [128, 4]), convw=kb.din("convw", [128, 16]), convb=kb.din("convb", [128, 4]), dtb=kb.din("dtb", [128, 8]),
        alog=kb.din("alog", [128, 8]), dsk=kb.din("dsk", [128, 4]), cst=kb.din("cst", [128, 6, 128]),
        lnp0=kb.din("lnp0", [1, 4 * D]), lnp1=kb.din("lnp1", [1, 4 * D]), w_out0=kb.din("w_out0", [2048, D]), w_out1=kb.din("w_out1", [D, D]),
        ng=kb.din("ng", [1, 2 * D]), router_w=kb.din("router_w", [D, 16]), router_b=kb.din("router_b", [1, 16]),
        wg0=kb.din("wg0", [NE, 4, 128, 2048]), wu0=kb.din("wu0", [NE, 4, 128, 2048]), wd0=kb.din("wd0", [NE, 4, 128, 2048]),
        wg1=kb.din("wg1", [NE, 4, 128, 2048]), wu1=kb.din("wu1", [NE, 4, 128, 2048]), wd1=kb.din("wd1", [NE, 4, 128, 2048]),
        at_w_in=kb.din("at_w_in", [D, 1536]), gqk=kb.din("gqk", [1, 1280]), cossin=kb.din("cossin", [tok, 2, 640]),
    )
    I["sel"] = kb.din("sel", [128, 4])
    out = kb.dout("out", [ntown * 128, D])
    groups = [[0, 1, 2, 3], [4, 5, 6, 7]]
    Sx = {n: kb.dscr(n, sh) for n, sh in dict(
        qT_s=[128, T], kT_s=[128, T], ktok_s=[T, 128], v_s=[T, 256], gates_s=[128, NCH * 4], dtr_s=[128, NCH * 8], xbcT_s=[512, T], xpost=[512, T],
        E=[T, D], EG=[4 * T, D], x1a=[tok, D], x0_s=[tok, D], qkv_s=[tok, 1536], kv_own=[ntown * 128, 512], kvg=[4 * ntown * 128, 512],
        attn_s=[tok, D], x1b=[tok, D]).items()}

    with kb.scope():
        emit_S1(kb, dict(xb=I["xb"], cpair=I["cpair"], ada_w=I["ada_w0"], ada_b=I["ada_b0"], w_in_j=I["w_in_j"], cst_d=I["cst_d"], **Sx), NCH)
    if stop < 1:
        return kb.done()
    E3 = Sx["E"].rearrange("(c p) f -> p c f", p=128)
    with kb.scope():
        emit_B(kb, dict(qT=Sx["qT_s"], kT=Sx["kT_s"], ktok=Sx["ktok_s"], v=Sx["v_s"], gates=Sx["gates_s"], xbcT=Sx["xbcT_s"], dtr=Sx["dtr_s"],
                        gb=I["gb"], convw=I["convw"], convb=I["convb"], dtb=I["dtb"], alog=I["alog"], dsk=I["dsk"], cst=I["cst"],
                        E=Sx["E"], xpost=Sx["xpost"]), nlat)
    if stop < 2:
        return kb.done()
    for k in range(T // 256):
        kb.collective("AllGather", Sx["E"][k * 256:(k + 1) * 256, :], Sx["EG"][k * 1024:(k + 1) * 1024, :], groups, ["hm_o", "ys_o", "E_o", "E_z"], "EG")
    kb.S.barrier()
    if stop < 3:
        return kb.done()
    with kb.scope():
        emit_P(kb, 0, dict(xres=I["xown"], cpair=I["cpair"], ada_w=I["ada_w0"], ada_b=I["ada_b0"], ada_b_row=I["ada_b0_row"], lnp=I["lnp0"],
                           w_out=I["w_out0"], cst_d=I["cst_d"], router_w=I["router_w"], router_b=I["router_b"], w_gate=I["wg0"], w_up=I["wu0"],
                           w_down=I["wd0"], ng=I["ng"], EG=Sx["EG"], sel=I["sel"], x1_d=Sx["x1a"],
                           xout_fn=lambda t: Sx["x0_s"][t * 128:(t + 1) * 128, :]), nt, NE)
    if stop < 4:
        return kb.done()
    with kb.scope():
        emit_Q(kb, dict(xin=Sx["x0_s"], cpair=I["cpair"], ada_w=I["ada_w1"], ada_b=I["ada_b1"], w_in=I["at_w_in"], cst_d=I["cst_d"],
                        gqk=I["gqk"], cossin=I["cossin"], qkv=Sx["qkv_s"], kv_own=Sx["kv_own"]), nt)
    for m in range(ntown * 128 // 512):
        kb.collective("AllGather", Sx["kv_own"][m * 512:(m + 1) * 512, :], Sx["kvg"][m * 2048:(m + 1) * 2048, :], groups, ["kv_own"], "kvg")
    kb.S.barrier()
    if stop < 5:
        return kb.done()
    with kb.scope():
        emit_D2(kb, dict(qkv=Sx["qkv_s"], kvg=Sx["kvg"], attn_s=Sx["attn_s"], cst_d=I["cst_d"]), ntown, nlat + 2)
    with kb.scope():
        emit_P(kb, 1, dict(xres=Sx["x0_s"][256:, :], cpair=I["cpair"], ada_w=I["ada_w1"], ada_b=I["ada_b1"], ada_b_row=I["ada_b1_row"], lnp=I["lnp1"],
                           w_out=I["w_out1"], cst_d=I["cst_d"], router_w=I["router_w"], router_b=I["router_b"], w_gate=I["wg1"], w_up=I["wu1"],
                           w_down=I["wd1"], attn=Sx["attn_s"][256:, :], x1_d=Sx["x1b"],
                           xout_fn=lambda t: out[t * 128:(t + 1) * 128, :]), ntown, NE, nctx=0, TB=(512 if ntown % 8 == 0 else 256))
    return kb.done()


def w_in_cols(j):
    g = j // 2
    ar = np.arange
    tm = np.concatenate([1024 + j * 256 + ar(256), 2048 + j * 256 + ar(256), 3088 + j * 256 + ar(256), 512 + j * 128 + ar(128),
                         [3072 + j, 3076 + j, 3080 + j, 3084 + j], [5648 + d * 16 + 4 * j + h for d in range(2) for h in range(4)]])
    fm = np.concatenate([j * 128 + ar(128), 512 + j * 128 + ar(128), 4112 + j * 256 + ar(256), 4112 + 1024 + g * 128 + ar(128), 4112 + 1280 + g * 128 + ar(128)])
    return np.concatenate([tm, fm]).astype(np.int64)


def scan_params(inp, j):
    g = j // 2
    chs = np.concatenate([np.arange(j * 256, (j + 1) * 256), 1024 + np.arange(g * 128, (g + 1) * 128), 1280 + np.arange(g * 128, (g + 1) * 128)])
    cwt = inp["ssm_conv_w"][0][:, chs]
    return {
        "gb": rep(np.concatenate([inp["ml_ig_b"][0][:, j], inp["ml_fg_b"][0][:, j]])),
        "convw": np.ascontiguousarray(cwt.T.reshape(4, 128, 4).transpose(1, 0, 2).reshape(128, 16)),
        "convb": np.ascontiguousarray(inp["ssm_conv_b"][0][chs].reshape(4, 128).T),
        "dtb": rep(inp["ssm_dt_b"][0][:, 4 * j:4 * j + 4].reshape(-1)),
        "alog": rep(inp["ssm_a_log"][0][:, 4 * j:4 * j + 4].reshape(-1)),
        "dsk": rep(inp["ssm_d"][0][4 * j:4 * j + 4]),
        "cst": consts_B(),
    }


def fused_maps(inp, ntown=16):
    nt = ntown + 2
    own = ntown * 128
    T = 256 + 4 * own
    maps = []
    shared = {
        "ada_w0": np.ascontiguousarray(inp["ada_w"][0]), "ada_b0": pp_layout(inp["ada_b"][0]), "ada_b0_row": np.ascontiguousarray(inp["ada_b"][0].reshape(1, -1)),
        "ada_w1": np.ascontiguousarray(inp["ada_w"][1]), "ada_b1": pp_layout(inp["ada_b"][1]), "ada_b1_row": np.ascontiguousarray(inp["ada_b"][1].reshape(1, -1)),
        "cst_d": consts()["ident"],
        "lnp0": np.ascontiguousarray(np.concatenate([inp["ln_g"][0, 0], inp["ln_b"][0, 0], inp["ln_g"][0, 1], inp["ln_b"][0, 1]]).reshape(1, -1)),
        "lnp1": np.ascontiguousarray(np.concatenate([inp["ln_g"][1, 0], inp["ln_b"][1, 0], inp["ln_g"][1, 1], inp["ln_b"][1, 1]]).reshape(1, -1)),
        "w_out0": np.ascontiguousarray(inp["ab_w_out"][0]), "w_out1": np.ascontiguousarray(inp["at_w_out"][0]),
        "ng": np.ascontiguousarray(np.concatenate([inp["ml_norm_g"][0], inp["ssm_norm_g"][0]]).reshape(1, -1)),
        "router_w": np.ascontiguousarray(inp["router_w"]), "router_b": np.ascontiguousarray(inp["router_b"].reshape(1, -1)),
        "wg0": moe_layout("w_gate", inp["moe_w_gate"][0]), "wu0": moe_layout("w_up", inp["moe_w_up"][0]), "wd0": moe_layout("w_down", inp["moe_w_down"][0]),
        "wg1": moe_layout("w_gate", inp["moe_w_gate"][1]), "wu1": moe_layout("w_up", inp["moe_w_up"][1]), "wd1": moe_layout("w_down", inp["moe_w_down"][1]),
        "at_w_in": np.ascontiguousarray(inp["at_w_in"][0]),
        "gqk": np.ascontiguousarray(np.concatenate([np.tile(inp["at_q_g"][0], 8), np.tile(inp["at_k_g"][0], 2)]).reshape(1, -1)),
    }
    for core in range(NCORES):
        b, r = core // 4, core % 4
        xb = np.concatenate([inp["ctx"][b], inp["x"][b, :4 * own]], axis=0)
        xown = np.concatenate([inp["ctx"][b], inp["x"][b, r * own:(r + 1) * own]], axis=0)
        sel = np.zeros((128, 4), np.float32)
        sel[:, r] = 1.0
        m = dict(shared)
        m.update(scan_params(inp, r))
        m.update({"xb": np.ascontiguousarray(xb), "xown": np.ascontiguousarray(xown), "cpair": cpair_pp(inp, b),
                  "w_in_j": np.ascontiguousarray(inp["ab_w_in"][0][:, w_in_cols(r)]), "cossin": cossin_for(core, nt),
                  "sel": sel})
        maps.append(m)
    return maps


def kernel(x, c, ctx, c_ctx, ada_w, ada_b, ln_g, ln_b, ab_w_in, ab_w_out, ml_ig_b, ml_fg_b, ml_norm_g,
           ssm_conv_w, ssm_conv_b, ssm_dt_b, ssm_a_log, ssm_d, ssm_norm_g, at_w_in, at_w_out, at_q_g, at_k_g,
           router_w, router_b, moe_w_gate, moe_w_up, moe_w_down):
    inp = {k: np.asarray(v, dtype=np.float32) for k, v in dict(
        x=x, c=c, ctx=ctx, c_ctx=c_ctx, ada_w=ada_w, ada_b=ada_b, ln_g=ln_g, ln_b=ln_b, ab_w_in=ab_w_in, ab_w_out=ab_w_out,
        ml_ig_b=ml_ig_b, ml_fg_b=ml_fg_b, ml_norm_g=ml_norm_g, ssm_conv_w=ssm_conv_w, ssm_conv_b=ssm_conv_b, ssm_dt_b=ssm_dt_b,
        ssm_a_log=ssm_a_log, ssm_d=ssm_d, ssm_norm_g=ssm_norm_g, at_w_in=at_w_in, at_w_out=at_w_out, at_q_g=at_q_g, at_k_g=at_k_g,
        router_w=router_w, router_b=router_b, moe_w_gate=moe_w_gate, moe_w_up=moe_w_up, moe_w_down=moe_w_down).items()}
    nc = build_fused(16)
    res = run_bass_kernel_spmd(nc, fused_maps(inp, 16), core_ids=list(range(NCORES)))
    out = np.zeros((2, SEQ, D), np.float32)
    for core in range(NCORES):
        b, r = core // 4, core % 4
        out[b, r * TOWN:(r + 1) * TOWN] = res.results[core]["out"]
    return out
```

```python
import numpy as np
from contextlib import ExitStack
import concourse.bass as bass
import concourse.mybir as mybir
from concourse.bass_utils import run_bass_kernel_spmd

F32 = mybir.dt.float32
F32R = mybir.dt.float32r
AF = mybir.ActivationFunctionType
ALU = mybir.AluOpType
AX = mybir.AxisListType

D = 1024
NCORES = 8
CTX = 256
SEQ = 8192
TOWN = 2048
NT = 18
TOK = NT * 128
AB_IN = 5680
ALPHA = 4 ** 0.25
LN_EPS = 1e-5
RMS_EPS = 1e-6
NE = 16


class Sched:
    SEM_CAP = 30000
    NSLOT = 12

    def __init__(self, nc, es):
        self.nc = nc
        self.es = es
        self.eng = {"pe": nc.tensor, "act": nc.scalar, "dve": nc.vector, "pool": nc.gpsimd, "sp": nc.sync}
        self.count = {k: 0 for k in self.eng}
        self.sems = {k: [] for k in self.eng}
        self.waited = {k: {} for k in self.eng}
        self.last_w = {}
        self.readers = {}
        self.n_dma = 0
        self.psum_keys = set()

    def _sem(self, e, idx):
        if e.startswith("cc_ig"):
            return self.sems[e][0], 16, 0
        if e == "cc_all":
            return self.sems[e][0], idx + 1, 0
        if e.startswith("cc_"):
            return self.sems[e][0], 1, 0
        cap = self.SEM_CAP // 16 if e.startswith("dma") else self.SEM_CAP
        mul = 16 if e.startswith("dma") else 1
        k = idx // cap
        while len(self.sems[e]) <= k:
            self.sems[e].append(self.es.enter_context(self.nc.semaphore(f"s_{e}_{len(self.sems[e])}")))
        return self.sems[e][k], ((idx % cap) + 1) * mul, k

    def _wait(self, e, dep):
        p, pidx = dep
        sem, val, k = self._sem(p, pidx)
        key = (p, k)
        if self.waited[e].get(key, 0) >= val:
            return
        self.waited[e][key] = val
        self.eng[e].wait_ge(sem, val)

    def op(self, e, fn, reads=(), writes=(), dma=False):
        deps = set()
        for r in reads:
            if r in self.last_w:
                deps.add(self.last_w[r])
            if r in self.psum_keys:
                for rd in self.readers.get(r, ()):
                    if rd[0] != e:
                        deps.add(rd)
        for w in writes:
            if w in self.last_w:
                deps.add(self.last_w[w])
            for rd in self.readers.get(w, ()):
                deps.add(rd)
        for d in sorted(deps):
            if d[0] == "pe" and e == "pe":
                continue
            self._wait(e, d)
        if dma:
            slot = self.n_dma % self.NSLOT
            self.n_dma += 1
            name = f"dma{slot}"
            if name not in self.count:
                self.count[name] = 0
                self.sems[name] = []
            idx = self.count[name]
            if idx > 0:
                self._wait(e, (name, idx - 1))
            self.count[name] += 1
            sem, val, _ = self._sem(name, idx)
            inst = fn(self.eng[e])
            inst.then_inc(sem, 16)
            me = (name, idx)
        else:
            idx = self.count[e]
            self.count[e] += 1
            sem, val, _ = self._sem(e, idx)
            inst = fn(self.eng[e])
            inst.then_inc(sem, 1)
            me = (e, idx)
        for r in reads:
            self.readers.setdefault(r, []).append(me)
        for w in writes:
            self.last_w[w] = me
            self.readers[w] = []
        return inst

    def barrier(self):
        for e in self.eng:
            for name in list(self.count):
                if name == "cc_all":
                    continue
                if self.count[name] > 0:
                    self._wait(e, (name, self.count[name] - 1))
        self.last_w = {k: v for k, v in self.last_w.items() if v[0] == "cc_all"}
        self.readers = {}

    def finish(self, e="sp"):
        for name in list(self.count):
            if self.count[name] > 0 and name != e:
                self._wait(e, (name, self.count[name] - 1))


class KB:
    def __init__(self):
        self.nc = bass.Bass("TRN2", target_bir_lowering=False)
        self.es = ExitStack()
        self.S = Sched(self.nc, self.es)
        self.rr = 0
        self.stack = [self.es]
        self.nscope = 0
        self.pfx = ""

    def din(self, name, shape):
        return self.nc.dram_tensor(name, list(shape), F32, kind="ExternalInput").ap()

    def dout(self, name, shape):
        return self.nc.dram_tensor(name, list(shape), F32, kind="ExternalOutput").ap()

    def dscr(self, name, shape):
        return self.nc.dram_tensor(name, list(shape), F32, kind="Internal").ap()

    def sb(self, name, shape, dt=F32, es=None):
        return (es or self.stack[-1]).enter_context(self.nc.sbuf_tensor("sb_" + self.pfx + name, list(shape), dt))

    def ps(self, name, shape, dt=F32, es=None):
        self.S.psum_keys.add(name)
        return (es or self.stack[-1]).enter_context(self.nc.psum_tensor("ps_" + self.pfx + name, list(shape), dt))

    def load(self, out, in_, r, w, eng="sp"):
        return self.S.op(eng, lambda e: e.dma_start(out=out, in_=in_), reads=r, writes=w, dma=True)

    def store(self, out, in_, r, w, eng="sp"):
        return self.S.op(eng, lambda e: e.dma_start(out=out, in_=in_), reads=r, writes=w, dma=True)

    def mm(self, out, lhsT, rhs, r, w, start=True, stop=True):
        return self.S.op("pe", lambda e: e.matmul(out, lhsT=lhsT, rhs=rhs, start=start, stop=stop), reads=r, writes=w)

    def tr(self, out, in_, ident, r, w):
        return self.S.op("pe", lambda e: e.transpose(out, in_, ident), reads=list(r) + ["ident"], writes=w)

    def act(self, out, in_, func, r, w, bias=None, scale=None):
        kw = {}
        if bias is not None:
            kw["bias"] = bias
        if scale is not None:
            kw["scale"] = scale
        return self.S.op("act", lambda e: e.activation(out=out, in_=in_, func=func, **kw), reads=r, writes=w)

    def tt(self, out, in0, in1, op, r, w, eng="dve"):
        return self.S.op(eng, lambda e: e.tensor_tensor(out=out, in0=in0, in1=in1, op=op), reads=r, writes=w)

    def ts(self, out, in0, s1, op0, r, w, s2=None, op1=None, eng="dve"):
        if op1 is None:
            return self.S.op(eng, lambda e: e.tensor_scalar(out=out, in0=in0, scalar1=s1, scalar2=None, op0=op0), reads=r, writes=w)
        return self.S.op(eng, lambda e: e.tensor_scalar(out=out, in0=in0, scalar1=s1, scalar2=s2, op0=op0, op1=op1), reads=r, writes=w)

    def stt(self, out, in0, scalar, in1, op0, op1, r, w):
        return self.S.op("dve", lambda e: e.scalar_tensor_tensor(out=out, in0=in0, scalar=scalar, in1=in1, op0=op0, op1=op1), reads=r, writes=w)

    def cp(self, out, in_, r, w, eng="dve"):
        if eng == "act":
            return self.act(out, in_, AF.Identity, r, w)
        return self.S.op(eng, lambda e: e.tensor_copy(out=out, in_=in_), reads=r, writes=w)

    def cp_rr(self, out, in_, r, w, engs=("dve", "act")):
        self.rr += 1
        return self.cp(out, in_, r, w, eng=engs[self.rr % len(engs)])

    def memset(self, ap, val, w, eng="dve"):
        return self.S.op(eng, lambda e: e.memset(ap, val), reads=(), writes=w)

    def recip(self, out, in_, r, w):
        return self.S.op("dve", lambda e: e.reciprocal(out=out, in_=in_), reads=r, writes=w)

    def scope(self):
        kb = self

        class _Scope:
            def __enter__(self_):
                kb.nscope += 1
                self_.old = kb.pfx
                kb.pfx = f"z{kb.nscope}_"
                self_.es = ExitStack()
                kb.stack.append(self_.es)
                return self_

            def __exit__(self_, *a):
                kb.S.barrier()
                kb.stack.pop()
                self_.es.close()
                kb.pfx = self_.old
                return False
        return _Scope()

    def collective(self, kind, src, dst, groups, rkeys, wkey):
        S = self.S
        name = "cc_all"
        if name not in S.count:
            S.count[name] = 0
            S.sems[name] = [self.es.enter_context(self.nc.semaphore(name))]
        deps = set()
        for r in rkeys:
            if r in S.last_w:
                deps.add(S.last_w[r])
        for rd in S.readers.get(wkey, ()):
            deps.add(rd)
        if wkey in S.last_w:
            deps.add(S.last_w[wkey])
        for d in sorted(deps):
            S._wait("pool", d)
        inst = self.nc.gpsimd.collective_compute(kind, ALU.bypass, replica_groups=groups, ins=[src.opt()], outs=[dst.opt()])
        inst.then_inc(S.sems[name][0], 1)
        idx = S.count[name]
        S.count[name] += 1
        S.last_w[wkey] = (name, idx)
        S.readers[wkey] = []

    def gather_rows(self, out, src, idx_ap, r, w):
        S = self.S
        self.ngather = getattr(self, "ngather", 0) + 1
        name = f"cc_ig{self.ngather}"
        sem = self.es.enter_context(self.nc.semaphore(name))
        deps = set()
        for k in r:
            if k in S.last_w:
                deps.add(S.last_w[k])
        for k in w:
            if k in S.last_w:
                deps.add(S.last_w[k])
            for rd in S.readers.get(k, ()):
                deps.add(rd)
        for d in sorted(deps):
            S._wait("pool", d)
        inst = self.nc.gpsimd.indirect_dma_start(out=out, out_offset=None, in_=src, in_offset=bass.IndirectOffsetOnAxis(ap=idx_ap, axis=0))
        inst.then_inc(sem, 16)
        S.count[name] = 1
        S.sems[name] = [sem]
        for k in r:
            S.readers.setdefault(k, []).append((name, 0))
        for k in w:
            S.last_w[k] = (name, 0)
            S.readers[k] = []

    def done(self):
        self.S.finish("sp")
        self.es.close()
        return self.nc


def emit_mod_vectors(kb, ada_w, ada_b, cpair, vec_ids, pfx, es):
    nv = len(vec_ids)
    mod = kb.sb(pfx + "mod", [128, nv, 8, 2])
    with ExitStack() as les:
        csb = kb.sb(pfx + "c", [128, 8, 2], es=les)
        sig = kb.sb(pfx + "sig", [128, 8, 2], es=les)
        bpp = kb.sb(pfx + "bpp", [128, nv, 8], es=les)
        wsb = kb.sb(pfx + "w", [128, 8, 1024], es=les)
        pm = kb.ps(pfx + "pm", [128, 8, 2], es=les)
        kb.load(csb[:], cpair.rearrange("p (k w) -> p k w", w=2), ["cpair"], [pfx + "c"])
        kb.act(sig[:], csb[:], AF.Sigmoid, [pfx + "c"], [pfx + "sig"])
        kb.tt(csb[:], csb[:], sig[:], ALU.mult, [pfx + "c", pfx + "sig"], [pfx + "c"])
        for vi, v in enumerate(vec_ids):
            kb.load(bpp[:, vi, :], ada_b[:, v * 8:(v + 1) * 8], ["ada_b"], [(pfx + "bpp", vi)])
        for vi, v in enumerate(vec_ids):
            kb.load(wsb[:], ada_w[:, v * 1024:(v + 1) * 1024].rearrange("(k p) n -> p k n", p=128), ["ada_w"], [pfx + "w"])
            for cc in range(8):
                for k in range(8):
                    kb.mm(pm[:, cc, :], wsb[:, k, cc * 128:(cc + 1) * 128], csb[:, k, :], [pfx + "w", pfx + "c"], [pfx + "pm"],
                          start=(k == 0), stop=(k == 7))
            for who in range(2):
                kb.tt(mod[:, vi, :, who], pm[:, :, who], bpp[:, vi, :], ALU.add, [pfx + "pm", (pfx + "bpp", vi)], [(pfx + "mod", vi)])
        kb.S.barrier()
    return mod


def emit_bcast_vectors(kb, ada_w, ada_b_row, cpair, vec_ids, pfx, out_tiles, es):
    with ExitStack() as les:
        csb = kb.sb(pfx + "c", [128, 8, 2], es=les)
        sig = kb.sb(pfx + "sig", [128, 8, 2], es=les)
        cb = kb.sb(pfx + "cb", [128, 2, 8, 128], es=les)
        wsb = kb.sb(pfx + "w", [128, 8, 1024], es=les)
        bb = kb.sb(pfx + "bb", [128, 1024], es=les)
        pm = kb.ps(pfx + "pm", [128, 2, 512], es=les)
        kb.load(csb[:], cpair.rearrange("p (k w) -> p k w", w=2), ["cpair"], [pfx + "c"])
        kb.act(sig[:], csb[:], AF.Sigmoid, [pfx + "c"], [pfx + "sig"])
        kb.tt(csb[:], csb[:], sig[:], ALU.mult, [pfx + "c", pfx + "sig"], [pfx + "c"])
        for who in range(2):
            for k in range(8):
                kb.cp(cb[:, who, k, :], csb[:, k, who:who + 1].to_broadcast([128, 128]), [pfx + "c"], [pfx + "cb"])
        for vi, v in enumerate(vec_ids):
            kb.load(wsb[:], ada_w[:, v * 1024:(v + 1) * 1024].rearrange("(k p) n -> p k n", p=128), ["ada_w"], [pfx + "w"])
            kb.load(bb[:], ada_b_row[:, v * 1024:(v + 1) * 1024].partition_broadcast(128), ["ada_b"], [pfx + "bb"])
            for who in range(2):
                for nb in range(2):
                    for k in range(8):
                        kb.mm(pm[:, nb, :], cb[:, who, k, :], wsb[:, k, nb * 512:(nb + 1) * 512], [pfx + "w", pfx + "cb"], [pfx + "pm"],
                              start=(k == 0), stop=(k == 7))
                ot, okey = out_tiles[vi][who]
                kb.tt(ot, pm[:].rearrange("p a b -> p (a b)"), bb[:], ALU.add, [pfx + "pm", pfx + "bb"], [okey])
        kb.S.barrier()


def emit_transpose_mod(kb, src_tile, skey, hT, hkey, t, ident, ptr, mod, vi_shift, vi_scale1p, who, ptr_key):
    for half in range(2):
        for kk in range(4):
            k = half * 4 + kk
            kb.tr(ptr[:, kk, :], src_tile[:, k * 128:(k + 1) * 128], ident, [skey], [ptr_key])
        for kk in range(4):
            k = half * 4 + kk
            kb.act(hT[:, k, t * 128:(t + 1) * 128], ptr[:, kk, :], AF.Identity, [ptr_key, "mod"], [(hkey, t, k)],
                   bias=mod[:, vi_shift, k, who:who + 1], scale=mod[:, vi_scale1p, k, who:who + 1])


def build_A():
    kb = KB()
    xin = kb.din("xin", [TOK, D])
    cpair = kb.din("cpair", [128, 16])
    ada_w = kb.din("ada_w", [D, 6 * D])
    ada_b = kb.din("ada_b", [128, 48])
    w_in = kb.din("w_in", [D, AB_IN])
    ident_d = kb.din("ident_d", [128, 128])
    proj = kb.dout("proj", [TOK, AB_IN])

    ident = kb.sb("ident", [128, 128])
    kb.load(ident[:], ident_d, ["ident_d"], ["ident"])
    mod = emit_mod_vectors(kb, ada_w, ada_b, cpair, [0, 1], "m0", kb.es)
    kb.ts(mod[:, 1], mod[:, 1], 1.0, ALU.add, [("m0mod", 1)], ["mod"])
    kb.S.barrier()

    hT = kb.sb("hT", [128, 8, TOK], F32R)
    xt = [kb.sb(f"xt{i}", [128, D]) for i in range(2)]
    ptr = [kb.ps(f"ptr{i}", [128, 4, 128]) for i in range(2)]
    for t in range(NT):
        b = t % 2
        kb.load(xt[b][:], xin[t * 128:(t + 1) * 128, :], ["xin"], [f"xt{b}"])
        who = 1 if t < 2 else 0
        emit_transpose_mod(kb, xt[b], f"xt{b}", hT, "hT", t, ident[:], ptr[b], mod, 0, 1, who, f"ptr{b}")

    wf = [kb.sb(f"wf{i}", [128, 8, 512]) for i in range(2)]
    wr = [kb.sb(f"wr{i}", [128, 8, 512], F32R) for i in range(2)]
    po = [kb.ps(f"po{i}", [128, 512]) for i in range(2)]
    ot = [kb.sb(f"ot{i}", [128, 512]) for i in range(3)]
    nblk = (AB_IN + 511) // 512
    it = 0

    def ldw(cb):
        c0 = cb * 512
        cw = min(512, AB_IN - c0)
        kb.load(wf[cb % 2][:, :, :cw], w_in[:, c0:c0 + cw].rearrange("(k p) n -> p k n", p=128), ["w_in"], [f"wf{cb % 2}"])

    ldw(0)
    for cb in range(nblk):
        c0 = cb * 512
        cw = min(512, AB_IN - c0)
        b = cb % 2
        if cb + 1 < nblk:
            ldw(cb + 1)
        for k in range(8):
            kb.cp(wr[b][:, k, :cw], wf[b][:, k, :cw], [f"wf{b}"], [(f"wr{b}", k)], eng=("pool" if k % 2 else "dve"))
        for t in range(NT):
            pb = it % 2
            ob = it % 3
            it += 1
            for k in range(8):
                kb.mm(po[pb][:, :cw], hT[:, k, t * 128:(t + 1) * 128], wr[b][:, k, :cw], [("hT", t, k), (f"wr{b}", k)], [f"po{pb}"],
                      start=(k == 0), stop=(k == 7))
            kb.cp_rr(ot[ob][:, :cw], po[pb][:, :cw], [f"po{pb}"], [f"ot{ob}"])
            kb.store(proj[t * 128:(t + 1) * 128, c0:c0 + cw], ot[ob][:, :cw], [f"ot{ob}"], ["proj"])
    return kb.done()


def consts():
    ident = np.eye(128, dtype=np.float32)
    return {"ident": ident}


def pp_layout(v):
    return np.ascontiguousarray(v.reshape(-1, 128).T)


def cpair_pp(inp, b):
    a = np.stack([inp["c"][b], inp["c_ctx"]], axis=1)
    return np.ascontiguousarray(a.reshape(8, 128, 2).transpose(1, 0, 2).reshape(128, 16))


def core_tokens(x, ctx, core):
    b, r = core // 4, core % 4
    return np.concatenate([ctx[b], x[b, r * TOWN:(r + 1) * TOWN]], axis=0)


def run_A(inp):
    nc = build_A()
    maps = []
    for core in range(NCORES):
        b = core // 4
        maps.append({
            "xin": np.ascontiguousarray(core_tokens(inp["x"], inp["ctx"], core)),
            "cpair": cpair_pp(inp, b),
            "ada_w": np.ascontiguousarray(inp["ada_w"][0]),
            "ada_b": pp_layout(inp["ada_b"][0]),
            "w_in": np.ascontiguousarray(inp["ab_w_in"][0]),
            "ident_d": consts()["ident"],
        })
    res = run_bass_kernel_spmd(nc, maps, core_ids=list(range(NCORES)))
    return [r["proj"] for r in res.results]


def build_B(nlat=64, dbg=99):
    NCH = nlat + 2
    T = NCH * 128
    kb = KB()
    aps = dict(
        qT=kb.din("qT", [128, T]), kT=kb.din("kT", [128, T]), ktok=kb.din("ktok", [T, 128]), v=kb.din("v", [T, 256]),
        gates=kb.din("gates", [128, NCH * 4]), xbcT=kb.din("xbcT", [512, T]), dtr=kb.din("dtr", [128, NCH * 8]),
        gb=kb.din("gb", [128, 4]), convw=kb.din("convw", [128, 16]), convb=kb.din("convb", [128, 4]), dtb=kb.din("dtb", [128, 8]),
        alog=kb.din("alog", [128, 8]), dsk=kb.din("dsk", [128, 4]), cst=kb.din("cst", [128, 6, 128]))
    hm_o = kb.dout("hm", [128, NCH * 256])
    ys_o = kb.dout("ys", [128, NCH * 256])
    aps["hm3"] = hm_o.rearrange("p (c f) -> p c f", f=256)
    aps["ys3"] = ys_o.rearrange("p (c f) -> p c f", f=256)
    aps["xpost"] = kb.dscr("xpost", [512, T])
    with kb.scope():
        emit_B(kb, aps, nlat)
    return kb.done()


def emit_B(kb, aps, nlat):
    NCH = nlat + 2
    T = NCH * 128
    dbg = 99
    qT_d, kT_d, kt_d, v_d, g_d, xbc_d, dtr_d = aps["qT"], aps["kT"], aps["ktok"], aps["v"], aps["gates"], aps["xbcT"], aps["dtr"]
    gb_d, cw_d, cb_d, dtb_d, alog_d, dsk_d, cst_d = aps["gb"], aps["convw"], aps["convb"], aps["dtb"], aps["alog"], aps["dsk"], aps["cst"]
    xpost = aps["xpost"]
    cst = kb.sb("cst", [128, 6, 128])
    kb.load(cst[:], cst_d, ["cst_d"], ["cst"])
    ident = cst[:, 0, :]
    tri = [cst[:, 1, :], cst[:, 2, :]]
    strict = [cst[:, 3, :], cst[:, 4, :]]
    ones = cst[:, 5, :]
    par = kb.sb("par", [128, 48])
    for nm, ap_, o, n in (("gb", gb_d, 0, 4), ("cw", cw_d, 4, 16), ("cb", cb_d, 20, 4), ("dtb", dtb_d, 24, 8), ("alog", alog_d, 32, 8), ("dsk", dsk_d, 40, 4)):
        kb.load(par[:, o:o + n], ap_, [nm], ["par"])
    gb, cw, cbias, dtb, alog, dsk = par[:, 0:4], par[:, 4:20], par[:, 20:24], par[:, 24:32], par[:, 32:40], par[:, 40:44]

    G = kb.sb("G", [128, NCH, 4])
    kb.load(G[:], g_d.rearrange("p (c g) -> p c g", g=4), ["g_d"], ["G"])
    negb = kb.sb("negb", [128, 4])
    kb.ts(negb[:], gb, -1.0, ALU.mult, ["par"], ["negb"])
    LF = [kb.sb(f"LF{d}", [128, NCH]) for d in range(2)]
    IG = [kb.sb(f"IG{d}", [128, NCH]) for d in range(2)]
    WK = [kb.sb(f"WK{d}", [128, NCH]) for d in range(2)]
    QS = [kb.sb(f"QS{d}", [128, NCH]) for d in range(2)]
    EB = [kb.sb(f"EB{d}", [128, NCH]) for d in range(2)]
    pes = ExitStack()
    pg = kb.ps("pg", [128, 512], es=pes)
    for d in range(2):
        kb.ts(IG[d][:], G[:, :, d], gb[:, d:d + 1], ALU.add, ["G", "par"], [f"IG{d}"])
        kb.act(LF[d][:], G[:, :, 2 + d], AF.Exp, ["G", "negb"], [f"LF{d}"], bias=negb[:, 2 + d:3 + d], scale=-1.0)
        kb.act(LF[d][:], LF[d][:], AF.Ln, [f"LF{d}"], [f"LF{d}"], bias=1.0)
        kb.ts(LF[d][:], LF[d][:], -1.0, ALU.mult, [f"LF{d}"], [f"LF{d}"])
        kb.mm(pg[:, :NCH], tri[d], LF[d][:], ["cst", f"LF{d}"], ["pg"])
        kb.tt(WK[d][:], IG[d][:], pg[:, :NCH], ALU.subtract, [f"IG{d}", "pg"], [f"WK{d}"])
        kb.act(WK[d][:], WK[d][:], AF.Exp, [f"WK{d}"], [f"WK{d}"])
        kb.act(QS[d][:], pg[:, :NCH], AF.Exp, ["pg"], [f"QS{d}"])
        kb.ts(QS[d][:], QS[d][:], 128 ** -0.5, ALU.mult, [f"QS{d}"], [f"QS{d}"])
        kb.mm(pg[:, :NCH], ones, LF[d][:], ["cst", f"LF{d}"], ["pg"])
        kb.act(EB[d][:], pg[:, :NCH], AF.Exp, ["pg"], [f"EB{d}"])

    DT = kb.sb("DT", [128, NCH, 8])
    kb.load(DT[:], dtr_d.rearrange("p (c g) -> p c g", g=8), ["dtr_d"], ["DT"])
    for i in range(8):
        kb.ts(DT[:, :, i], DT[:, :, i], dtb[:, i:i + 1], ALU.add, ["DT", "par"], ["DT"])
    kb.act(DT[:], DT[:], AF.Exp, ["DT"], ["DT"])
    kb.act(DT[:], DT[:], AF.Ln, ["DT"], ["DT"], bias=1.0)
    aneg = kb.sb("aneg", [128, 8])
    kb.act(aneg[:], alog, AF.Exp, ["par"], ["aneg"])
    kb.ts(aneg[:], aneg[:], -1.0, ALU.mult, ["aneg"], ["aneg"])
    A = [kb.sb(f"A{d}", [128, NCH, 4]) for d in range(2)]
    EACS = [kb.sb(f"EACS{d}", [128, NCH, 4]) for d in range(2)]
    DTW = [kb.sb(f"DTW{d}", [128, NCH, 4]) for d in range(2)]
    EAE = [kb.sb(f"EAE{d}", [128, NCH, 4]) for d in range(2)]
    pg2 = kb.ps("pg2", [128, 512], es=pes)
    for d in range(2):
        for h in range(4):
            kb.ts(A[d][:, :, h], DT[:, :, d * 4 + h], aneg[:, d * 4 + h:d * 4 + h + 1], ALU.mult, ["DT", "aneg"], [f"A{d}"])
        Af = A[d][:].rearrange("p c h -> p (c h)")
        kb.mm(pg[:, :NCH * 4], tri[d], Af, ["cst", f"A{d}"], ["pg"])
        kb.mm(pg2[:, :NCH * 4], ones, Af, ["cst", f"A{d}"], ["pg2"])
        kb.act(EACS[d][:].rearrange("p c h -> p (c h)"), pg[:, :NCH * 4], AF.Exp, ["pg"], [f"EACS{d}"])
        kb.act(EAE[d][:].rearrange("p c h -> p (c h)"), pg2[:, :NCH * 4], AF.Exp, ["pg2"], [f"EAE{d}"])
        kb.cp(DTW[d][:].rearrange("p c h -> p (c h)"), pg[:, :NCH * 4], ["pg"], [f"DTW{d}"])
        kb.tt(DTW[d][:].rearrange("p c h -> p (c h)"), pg2[:, :NCH * 4], DTW[d][:].rearrange("p c h -> p (c h)"), ALU.subtract, ["pg2", f"DTW{d}"], [f"DTW{d}"])
        DWf = DTW[d][:].rearrange("p c h -> p (c h)")
        kb.act(DWf, DWf, AF.Exp, [f"DTW{d}"], [f"DTW{d}"])
        for h in range(4):
            kb.tt(DTW[d][:, :, h], DTW[d][:, :, h], DT[:, :, d * 4 + h], ALU.mult, [f"DTW{d}", "DT"], [f"DTW{d}"])

    kb.S.barrier()
    pes.close()
    with ExitStack() as les:
        Lmax = max(256, nlat * 128)
        xp = kb.sb("xp", [128, Lmax + 3], es=les)
        acc = kb.sb("acc", [128, Lmax], es=les)
        for cc in range(4):
            for (t0, L) in ((0, 256), (256, nlat * 128)):
                kb.memset(xp[:, 0:2], 0.0, ["xp"])
                kb.memset(xp[:, L + 2:L + 3], 0.0, ["xp"])
                kb.load(xp[:, 2:2 + L], xbc_d[cc * 128:(cc + 1) * 128, t0:t0 + L], ["xbc_d"], ["xp"])
                kb.ts(acc[:, :L], xp[:, 0:L], cw[:, cc * 4:cc * 4 + 1], ALU.mult, ["xp", "par"], ["acc"], s2=cbias[:, cc:cc + 1], op1=ALU.add)
                for j in range(1, 4):
                    kb.stt(acc[:, :L], xp[:, j:j + L], cw[:, cc * 4 + j:cc * 4 + j + 1], acc[:, :L], ALU.mult, ALU.add, ["xp", "acc", "par"], ["acc"])
                kb.act(acc[:, :L], acc[:, :L], AF.Silu, ["acc"], ["acc"])
                kb.store(xpost[cc * 128:(cc + 1) * 128, t0:t0 + L], acc[:, :L], ["acc"], ["xpost"])
        kb.S.barrier()

    HM = kb.sb("HM", [128, NCH, 256])
    YS = kb.sb("YS", [128, NCH, 256])
    nb = 2

    class TS:
        pass

    TD = []
    for d in range(2):
        T_ = TS()
        sfx = f"_{d}"
        T_.sfx = sfx
        T_.qTc = [kb.sb(f"qTc{i}{sfx}", [128, 128]) for i in range(nb)]
        T_.kTc = [kb.sb(f"kTc{i}{sfx}", [128, 128]) for i in range(nb)]
        T_.ktc = [kb.sb(f"ktc{i}{sfx}", [128, 128]) for i in range(nb)]
        T_.vaug = [kb.sb(f"vaug{i}{sfx}", [128, 257]) for i in range(nb)]
        T_.xsT = [kb.sb(f"xsT{i}{sfx}", [128, 2, 128]) for i in range(nb)]
        T_.BTc = [kb.sb(f"BTc{i}{sfx}", [128, 128]) for i in range(nb)]
        T_.CTc = [kb.sb(f"CTc{i}{sfx}", [128, 128]) for i in range(nb)]
        for i in range(nb):
            kb.memset(T_.vaug[i][:, 256:257], 1.0, [(f"vaug{i}{sfx}", "one")])
        for nm, shp in (("xs_tok", [128, 256]), ("Btok", [128, 128]), ("PT", [128, 128]), ("vw", [128, 257]), ("sm", [128, 4]), ("cbm", [128, 128]),
                        ("xsdt", [128, 256]), ("xw", [128, 256]), ("tmp", [128, 256]), ("tmp2", [128, 256])):
            setattr(T_, nm, kb.sb(nm + sfx, shp))
        T_.CTst = [kb.sb(f"CTst{i}{sfx}", [128, 257]) for i in range(2)]
        T_.Hst = [kb.sb(f"Hst{i}{sfx}", [128, 256]) for i in range(2)]
        T_.Lh = [kb.sb(f"Lh{i}{sfx}", [128, 128]) for i in range(2)]
        T_.dec = [kb.sb(f"dec{i}{sfx}", [128, 128]) for i in range(2)]
        T_.MT = [kb.sb(f"MT{i}{sfx}", [128, 128]) for i in range(2)]
        T_.cur = 0
        T_.it = 0
        kb.memset(T_.CTst[0][:], 0.0, [f"CTst0{sfx}"])
        kb.memset(T_.Hst[0][:], 0.0, [f"Hst0{sfx}"])
        TD.append(T_)
    ptr = kb.ps("ptr", [128, 3, 128])
    pA = kb.ps("pA", [128, 2, 128])
    pN = kb.ps("pN", [128, 257])
    pC = kb.ps("pC", [128, 257])
    pSeg = kb.ps("pSeg", [128, 128])
    pY = kb.ps("pY", [128, 2, 256])
    pH = kb.ps("pH", [128, 256])
    written = set()

    def proc(d, c):
        T_ = TD[d]
        x_ = T_.sfx
        i = T_.it % nb
        T_.it += 1
        cur = T_.cur
        nxt = 1 - cur
        first = c not in written
        written.add(c)
        qTc, kTc, ktc, vaug, xsT, BTc, CTc = T_.qTc, T_.kTc, T_.ktc, T_.vaug, T_.xsT, T_.BTc, T_.CTc
        xs_tok, Btok, PT, vw, sm, cbm, xsdt, xw, tmp, tmp2 = T_.xs_tok, T_.Btok, T_.PT, T_.vw, T_.sm, T_.cbm, T_.xsdt, T_.xw, T_.tmp, T_.tmp2
        CTst, Hst, Lh, dec, MT = T_.CTst, T_.Hst, T_.Lh, T_.dec, T_.MT
        sl = slice(c * 128, (c + 1) * 128)
        kb.load(qTc[i][:], qT_d[:, sl], ["qT_d"], [f"qTc{i}{x_}"])
        kb.load(kTc[i][:], kT_d[:, sl], ["kT_d"], [f"kTc{i}{x_}"])
        kb.load(ktc[i][:], kt_d[sl, :], ["kt_d"], [f"ktc{i}{x_}"])
        kb.load(vaug[i][:, :256], v_d[sl, :], ["v_d"], [f"vaug{i}{x_}"])
        kb.load(xsT[i][:], xpost[0:256, sl].rearrange("(a p) t -> p a t", p=128), ["xpost"], [f"xsT{i}{x_}"])
        kb.load(BTc[i][:], xpost[256:384, sl], ["xpost"], [f"BTc{i}{x_}"])
        kb.load(CTc[i][:], xpost[384:512, sl], ["xpost"], [f"CTc{i}{x_}"])
        kb.tr(ptr[:, 0, :], xsT[i][:, 0, :], ident, [f"xsT{i}{x_}"], ["ptr"])
        kb.tr(ptr[:, 1, :], xsT[i][:, 1, :], ident, [f"xsT{i}{x_}"], ["ptr"])
        kb.tr(ptr[:, 2, :], BTc[i][:], ident, [f"BTc{i}{x_}"], ["ptr"])
        kb.cp(xs_tok[:], ptr[:, 0:2, :].rearrange("p a t -> p (a t)"), ["ptr"], ["xs_tok" + x_], eng="act")
        kb.cp(Btok[:], ptr[:, 2, :], ["ptr"], ["Btok" + x_], eng="act")
        wkc = WK[d][:, c:c + 1]
        kb.mm(pA[:, 0, :], kTc[i][:], qTc[i][:], [f"kTc{i}{x_}", f"qTc{i}{x_}"], ["pA"])
        kb.mm(pA[:, 1, :], BTc[i][:], CTc[i][:], [f"BTc{i}{x_}", f"CTc{i}{x_}"], ["pA"])
        kb.stt(PT[:], pA[:, 0, :], wkc, tri[d], ALU.mult, ALU.mult, ["pA", f"WK{d}", "cst"], ["PT" + x_])
        kb.tt(cbm[:], pA[:, 1, :], tri[d], ALU.mult, ["pA", "cst"], ["cbm" + x_])
        kb.act(vw[:], vaug[i][:], AF.Identity, [f"vaug{i}{x_}", (f"vaug{i}{x_}", "one"), f"WK{d}"], ["vw" + x_], scale=wkc)
        kb.mm(pN[:], PT[:], vaug[i][:], ["PT" + x_, f"vaug{i}{x_}", (f"vaug{i}{x_}", "one")], ["pN"], start=True, stop=False)
        kb.mm(pN[:], qTc[i][:], CTst[cur][:], [f"qTc{i}{x_}", f"CTst{cur}{x_}"], ["pN"], start=False, stop=True)
        kb.mm(pC[:], ktc[i][:], vw[:], [f"ktc{i}{x_}", "vw" + x_], ["pC"])
        ebc = EB[d][:, c:c + 1]
        kb.act(CTst[nxt][:], CTst[cur][:], AF.Identity, [f"CTst{cur}{x_}", f"EB{d}"], [f"CTst{nxt}{x_}"], scale=ebc)
        kb.stt(CTst[nxt][:], pC[:], ebc, CTst[nxt][:], ALU.mult, ALU.add, ["pC", f"EB{d}", f"CTst{nxt}{x_}"], [f"CTst{nxt}{x_}"])
        qsc = QS[d][:, c:c + 1]
        smk = "sm" + x_
        kb.ts(sm[:, 0:1], pN[:, 256:257], qsc, ALU.mult, ["pN", f"QS{d}"], [smk])
        kb.act(sm[:, 1:2], sm[:, 0:1], AF.Abs, [smk], [smk])
        kb.ts(sm[:, 1:2], sm[:, 1:2], 1.0, ALU.max, [smk], [smk])
        kb.recip(sm[:, 2:3], sm[:, 1:2], [smk], [smk])
        kb.tt(sm[:, 3:4], sm[:, 2:3], qsc, ALU.mult, [smk, f"QS{d}"], [smk])
        if first:
            kb.ts(HM[:, c, :], pN[:, 0:256], sm[:, 3:4], ALU.mult, ["pN", smk], [("HM", c)])
        else:
            kb.stt(HM[:, c, :], pN[:, 0:256], sm[:, 3:4], HM[:, c, :], ALU.mult, ALU.add, ["pN", smk, ("HM", c)], [("HM", c)])
        for h in range(4):
            hs = slice(h * 64, (h + 1) * 64)
            kb.act(xsdt[:, hs], xs_tok[:, hs], AF.Identity, ["xs_tok" + x_, "DT"], ["xsdt" + x_], scale=DT[:, c, d * 4 + h:d * 4 + h + 1])
            kb.act(xw[:, hs], xs_tok[:, hs], AF.Identity, ["xs_tok" + x_, f"DTW{d}"], ["xw" + x_], scale=DTW[d][:, c, h:h + 1])
        for h in range(4):
            hs = slice(h * 64, (h + 1) * 64)
            j = h % 2
            kb.act(Lh[j][:], strict[d], AF.Identity, ["cst", f"A{d}"], [f"Lh{j}{x_}"], scale=A[d][:, c, h:h + 1])
            kb.mm(pSeg[:], Lh[j][:], tri[d], [f"Lh{j}{x_}", "cst"], ["pSeg"])
            kb.act(dec[j][:], pSeg[:], AF.Exp, ["pSeg"], [f"dec{j}{x_}"])
            kb.tt(MT[j][:], dec[j][:], cbm[:], ALU.mult, [f"dec{j}{x_}", "cbm" + x_], [f"MT{j}{x_}"])
            kb.mm(pY[:, 0, hs], MT[j][:], xsdt[:, hs], [f"MT{j}{x_}", "xsdt" + x_], ["pY"])
        kb.mm(pY[:, 1, :], CTc[i][:], Hst[cur][:], [f"CTc{i}{x_}", f"Hst{cur}{x_}"], ["pY"])
        for h in range(4):
            hs = slice(h * 64, (h + 1) * 64)
            kb.ts(tmp[:, hs], pY[:, 1, hs], EACS[d][:, c, h:h + 1], ALU.mult, ["pY", f"EACS{d}"], ["tmp" + x_])
        if first:
            kb.tt(YS[:, c, :], pY[:, 0, :], tmp[:], ALU.add, ["pY", "tmp" + x_], [("YS", c)])
        else:
            kb.tt(tmp2[:], pY[:, 0, :], tmp[:], ALU.add, ["pY", "tmp" + x_], ["tmp2" + x_])
            kb.tt(YS[:, c, :], YS[:, c, :], tmp2[:], ALU.add, ["tmp2" + x_, ("YS", c)], [("YS", c)])
        if d == 0:
            for h in range(4):
                hs = slice(h * 64, (h + 1) * 64)
                kb.stt(YS[:, c, hs], xs_tok[:, hs], dsk[:, h:h + 1], YS[:, c, hs], ALU.mult, ALU.add, ["xs_tok" + x_, "par", ("YS", c)], [("YS", c)])
        kb.mm(pH[:], Btok[:], xw[:], ["Btok" + x_, "xw" + x_], ["pH"])
        for h in range(4):
            hs = slice(h * 64, (h + 1) * 64)
            kb.act(Hst[nxt][:, hs], Hst[cur][:, hs], AF.Identity, [f"Hst{cur}{x_}", f"EAE{d}"], [f"Hst{nxt}{x_}"], scale=EAE[d][:, c, h:h + 1])
        kb.tt(Hst[nxt][:], Hst[nxt][:], pH[:], ALU.add, [f"Hst{nxt}{x_}", "pH"], [f"Hst{nxt}{x_}"])
        T_.cur = nxt

    orders = [list(range(NCH)), [1, 0] + list(range(NCH - 1, 1, -1))]
    for step in range(NCH):
        for d in range(2):
            proc(d, orders[d][step])
    if "E" in aps:
        E = aps["E"]
        for c in range(NCH):
            kb.store(E[c * 128:(c + 1) * 128, 0:256], HM[:, c, :], [("HM", c)], ["hm_o"])
            kb.store(E[c * 128:(c + 1) * 128, 256:512], YS[:, c, :], [("YS", c)], ["ys_o"])
        return
    hm3, ys3 = aps["hm3"], aps["ys3"]
    step = aps.get("ostep", 16)
    for c0 in range(0, NCH, step):
        c1 = min(NCH, c0 + step)
        kb.store(hm3[:, c0:c1, :], HM[:, c0:c1, :], [("HM", c) for c in range(c0, c1)], ["hm_o"])
        kb.store(ys3[:, c0:c1, :], YS[:, c0:c1, :], [("YS", c) for c in range(c0, c1)], ["ys_o"])


def consts_B():
    s = np.arange(128)[:, None]
    t = np.arange(128)[None, :]
    return np.ascontiguousarray(np.stack([np.eye(128), s <= t, s >= t, s > t, s < t, np.ones((128, 128))], axis=1).astype(np.float32))


def pmajor(a):
    C = a.shape[0] // 128
    return np.ascontiguousarray(a.reshape(C, 128, -1).transpose(1, 0, 2).reshape(128, -1))


def unpmajor(a, F):
    C = a.shape[1] // F
    return np.ascontiguousarray(a.reshape(128, C, F).transpose(1, 0, 2).reshape(C * 128, F))


def rep(v, n=128):
    return np.ascontiguousarray(np.broadcast_to(np.asarray(v, np.float32).reshape(1, -1), (n, np.asarray(v).size)))


def maps_B(inp, projb, b, j):
    g = j // 2
    q = projb[:, j * 128:(j + 1) * 128]
    k = projb[:, 512 + j * 128:512 + (j + 1) * 128]
    v = projb[:, 1024 + j * 256:1024 + (j + 1) * 256]
    ig = projb[:, 3072:3080].reshape(-1, 2, 4)[:, :, j]
    fg = projb[:, 3080:3088].reshape(-1, 2, 4)[:, :, j]
    xbc0 = 3088 + 1024
    xs = projb[:, xbc0 + j * 256:xbc0 + (j + 1) * 256]
    Bm = projb[:, xbc0 + 1024 + g * 128:xbc0 + 1024 + (g + 1) * 128]
    Cm = projb[:, xbc0 + 1280 + g * 128:xbc0 + 1280 + (g + 1) * 128]
    dt = projb[:, 5648:5680].reshape(-1, 2, 16)[:, :, 4 * j:4 * j + 4].reshape(-1, 8)
    chs = np.concatenate([np.arange(j * 256, (j + 1) * 256), 1024 + np.arange(g * 128, (g + 1) * 128), 1280 + np.arange(g * 128, (g + 1) * 128)])
    cwt = inp["ssm_conv_w"][0][:, chs]
    convw = cwt.T.reshape(4, 128, 4).transpose(1, 0, 2).reshape(128, 16)
    convb = inp["ssm_conv_b"][0][chs].reshape(4, 128).T
    return {
        "qT": np.ascontiguousarray(q.T), "kT": np.ascontiguousarray(k.T), "ktok": np.ascontiguousarray(k), "v": np.ascontiguousarray(v),
        "gates": pmajor(np.concatenate([ig, fg], axis=1)),
        "xbcT": np.ascontiguousarray(np.concatenate([xs, Bm, Cm], axis=1).T),
        "dtr": pmajor(dt),
        "gb": rep(np.concatenate([inp["ml_ig_b"][0][:, j], inp["ml_fg_b"][0][:, j]])),
        "convw": np.ascontiguousarray(convw), "convb": np.ascontiguousarray(convb),
        "dtb": rep(inp["ssm_dt_b"][0][:, 4 * j:4 * j + 4].reshape(-1)),
        "alog": rep(inp["ssm_a_log"][0][:, 4 * j:4 * j + 4].reshape(-1)),
        "dsk": rep(inp["ssm_d"][0][4 * j:4 * j + 4]),
        "cst": consts_B(),
    }


def emit_ln(kb, u, ukey, out, okey, g_bc, b_bc, st, mv, pfx):
    ukeys = list(ukey) if isinstance(ukey, list) else [ukey]
    for hh in range(2):
        kb.S.op("dve", lambda e, hh=hh: e.bn_stats(out=st[:, hh, :], in_=u[:, hh * 512:(hh + 1) * 512]), reads=ukeys, writes=[pfx + "st"])
    kb.S.op("dve", lambda e: e.bn_aggr(out=mv[:, 0:2], in_=st[:].rearrange("p a b -> p (a b)")), reads=[pfx + "st"], writes=[pfx + "mv"])
    kb.act(mv[:, 2:3], mv[:, 1:2], AF.Sqrt, [pfx + "mv"], [pfx + "mv"], bias=LN_EPS)
    kb.recip(mv[:, 3:4], mv[:, 2:3], [pfx + "mv"], [pfx + "mv"])
    kb.ts(out, u[:], mv[:, 0:1], ALU.subtract, ukeys + [pfx + "mv"], [okey], s2=mv[:, 3:4], op1=ALU.mult)
    kb.tt(out, out, g_bc, ALU.mult, [okey, "bc"], [okey])
    kb.tt(out, out, b_bc, ALU.add, [okey, "bc"], [okey], eng="pool")


def build_P(layer, nt=NT, ne=NE):
    K_ = 2048 if layer == 0 else 1024
    tok = nt * 128
    kb = KB()
    aps = dict(
        xres=kb.din("xres", [tok, D]), cpair=kb.din("cpair", [128, 16]), ada_w=kb.din("ada_w", [D, 6 * D]), ada_b=kb.din("ada_b", [128, 48]),
        ada_b_row=kb.din("ada_b_row", [1, 6 * D]), lnp=kb.din("lnp", [1, 4 * D]), w_out=kb.din("w_out", [K_, D]), cst_d=kb.din("cst_d", [128, 128]),
        router_w=kb.din("router_w", [D, 16]), router_b=kb.din("router_b", [1, 16]),
        w_gate=kb.din("w_gate", [ne, 4, 128, 2048]), w_up=kb.din("w_up", [ne, 4, 128, 2048]), w_down=kb.din("w_down", [ne, 4, 128, 2048]))
    if layer == 0:
        aps.update(hm=kb.din("hm", [tok, D]), ysd=kb.din("ysd", [tok, D]), o=kb.din("o", [tok, D]), z=kb.din("z", [tok, D]), ng=kb.din("ng", [1, 2 * D]))
    else:
        aps.update(attn=kb.din("attn", [tok, D]))
    xout = kb.dout("xout", [tok, D])
    aps["xout_fn"] = lambda t: xout[t * 128:(t + 1) * 128, :]
    aps["x1_d"] = kb.dscr("x1_d", [tok, D])
    with kb.scope():
        emit_P(kb, layer, aps, nt, ne)
    return kb.done()


def emit_P(kb, layer, aps, nt=NT, ne=NE, nctx=2, TB=384):
    K_ = 2048 if layer == 0 else 1024
    KC = K_ // 128
    tok = nt * 128
    TPP = nt // 2
    NTB = TPP * 128 // TB
    assert NTB * TB == TPP * 128
    xres, cpair, ada_w, ada_b, ada_b_row, lnp, w_out, cst_d = (aps[k] for k in ("xres", "cpair", "ada_w", "ada_b", "ada_b_row", "lnp", "w_out", "cst_d"))
    rw_d, rb_d, wg_d, wu_d, wd_d = (aps[k] for k in ("router_w", "router_b", "w_gate", "w_up", "w_down"))
    x1_d = aps["x1_d"]
    gathered = "EG" in aps
    if layer == 0:
        ng_d = aps["ng"]
        if not gathered:
            hm_d, ys_d, o_d, z_d = aps["hm"], aps["ysd"], aps["o"], aps["z"]
    else:
        at_d = aps["attn"]

    ident = kb.sb("ident", [128, 128])
    kb.load(ident[:], cst_d, ["cst_d"], ["ident"])
    mod = emit_mod_vectors(kb, ada_w, ada_b, cpair, [3, 4], "m", kb.es)
    kb.ts(mod[:, 1], mod[:, 1], 1.0, ALU.add, [("mmod", 1)], ["mod"])
    bc = kb.sb("bc", [128, 4, D])

    def fill_bc(stage):
        emit_bcast_vectors(kb, ada_w, ada_b_row, cpair, [2 if stage == 0 else 5], f"g{stage}",
                           [[(bc[:, 0, :], "bc"), (bc[:, 1, :], "bc")]], kb.es)
        for i in range(2):
            kb.load(bc[:, 2 + i, :], lnp[:, (2 * stage + i) * D:(2 * stage + i + 1) * D].partition_broadcast(128), ["lnp"], ["bc"])
        kb.S.barrier()

    fill_bc(0)

    with ExitStack() as les:
        wo = kb.sb("wo", [128, KC, D], F32R, es=les)
        wtmp = [kb.sb(f"wtmp{i}", [128, D], es=les) for i in range(2)]
        for kc in range(KC):
            kb.load(wtmp[kc % 2][:], w_out[kc * 128:(kc + 1) * 128, :], ["w_out"], [f"wtmp{kc % 2}"])
            kb.cp(wo[:, kc, :], wtmp[kc % 2][:], [f"wtmp{kc % 2}"], [("wo", kc)], eng=("pool" if kc % 2 else "dve"))
        if layer == 0:
            ngb = kb.sb("ngb", [128, 2 * D], es=les)
            kb.load(ngb[:], ng_d.partition_broadcast(128), ["ng"], ["ngb"])
            if gathered:
                tinG = [kb.sb(f"tinG{i}", [128, 4, D], es=les) for i in range(2)]
                cands = [kb.sb(f"cand{q_}", [128, 4, D], es=les) for q_ in range(2)]
                selt = kb.sb("selt", [128, 4], es=les)
                kb.load(selt[:], aps["sel"], ["sel_d"], ["selt"])
            else:
                tin = [[kb.sb(f"tin{i}_{j}", [128, D], es=les) for j in range(4)] for i in range(2)]
        mo = [kb.sb(f"mo{i}", [128, K_], es=les) for i in range(2)]
        xr = [kb.sb(f"xr{i}", [128, D], es=les) for i in range(2)]
        yT = kb.sb("yT", [128, KC, 128], F32R, es=les)
        tmpa = kb.sb("tmpa", [128, D], es=les)
        u = kb.sb("u", [128, D], es=les)
        x1t = [kb.sb("x1t0", [128, D], es=les)] * 2
        st = kb.sb("st", [128, 4, 6], es=les)
        mv = kb.sb("mv", [128, 4, 4], es=les)
        lst = kb.sb("lst", [128, 2, 6], es=les)
        lmv = kb.sb("lmv", [128, 4], es=les)
        ss = kb.sb("ss", [128, 4], es=les)
        ptr = [kb.ps(f"ptr{i}", [128, 4, 128], es=les) for i in range(2)]
        pO = [kb.ps(f"pO{i}", [128, 512], es=les) for i in range(2)]
        def load_in(t):
            i = t % 2
            rows = slice(t * 128, (t + 1) * 128)
            kb.load(xr[i][:], xres[rows, :], ["xres"], [f"xr{i}"])
            if layer == 0:
                if gathered:
                    tk = [f"tin{i}_hm", f"tin{i}_ysd", f"tin{i}_o", f"tin{i}_z"]
                    own_rows = (nt - 2) * 128

                    def cand_ap(token0):
                        piece, off = token0 // 256, token0 % 256
                        return aps["EG"][piece * 1024:(piece + 1) * 1024, :].rearrange("(j r) f -> r j f", j=4)[off:off + 128]

                    if t < 2:
                        kb.load(tinG[i][:], cand_ap(t * 128), ["EG"], tk)
                    else:
                        for rho in range(4):
                            cand, ck = cands[rho % 2], f"cand{rho % 2}"
                            kb.load(cand[:], cand_ap(256 + rho * own_rows + (t - 2) * 128), ["EG"], [ck])
                            if rho == 0:
                                kb.ts(tinG[i][:], cand[:], selt[:, 0:1], ALU.mult, [ck, "selt"], tk)
                            else:
                                kb.stt(tinG[i][:].rearrange("p j f -> p (j f)"), cand[:].rearrange("p j f -> p (j f)"), selt[:, rho:rho + 1],
                                       tinG[i][:].rearrange("p j f -> p (j f)"), ALU.mult, ALU.add, [ck, "selt"] + tk, tk)
                else:
                    for tl, src, nm in zip(tin[i], (hm_d, ys_d, o_d, z_d), ("hm", "ysd", "o", "z")):
                        kb.load(tl[:], src[rows, :], [nm], [f"tin{i}_{nm}"])
            else:
                kb.load(mo[i][:], at_d[rows, :], ["attn"], [f"mo{i}a"])

        load_in(0)
        for t in range(nt):
            i = t % 2
            who = 1 if t < nctx else 0
            rows = slice(t * 128, (t + 1) * 128)
            if t + 1 < nt:
                load_in(t + 1)
            if layer == 0:
                if gathered:
                    hm3, ys3, o3, z3 = (tinG[i][:, :, q * 256:(q + 1) * 256] for q in range(4))
                else:
                    hm_t, ys_t, o_t, z_t = tin[i]
                    hm3, ys3, o3, z3 = (tl[:].rearrange("p (h d) -> p h d", d=256) for tl in (hm_t, ys_t, o_t, z_t))
                moA = mo[i][:, :D].rearrange("p (h d) -> p h d", d=256)
                moB = mo[i][:, D:].rearrange("p (h d) -> p h d", d=256)
                for h in range(4):
                    kb.S.op("dve", lambda e, h=h: e.bn_stats(out=st[:, h, :], in_=hm3[:, h, :]), reads=[f"tin{i}_hm"], writes=["st"])
                    kb.S.op("dve", lambda e, h=h: e.bn_aggr(out=mv[:, h, 0:2], in_=st[:, h, :]), reads=["st"], writes=["mv"])
                kb.act(mv[:, :, 2], mv[:, :, 1], AF.Sqrt, ["mv"], ["mv"], bias=LN_EPS)
                kb.recip(mv[:, :, 3], mv[:, :, 2], ["mv"], ["mv"])
                for h in range(4):
                    kb.ts(moA[:, h, :], hm3[:, h, :], mv[:, h, 0:1], ALU.subtract, [f"tin{i}_hm", "mv"], [f"mo{i}a"], s2=mv[:, h, 3:4], op1=ALU.mult)
                kb.tt(mo[i][:, :D], mo[i][:, :D], ngb[:, :D], ALU.mult, [f"mo{i}a", "ngb"], [f"mo{i}a"], eng="pool")
                kb.act(o3, o3, AF.Sigmoid, [f"tin{i}_o"], [f"tin{i}_o"])
                kb.tt(moA, moA, o3, ALU.mult, [f"mo{i}a", f"tin{i}_o"], [f"mo{i}a"])
                kb.act(z3, z3, AF.Silu, [f"tin{i}_z"], [f"tin{i}_z"])
                kb.tt(ys3, ys3, z3, ALU.mult, [f"tin{i}_ysd", f"tin{i}_z"], [f"tin{i}_ysd"])
                kb.S.op("act", lambda e: e.activation(out=z3, in_=ys3, func=AF.Square, accum_out=ss[:, 0:1]),
                        reads=[f"tin{i}_ysd"], writes=[f"tin{i}_z", "ss"])
                kb.act(ss[:, 1:2], ss[:, 0:1], AF.Sqrt, ["ss"], ["ss"], bias=RMS_EPS, scale=1.0 / D)
                kb.recip(ss[:, 2:3], ss[:, 1:2], ["ss"], ["ss"])
                kb.stt(moB, ys3, ss[:, 2:3], ngb[:, D:].rearrange("p (h d) -> p h d", d=256), ALU.mult, ALU.mult, [f"tin{i}_ysd", "ss", "ngb"], [f"mo{i}b"])
                mokeys = [f"mo{i}a", f"mo{i}b"]
            else:
                mokeys = [f"mo{i}a"]
            for g4 in range(KC // 4):
                pb = g4 % 2
                for kk in range(4):
                    kc = g4 * 4 + kk
                    kb.tr(ptr[pb][:, kk, :], mo[i][:, kc * 128:(kc + 1) * 128], ident[:], mokeys, [f"ptr{pb}"])
                kb.cp_rr(yT[:, g4 * 4:(g4 + 1) * 4, :], ptr[pb][:], [f"ptr{pb}"], [("yT", g4)])
            for nb in range(2):
                for kc in range(KC):
                    kb.mm(pO[nb][:], yT[:, kc, :], wo[:, kc, nb * 512:(nb + 1) * 512], [("yT", kc // 4), ("wo", kc)], [f"pO{nb}"],
                          start=(kc == 0), stop=(kc == KC - 1))
            for nb in range(2):
                cs = slice(nb * 512, (nb + 1) * 512)
                kb.tt(tmpa[:, cs], pO[nb][:], bc[:, who, cs], ALU.mult, [f"pO{nb}", "bc"], [("tmpa", nb)])
                kb.stt(u[:, cs], xr[i][:, cs], ALPHA, tmpa[:, cs], ALU.mult, ALU.add, [f"xr{i}", ("tmpa", nb)], ["u"])
            emit_ln(kb, u, "u", x1t[i][:], "x1t", bc[:, 2, :], bc[:, 3, :], lst, lmv, "l1")
            kb.store(x1_d[rows, :], x1t[i][:], ["x1t"], ["x1_d"])
        kb.S.barrier()

    fill_bc(1)
    ptok = TPP * 128
    rwf = kb.sb("rwf", [128, 8, 16])
    rwr = kb.sb("rwr", [128, 8, 16], F32R)
    kb.load(rwf[:], rw_d.rearrange("(k p) e -> p k e", p=128), ["rw_d"], ["rwf"])
    kb.cp(rwr[:], rwf[:], ["rwf"], ["rwr"])
    rbb = kb.sb("rbb", [128, 16])
    kb.load(rbb[:], rb_d.partition_broadcast(128), ["rb_d"], ["rbb"])
    ones16 = kb.sb("ones16", [16, 128])
    kb.memset(ones16[:], 1.0, ["ones16"])
    GTm = kb.sb("GTm", [16, ptok])
    hT = kb.sb("hT2", [128, 8, ptok], F32R)
    accT = kb.sb("accT", [128, 8, ptok])
    GT = kb.sb("GT", [16, ptok])
    gbc = kb.sb("gbc", [128, ptok])
    x1r = [kb.sb(f"x1r{i}", [128, D]) for i in range(2)]
    rt = kb.sb("rt", [128, 8, 16])
    rs = kb.sb("rs", [128, 16])
    g16 = kb.sb("g16", [128, 16])
    NWB = 3
    wgr = [kb.sb(f"wgr{i}", [128, 8, 256], F32R) for i in range(NWB)]
    wur = [kb.sb(f"wur{i}", [128, 8, 256], F32R) for i in range(NWB)]
    wdr = [kb.sb(f"wdr{i}", [128, 2, D], F32R) for i in range(NWB)]
    sg = [kb.sb(f"sg{i}", [128, TB]) for i in range(2)]
    AT = [kb.sb(f"AT{i}", [128, 2, TB], F32R) for i in range(2)]
    tmpb = kb.sb("tmpb", [128, D])
    u2 = tmpb
    xo = [kb.sb("xo0", [128, D])] * 2
    lst2 = kb.sb("lst2", [128, 2, 6])
    lmv2 = kb.sb("lmv2", [128, 4])
    ptr2 = [kb.ps(f"ptrb{i}", [128, 4, 128]) for i in range(2)]
    pG = [kb.ps(f"pG{i}", [128, 512]) for i in range(2)]
    pU = [kb.ps(f"pU{i}", [128, 512]) for i in range(2)]
    pD = [kb.ps(f"pD{i}", [128, 512]) for i in range(2)]

    units = [(e, q) for e in range(ne) for q in range(4)]

    def load_unit(ui):
        e, q = units[ui]
        b = ui % NWB
        kb.load(wgr[b][:].rearrange("p k f -> p (k f)"), wg_d[e, q], ["wg_d"], [f"wgr{b}"], eng="pool")
        kb.load(wur[b][:].rearrange("p k f -> p (k f)"), wu_d[e, q], ["wu_d"], [f"wur{b}"], eng="pool")
        kb.load(wdr[b][:].rearrange("p a n -> p (a n)"), wd_d[e, q], ["wd_d"], [f"wdr{b}"], eng="pool")

    for p in range(2):
        t0 = p * TPP
        for lt in range(TPP):
            t = t0 + lt
            i = lt % 2
            who = 1 if t < nctx else 0
            kb.load(x1r[i][:], x1_d[t * 128:(t + 1) * 128, :], ["x1_d"], [f"x1r{i}"])
            emit_transpose_mod(kb, x1r[i], f"x1r{i}", hT, "hT2", lt, ident[:], ptr2[i], mod, 0, 1, who, f"ptrb{i}")
            pR = pG[lt % 2]
            for k in range(8):
                kb.mm(pR[:, :16], hT[:, k, lt * 128:(lt + 1) * 128], rwr[:, k, :], [("hT2", lt, k), "rwr"], [f"pG{lt % 2}"],
                      start=(k == 0), stop=(k == 7))
            probs, sel, pr, gs = rt[:, 0, :], rt[:, 1, :], rt[:, 2:4, :].rearrange("p a b -> p (a b)"), rt[:, 4, :]
            kb.S.op("act", lambda e, pR=pR: e.activation(out=probs, in_=pR[:, :16], func=AF.Exp, accum_out=rs[:, 0:1]),
                    reads=[f"pG{lt % 2}"], writes=["rt0", "rs"])
            kb.recip(rs[:, 1:2], rs[:, 0:1], ["rs"], ["rs"])
            kb.ts(probs, probs, rs[:, 1:2], ALU.mult, ["rt0", "rs"], ["rt0"])
            kb.tt(sel, probs, rbb[:], ALU.add, ["rt0", "rbb"], ["rt1"])
            sel3 = sel.rearrange("p (g e) -> p g e", e=4)
            pr3 = pr[:, :24].rearrange("p (g e) -> p g e", e=6)
            for pi, (a, b_) in enumerate(((0, 1), (0, 2), (0, 3), (1, 2), (1, 3), (2, 3))):
                kb.tt(pr3[:, :, pi:pi + 1], sel3[:, :, a:a + 1], sel3[:, :, b_:b_ + 1], ALU.add, ["rt1"], ["rt2"])
            kb.S.op("dve", lambda e: e.tensor_reduce(out=gs[:, 0:4], in_=pr3, axis=AX.X, op=ALU.max), reads=["rt2"], writes=["rt4"])
            kb.S.op("dve", lambda e: e.tensor_reduce(out=rs[:, 2:3], in_=gs[:, 0:4], axis=AX.X, op=ALU.max), reads=["rt4"], writes=["rs"])
            kb.ts(gs[:, 4:8], gs[:, 0:4], rs[:, 2:3], ALU.is_ge, ["rt4", "rs"], ["rt4"])
            kb.ts(gs[:, 8:12], gs[:, 4:8], -1.0, ALU.add, ["rt4"], ["rt4"], s2=4.0, op1=ALU.mult)
            selm = rt[:, 5, :]
            selm3 = selm.rearrange("p (g e) -> p g e", e=4)
            for g in range(4):
                kb.ts(selm3[:, g, :], sel3[:, g, :], gs[:, 4 + g:5 + g], ALU.mult, ["rt1", "rt4"], ["rt5"], s2=gs[:, 8 + g:9 + g], op1=ALU.add)
            top8 = rt[:, 6, 0:8]
            kb.S.op("dve", lambda e: e.max(out=top8, in_=selm), reads=["rt5"], writes=["rt6"])
            em = rt[:, 7, :]
            kb.ts(em, selm, rt[:, 6, 1:2], ALU.is_ge, ["rt5", "rt6"], ["rt7"])
            kb.tt(em, em, probs, ALU.mult, ["rt7", "rt0"], ["rt7"])
            kb.S.op("dve", lambda e: e.tensor_reduce(out=rs[:, 3:4], in_=em, axis=AX.X, op=ALU.add), reads=["rt7"], writes=["rs"])
            kb.recip(rs[:, 4:5], rs[:, 3:4], ["rs"], ["rs"])
            kb.ts(g16[:], em, rs[:, 4:5], ALU.mult, ["rt7", "rs"], ["g16"])
            kb.tr(ptr2[i][:16, 0, :], g16[:], ident[:], ["g16"], [f"ptrb{i}"])
            kb.cp(GT[:, lt * 128:(lt + 1) * 128], ptr2[i][:16, 0, :], [f"ptrb{i}"], ["GT"])
        load_unit(0)
        load_unit(1)
        for ui, (e, q) in enumerate(units):
            b = ui % NWB
            if ui + 2 < len(units):
                load_unit(ui + 2)
            if q == 0:
                kb.ts(GTm[:], GT[:], ident[:16, e:e + 1], ALU.mult, ["GT", "ident"], ["GTm"], eng="pool")
                for tb in range(NTB):
                    ts_ = slice(tb * TB, (tb + 1) * TB)
                    kb.mm(pD[tb % 2][:, :TB], ones16[:], GTm[:, ts_], ["ones16", "GTm"], [f"pD{tb % 2}"])
                    kb.cp(gbc[:, ts_], pD[tb % 2][:, :TB], [f"pD{tb % 2}"], ["gbc"], eng="act")
            def emit_GU(tb):
                ts_ = slice(tb * TB, (tb + 1) * TB)
                ab = tb % 2
                for fc in range(2):
                    fs = slice(fc * 128, (fc + 1) * 128)
                    pb = fc
                    for k in range(8):
                        kb.mm(pG[pb][:, :TB], wgr[b][:, k, fs], hT[:, k, ts_], [f"wgr{b}", "hT2all"], [f"pG{pb}"], start=(k == 0), stop=(k == 7))
                    for k in range(8):
                        kb.mm(pU[pb][:, :TB], wur[b][:, k, fs], hT[:, k, ts_], [f"wur{b}", "hT2all"], [f"pU{pb}"], start=(k == 0), stop=(k == 7))
                    kb.act(sg[pb][:], pG[pb][:, :TB], AF.Silu, [f"pG{pb}"], [f"sg{pb}"])
                    kb.tt(sg[pb][:], sg[pb][:], pU[pb][:, :TB], ALU.mult, [f"sg{pb}", f"pU{pb}"], [f"sg{pb}"])
                    kb.tt(AT[ab][:, fc, :], sg[pb][:], gbc[:, ts_], ALU.mult, [f"sg{pb}", "gbc"], [f"AT{ab}"], eng="pool")

            def emit_Dn(tb):
                ts_ = slice(tb * TB, (tb + 1) * TB)
                ab = tb % 2
                for dc in range(8):
                    pb = dc % 2
                    for fc in range(2):
                        kb.mm(pD[pb][:, :TB], wdr[b][:, fc, dc * 128:(dc + 1) * 128], AT[ab][:, fc, :], [f"wdr{b}", f"AT{ab}"], [f"pD{pb}"],
                              start=(fc == 0), stop=(fc == 1))
                    if ui == 0:
                        kb.cp(accT[:, dc, ts_], pD[pb][:, :TB], [f"pD{pb}"], [("accT", dc, tb)])
                    else:
                        kb.tt(accT[:, dc, ts_], accT[:, dc, ts_], pD[pb][:, :TB], ALU.add, [f"pD{pb}", ("accT", dc, tb)], [("accT", dc, tb)])

            for tb in range(NTB + 1):
                if tb < NTB:
                    emit_GU(tb)
                if tb >= 1:
                    emit_Dn(tb - 1)
        for lt in range(TPP):
            t = t0 + lt
            i = lt % 2
            who = 1 if t < nctx else 0
            tb = lt * 128 // TB
            kb.load(x1r[i][:], x1_d[t * 128:(t + 1) * 128, :], ["x1_d"], [f"x1r{i}"])
            for nb in range(2):
                for kk in range(4):
                    dc = nb * 4 + kk
                    kb.tr(pD[nb][:, kk * 128:(kk + 1) * 128], accT[:, dc, lt * 128:(lt + 1) * 128], ident[:], [("accT", dc, tb)], [f"pD{nb}"])
            for nb in range(2):
                cs = slice(nb * 512, (nb + 1) * 512)
                kb.tt(tmpb[:, cs], pD[nb][:], bc[:, who, cs], ALU.mult, [f"pD{nb}", "bc"], [("tmpb", nb)])
                kb.stt(u2[:, cs], x1r[i][:, cs], ALPHA, tmpb[:, cs], ALU.mult, ALU.add, [f"x1r{i}", ("tmpb", nb)], [("tmpb", nb)])
            emit_ln(kb, u2, [("tmpb", 0), ("tmpb", 1)], xo[i][:], "xo", bc[:, 2, :], bc[:, 3, :], lst2, lmv2, "l2")
            dst = aps["xout_fn"](t)
            if dst is not None:
                kb.store(dst, xo[i][:], ["xo"], ["xout"])


def moe_layout(kind, w):
    E = w.shape[0]
    if kind in ("w_gate", "w_up"):
        return np.ascontiguousarray(w.reshape(E, 8, 128, 4, 256).transpose(0, 3, 2, 1, 4).reshape(E, 4, 128, 2048))
    return np.ascontiguousarray(w.reshape(E, 4, 2, 128, 1024).transpose(0, 1, 3, 2, 4).reshape(E, 4, 128, 2048))


def esel_const():
    m = np.zeros((16, 16, 128), np.float32)
    for e in range(16):
        m[e, e, :] = 1.0
    return np.ascontiguousarray(m.transpose(1, 0, 2).reshape(16, 16 * 128))


def maps_P_common(inp, layer, b):
    return {
        "cpair": cpair_pp(inp, b),
        "ada_w": np.ascontiguousarray(inp["ada_w"][layer]),
        "ada_b": pp_layout(inp["ada_b"][layer]),
        "ada_b_row": np.ascontiguousarray(inp["ada_b"][layer].reshape(1, -1)),
        "lnp": np.ascontiguousarray(np.concatenate([inp["ln_g"][layer, 0], inp["ln_b"][layer, 0], inp["ln_g"][layer, 1], inp["ln_b"][layer, 1]]).reshape(1, -1)),
        "cst_d": consts()["ident"],
        "router_w": np.ascontiguousarray(inp["router_w"]),
        "router_b": np.ascontiguousarray(inp["router_b"].reshape(1, -1)),
        "w_gate": np.ascontiguousarray(inp["moe_w_gate"][layer]),
        "w_up": np.ascontiguousarray(inp["moe_w_up"][layer]),
        "w_down": np.ascontiguousarray(inp["moe_w_down"][layer]),
    }


def build_Q(nt=NT):
    tok = nt * 128
    kb = KB()
    aps = dict(xin=kb.din("xin", [tok, D]), cpair=kb.din("cpair", [128, 16]), ada_w=kb.din("ada_w", [D, 6 * D]), ada_b=kb.din("ada_b", [128, 48]),
               w_in=kb.din("w_in", [D, 1536]), cst_d=kb.din("cst_d", [128, 128]), gqk=kb.din("gqk", [1, 1280]), cossin=kb.din("cossin", [tok, 2, 640]),
               qkv=kb.dout("qkv", [tok, 1536]))
    with kb.scope():
        emit_Q(kb, aps, nt)
    return kb.done()


def emit_Q(kb, aps, nt=NT):
    tok = nt * 128
    xin, cpair, ada_w, ada_b, w_in, cst_d, gq_d, cs_d, qkv = (aps[k] for k in ("xin", "cpair", "ada_w", "ada_b", "w_in", "cst_d", "gqk", "cossin", "qkv"))

    ident = kb.sb("ident", [128, 128])
    kb.load(ident[:], cst_d, ["cst_d"], ["ident"])
    mod = emit_mod_vectors(kb, ada_w, ada_b, cpair, [0, 1], "m", kb.es)
    kb.ts(mod[:, 1], mod[:, 1], 1.0, ALU.add, [("mmod", 1)], ["mod"])
    gq = kb.sb("gq", [128, 1280])
    kb.load(gq[:], gq_d.partition_broadcast(128), ["gq_d"], ["gq"])
    kb.S.barrier()
    wr = kb.sb("wr", [128, 8, 1536], F32R)
    wtmp = [kb.sb(f"wtmp{i}", [128, 1536]) for i in range(2)]
    for k in range(8):
        kb.load(wtmp[k % 2][:], w_in[k * 128:(k + 1) * 128, :], ["w_in"], [f"wtmp{k % 2}"])
        kb.cp(wr[:, k, :], wtmp[k % 2][:], [f"wtmp{k % 2}"], [("wr", k)], eng=("pool" if k % 2 else "dve"))
    hT = kb.sb("hT", [128, 8, 256], F32R)
    xt = [kb.sb(f"xt{i}", [128, D]) for i in range(2)]
    cs = [kb.sb(f"cs{i}", [128, 2, 640]) for i in range(2)]
    qk = kb.sb("qk", [128, 1536])
    sq = kb.sb("sq", [128, 1280])
    ms = kb.sb("ms", [128, 3, 10])
    ro = [kb.sb(f"ro{i}", [128, 1536]) for i in range(2)]
    t1 = kb.sb("t1", [128, 10, 64])
    t2 = kb.sb("t2", [128, 10, 64])
    ptr = [kb.ps(f"ptr{i}", [128, 4, 128]) for i in range(2)]
    po = [kb.ps(f"po{i}", [128, 512]) for i in range(3)]
    kb.load(xt[0][:], xin[0:128, :], ["xin"], ["xt0"])
    kb.load(cs[0][:], cs_d[0:128], ["cs_d"], ["cs0"])
    for t in range(nt):
        i = t % 2
        who = 1 if t < 2 else 0
        rows = slice(t * 128, (t + 1) * 128)
        if t + 1 < nt:
            kb.load(xt[1 - i][:], xin[(t + 1) * 128:(t + 2) * 128, :], ["xin"], [f"xt{1 - i}"])
            kb.load(cs[1 - i][:], cs_d[(t + 1) * 128:(t + 2) * 128], ["cs_d"], [f"cs{1 - i}"])
        emit_transpose_mod(kb, xt[i], f"xt{i}", hT, "hT", i, ident[:], ptr[i], mod, 0, 1, who, f"ptr{i}")
        for nb in range(3):
            for k in range(8):
                kb.mm(po[nb][:], hT[:, k, i * 128:(i + 1) * 128], wr[:, k, nb * 512:(nb + 1) * 512], [("hT", i, k), ("wr", k)], [f"po{nb}"],
                      start=(k == 0), stop=(k == 7))
            kb.cp_rr(qk[:, nb * 512:(nb + 1) * 512], po[nb][:], [f"po{nb}"], [("qk", nb)])
        qkk = [("qk", 0), ("qk", 1), ("qk", 2)]
        kb.tt(sq[:], qk[:, :1280], qk[:, :1280], ALU.mult, qkk, ["sq"], eng="pool")
        kb.S.op("dve", lambda e: e.tensor_reduce(out=ms[:, 0, :], in_=sq[:].rearrange("p (h d) -> p h d", d=128), axis=AX.X, op=ALU.add), reads=["sq"], writes=["ms"])
        kb.act(ms[:, 1, :], ms[:, 0, :], AF.Sqrt, ["ms"], ["ms"], bias=RMS_EPS, scale=1.0 / 128)
        kb.recip(ms[:, 2, :], ms[:, 1, :], ["ms"], ["ms"])
        for h in range(10):
            hs = slice(h * 128, (h + 1) * 128)
            kb.ts(qk[:, hs], qk[:, hs], ms[:, 2, h:h + 1], ALU.mult, qkk + ["ms"], qkk)
        kb.tt(qk[:, :1280], qk[:, :1280], gq[:], ALU.mult, qkk + ["gq"], qkk)
        q3 = qk[:, :1280].rearrange("p (h d) -> p h d", d=128)
        r3 = ro[i][:, :1280].rearrange("p (h d) -> p h d", d=128)
        C3 = cs[i][:, 0, :].rearrange("p (h d) -> p h d", d=64)
        S3 = cs[i][:, 1, :].rearrange("p (h d) -> p h d", d=64)
        x1, x2 = q3[:, :, 0:64], q3[:, :, 64:128]
        rk = [f"ro{i}"]
        kb.tt(t1[:], x1, C3, ALU.mult, qkk + [f"cs{i}"], ["t1"])
        kb.tt(t2[:], x2, S3, ALU.mult, qkk + [f"cs{i}"], ["t2"], eng="pool")
        kb.tt(r3[:, :, 0:64], t1[:], t2[:], ALU.subtract, ["t1", "t2"], rk)
        kb.tt(t1[:], x2, C3, ALU.mult, qkk + [f"cs{i}"], ["t1"])
        kb.tt(t2[:], x1, S3, ALU.mult, qkk + [f"cs{i}"], ["t2"], eng="pool")
        kb.tt(r3[:, :, 64:128], t1[:], t2[:], ALU.add, ["t1", "t2"], rk)
        kb.cp(ro[i][:, 1280:], qk[:, 1280:], qkk, rk, eng="act")
        kb.store(qkv[rows, :], ro[i][:], rk, ["qkv"])
        if "kv_own" in aps and t >= 2:
            kb.store(aps["kv_own"][(t - 2) * 128:(t - 1) * 128, :], ro[i][:, 1024:1536], rk, ["kv_own"])


def rope_tables(nt_lat_rows):
    t = np.asarray(nt_lat_rows)
    row = (t // 64).astype(np.float32)
    col = (t % 64).astype(np.float32)
    inv = (np.float32(10000.0) ** (-np.arange(32, dtype=np.float32) / np.float32(32))).astype(np.float32)
    ang = np.concatenate([row[:, None] * inv, col[:, None] * inv], -1).astype(np.float32)
    return np.cos(ang).astype(np.float32), np.sin(ang).astype(np.float32)


def cossin_for(core, nt=NT):
    r = core % 4
    lat = np.arange(r * (nt - 2) * 128, (r + 1) * (nt - 2) * 128)
    c, s = rope_tables(lat)
    c = np.concatenate([np.ones((256, 64), np.float32), c], 0)
    s = np.concatenate([np.zeros((256, 64), np.float32), s], 0)
    return np.ascontiguousarray(np.stack([np.tile(c, (1, 10)), np.tile(s, (1, 10))], axis=1))


def build_D(nq=16, nkc=66):
    NQ = nq * 128
    NK = nkc * 128
    QB = 4 if nq % 4 == 0 else nq
    kb = KB()
    qT_d = kb.din("qT", [8, 128, NQ])
    kT_d = kb.din("kT", [2, 128, NK])
    v_d = kb.din("vaug", [2, 128, nkc * 130])
    attn = kb.dout("attn", [128, nq * D])
    KTr = kb.sb("KTr", [128, NK], F32R)
    Vr = kb.sb("Vr", [128, nkc, 130], F32R)
    QTr = kb.sb("QTr", [128, NQ], F32R)
    stg = [kb.sb(f"stg{i}", [128, 1040]) for i in range(2)]
    PTt = [kb.sb(f"PTt{i}", [128, QB * 128], F32R) for i in range(2)]
    OT = kb.sb("OT", [128, nq, D])
    rc = kb.sb("rc", [128, 4])
    pS = [kb.ps(f"pS{i}", [128, 512]) for i in range(2)]
    pO = [kb.ps(f"pO{i}", [128, 130]) for i in range(4)]
    si = 0
    for g in range(2):
        for c0 in range(0, NK, 1024):
            w = min(1024, NK - c0)
            kb.load(stg[si % 2][:, :w], kT_d[g, :, c0:c0 + w], ["kT_d"], [f"stg{si % 2}"])
            kb.cp(KTr[:, c0:c0 + w], stg[si % 2][:, :w], [f"stg{si % 2}"], ["KTr"], eng=("pool" if si % 2 else "dve"))
            si += 1
        for c0 in range(0, nkc, 8):
            w = min(8, nkc - c0)
            kb.load(stg[si % 2][:, :w * 130], v_d[g, :, c0 * 130:(c0 + w) * 130], ["v_d"], [f"stg{si % 2}"])
            kb.cp(Vr[:, c0:c0 + w, :], stg[si % 2][:, :w * 130].rearrange("p (c f) -> p c f", f=130), [f"stg{si % 2}"], ["Vr"], eng=("pool" if si % 2 else "dve"))
            si += 1
        for hh in range(4):
            h = g * 4 + hh
            for c0 in range(0, NQ, 1024):
                w = min(1024, NQ - c0)
                kb.load(stg[si % 2][:, :w], qT_d[h, :, c0:c0 + w], ["qT_d"], [f"stg{si % 2}"])
                kb.cp(QTr[:, c0:c0 + w], stg[si % 2][:, :w], [f"stg{si % 2}"], ["QTr"], eng=("pool" if si % 2 else "dve"))
                si += 1
            for qb in range(nq // QB):
                qs = slice(qb * QB * 128, (qb + 1) * QB * 128)
                for kc in range(nkc):
                    pb = kc % 2
                    kb.mm(pS[pb][:, :QB * 128], KTr[:, kc * 128:(kc + 1) * 128], QTr[:, qs], ["KTr", "QTr"], [f"pS{pb}"])
                    kb.act(PTt[pb][:], pS[pb][:, :QB * 128], AF.Exp, [f"pS{pb}"], [f"PTt{pb}"], scale=128 ** -0.5)
                    for jq in range(QB):
                        kb.mm(pO[jq][:], PTt[pb][:, jq * 128:(jq + 1) * 128], Vr[:, kc, :], [f"PTt{pb}", "Vr"], [f"pO{jq}"],
                              start=(kc == 0), stop=(kc == nkc - 1))
                for jq in range(QB):
                    qt = qb * QB + jq
                    kb.recip(rc[:, jq:jq + 1], pO[jq][:, 128:129], [f"pO{jq}"], [("rc", jq)])
                    kb.ts(OT[:, qt, h * 128:(h + 1) * 128], pO[jq][:, 0:128], rc[:, jq:jq + 1], ALU.mult, [f"pO{jq}", ("rc", jq)], [("OT", qt)])
    at3 = attn.rearrange("p (c f) -> p c f", f=D)
    for c0 in range(0, nq, 8):
        c1 = min(nq, c0 + 8)
        kb.store(at3[:, c0:c1, :], OT[:, c0:c1, :], [("OT", q) for q in range(c0, c1)], ["attn"])
    return kb.done()


TM_W = 908
FM_W = 768


def emit_S1(kb, aps, nch):
    T = nch * 128
    xb, cpair, ada_w, ada_b, w_d, cst_d = (aps[k] for k in ("xb", "cpair", "ada_w", "ada_b", "w_in_j", "cst_d"))
    ident = kb.sb("ident", [128, 128])
    kb.load(ident[:], cst_d, ["cst_d"], ["ident"])
    mod = emit_mod_vectors(kb, ada_w, ada_b, cpair, [0, 1], "m", kb.es)
    kb.ts(mod[:, 1], mod[:, 1], 1.0, ALU.add, [("mmod", 1)], ["mod"])
    kb.S.barrier()
    W = TM_W + FM_W
    wr = kb.sb("wr", [128, 8, W], F32R)
    wtmp = [kb.sb(f"wtmp{i}", [128, W]) for i in range(2)]
    for k in range(8):
        kb.load(wtmp[k % 2][:], w_d[k * 128:(k + 1) * 128, :], ["w_d"], [f"wtmp{k % 2}"])
        kb.cp(wr[:, k, :], wtmp[k % 2][:], [f"wtmp{k % 2}"], [("wr", k)], eng=("pool" if k % 2 else "dve"))
    hT = kb.sb("hT", [128, 8, 256], F32R)
    xt = [kb.sb(f"xt{i}", [128, D]) for i in range(2)]
    tmo = [kb.sb(f"tmo{i}", [128, TM_W]) for i in range(2)]
    fmo = [kb.sb(f"fmo{i}", [128, 6, 128]) for i in range(2)]
    ptr = [kb.ps(f"ptr{i}", [128, 4, 128]) for i in range(2)]
    pt = [kb.ps(f"pt{i}", [128, 512]) for i in range(2)]
    pf = [kb.ps("pf0", [128, 4, 128]), kb.ps("pf1", [128, 2, 128])]
    E = aps["E"]
    kb.load(xt[0][:], xb[0:128, :], ["xb"], ["xt0"])
    for t in range(nch):
        i = t % 2
        who = 1 if t < 2 else 0
        rows = slice(t * 128, (t + 1) * 128)
        if t + 1 < nch:
            kb.load(xt[1 - i][:], xb[(t + 1) * 128:(t + 2) * 128, :], ["xb"], [f"xt{1 - i}"])
        emit_transpose_mod(kb, xt[i], f"xt{i}", hT, "hT", i, ident[:], ptr[i], mod, 0, 1, who, f"ptr{i}")
        hk = [("hT", i, k) for k in range(8)]
        for nb, (c0, cw) in enumerate(((0, 512), (512, TM_W - 512))):
            for k in range(8):
                kb.mm(pt[nb][:, :cw], hT[:, k, i * 128:(i + 1) * 128], wr[:, k, c0:c0 + cw], [("hT", i, k), ("wr", k)], [f"pt{nb}"], start=(k == 0), stop=(k == 7))
            kb.cp_rr(tmo[i][:, c0:c0 + cw], pt[nb][:, :cw], [f"pt{nb}"], [(f"tmo{i}", nb)])
        for fc in range(6):
            tl, sl_ = (pf[0], fc) if fc < 4 else (pf[1], fc - 4)
            for k in range(8):
                kb.mm(tl[:, sl_, :], wr[:, k, TM_W + fc * 128:TM_W + (fc + 1) * 128], hT[:, k, i * 128:(i + 1) * 128], [("hT", i, k), ("wr", k)],
                      ["pf0" if fc < 4 else "pf1"], start=(k == 0), stop=(k == 7))
        kb.cp_rr(fmo[i][:, 0:4, :], pf[0][:], ["pf0"], [(f"fmo{i}", 0)])
        kb.cp_rr(fmo[i][:, 4:6, :], pf[1][:], ["pf1"], [(f"fmo{i}", 1)])
        k0, k1 = [(f"tmo{i}", 0)], [(f"tmo{i}", 1)]
        kb.store(aps["v_s"][rows, :], tmo[i][:, 0:256], k0, ["v_s"])
        kb.store(E[rows, 512:768], tmo[i][:, 256:512], k0, ["E_o"])
        kb.store(E[rows, 768:1024], tmo[i][:, 512:768], k1, ["E_z"])
        kb.store(aps["ktok_s"][rows, :], tmo[i][:, 768:896], k1, ["ktok_s"])
        kb.store(aps["gates_s"][:, t * 4:(t + 1) * 4], tmo[i][:, 896:900], k1, ["gates_s"])
        kb.store(aps["dtr_s"][:, t * 8:(t + 1) * 8], tmo[i][:, 900:908], k1, ["dtr_s"])
        f0, f1 = [(f"fmo{i}", 0)], [(f"fmo{i}", 1)]
        kb.store(aps["qT_s"][:, rows], fmo[i][:, 0, :], f0, ["qT_s"])
        kb.store(aps["kT_s"][:, rows], fmo[i][:, 1, :], f0, ["kT_s"])
        kb.store(aps["xbcT_s"][0:256, rows].rearrange("(a p) t -> p a t", p=128), fmo[i][:, 2:4, :], f0, ["xbcT_s"])
        kb.store(aps["xbcT_s"][256:512, rows].rearrange("(a p) t -> p a t", p=128), fmo[i][:, 4:6, :], f1, ["xbcT_s"])


def emit_D2(kb, aps, nq, nkc):
    NQ = nq * 128
    NK = nkc * 128
    QB = 4
    qkv, kvg, attn_s, cst_d = aps["qkv"], aps["kvg"], aps["attn_s"], aps["cst_d"]
    ident = kb.sb("ident", [128, 128])
    kb.load(ident[:], cst_d, ["cst_d"], ["ident"])
    KTr = kb.sb("KTr", [128, NK], F32R)
    Vr = kb.sb("Vr", [128, nkc, 130], F32R)
    QTr = kb.sb("QTr", [128, NQ], F32R)
    stg = [kb.sb(f"stg{i}", [128, 4, 128]) for i in range(3)]
    PTt = [kb.sb(f"PTt{i}", [128, QB * 128], F32R) for i in range(2)]
    OT = kb.sb("OT", [128, nq, D])
    rc = kb.sb("rc", [128, 4])
    zt = kb.sb("zt", [128, D])
    pS = [kb.ps(f"pS{i}", [128, 512]) for i in range(2)]
    pO = [kb.ps(f"pO{i}", [128, 130]) for i in range(4)]
    pX = [kb.ps(f"pX{i}", [128, 4, 128]) for i in range(2)]
    kb.memset(zt[:], 0.0, ["zt"])
    for c in range(2):
        kb.store(attn_s[c * 128:(c + 1) * 128, :], zt[:], ["zt"], ["attn_s"])
    onez = kb.sb("onez", [128, nkc, 2])
    kb.memset(onez[:, :, 0:1], 1.0, ["onez"])
    kb.memset(onez[:, :, 1:2], 0.0, ["onez"])
    kb.cp(Vr[:, :, 128:130], onez[:], ["onez"], [("Vr", "one"), ("Vr", "zero")])
    si = 0

    def key_groups():
        yield 0, 2, None
        for c0 in range(2, nkc, 4):
            yield c0, 4, (c0 - 2) * 128

    for g in range(2):
        for (c0, n, krow) in key_groups():
            for which, col0 in (("k", g * 128), ("v", 256 + g * 128)):
                b_ = si % 3
                si += 1
                if krow is None:
                    src = qkv[0:256, 1024 + col0:1024 + col0 + 128].rearrange("(c p) f -> p c f", p=128)
                else:
                    src = kvg[krow:krow + n * 128, col0:col0 + 128].rearrange("(c p) f -> p c f", p=128)
                kb.load(stg[b_][:, :n, :], src, ["qkv", "kvg"], [f"stg{b_}"])
                if which == "v":
                    kb.cp(Vr[:, c0:c0 + n, 0:128], stg[b_][:, :n, :], [f"stg{b_}"], ["Vr"], eng=("pool" if si % 2 else "dve"))
                else:
                    px = pX[(si // 2) % 2]
                    pk = f"pX{(si // 2) % 2}"
                    for a in range(n):
                        kb.tr(px[:, a, :], stg[b_][:, a, :], ident[:], [f"stg{b_}"], [pk])
                    kb.cp_rr(KTr[:, c0 * 128:(c0 + n) * 128], px[:, :n, :].rearrange("p a t -> p (a t)"), [pk], ["KTr"])
        for hh in range(4):
            h = g * 4 + hh
            for q0 in range(0, nq, 4):
                b_ = si % 3
                si += 1
                kb.load(stg[b_][:], qkv[256 + q0 * 128:256 + (q0 + 4) * 128, h * 128:(h + 1) * 128].rearrange("(c p) f -> p c f", p=128), ["qkv"], [f"stg{b_}"])
                px = pX[si % 2]
                pk = f"pX{si % 2}"
                for a in range(4):
                    kb.tr(px[:, a, :], stg[b_][:, a, :], ident[:], [f"stg{b_}"], [pk])
                kb.cp_rr(QTr[:, q0 * 128:(q0 + 4) * 128], px[:].rearrange("p a t -> p (a t)"), [pk], ["QTr"])
            for qb in range(nq // QB):
                qs = slice(qb * QB * 128, (qb + 1) * QB * 128)
                def s_mm(kc_):
                    kb.mm(pS[kc_ % 2][:, :QB * 128], KTr[:, kc_ * 128:(kc_ + 1) * 128], QTr[:, qs], ["KTr", "QTr"], [f"pS{kc_ % 2}"])

                s_mm(0)
                for kc in range(nkc):
                    pb = kc % 2
                    if kc + 1 < nkc:
                        s_mm(kc + 1)
                    kb.act(PTt[pb][:], pS[pb][:, :QB * 128], AF.Exp, [f"pS{pb}"], [f"PTt{pb}"], scale=128 ** -0.5)
                    for jq in range(QB):
                        kb.mm(pO[jq][:], PTt[pb][:, jq * 128:(jq + 1) * 128], Vr[:, kc, :], [f"PTt{pb}", "Vr", ("Vr", "one"), ("Vr", "zero")], [f"pO{jq}"],
                              start=(kc == 0), stop=(kc == nkc - 1))
                for jq in range(QB):
                    qt = qb * QB + jq
                    kb.recip(rc[:, jq:jq + 1], pO[jq][:, 128:129], [f"pO{jq}"], [("rc", jq)])
                    kb.ts(OT[:, qt, h * 128:(h + 1) * 128], pO[jq][:, 0:128], rc[:, jq:jq + 1], ALU.mult, [f"pO{jq}", ("rc", jq)], [("OT", qt)])
    at3 = attn_s[256:256 + NQ, :].rearrange("(c p) f -> p c f", p=128)
    for c0 in range(0, nq, 4):
        kb.store(at3[:, c0:c0 + 4, :], OT[:, c0:c0 + 4, :], [("OT", q) for q in range(c0, c0 + 4)], ["attn_s"])


def build_fused(ntown=16, stop=99):
    nt = ntown + 2
    tok = nt * 128
    nlat = 4 * ntown
    NCH = nlat + 2
    T = NCH * 128
    kb = KB()
    nc = kb.nc
    W = TM_W + FM_W
    I = dict(
        xb=kb.din("xb", [T, D]), xown=kb.din("xown", [tok, D]), cpair=kb.din("cpair", [128, 16]),
        ada_w0=kb.din("ada_w0", [D, 6 * D]), ada_b0=kb.din("ada_b0", [128, 48]), ada_b0_row=kb.din("ada_b0_row", [1, 6 * D]),
        ada_w1=kb.din("ada_w1", [D, 6 * D]), ada_b1=kb.din("ada_b1", [128, 48]), ada_b1_row=kb.din("ada_b1_row", [1, 6 * D]),
        w_in_j=kb.din("w_in_j", [D, W]), cst_d=kb.din("cst_d", [128, 128]),
        gb=kb.din("gb", [128, 4]), convw=kb.din("convw", [128, 16]), convb=kb.din("convb", [128, 4]), dtb=kb.din("dtb", [128, 8]),
        alog=kb.din("alog", [128, 8]), dsk=kb.din("dsk", [128, 4]), cst=kb.din("cst", [128, 6, 128]),
        lnp0=kb.din("lnp0", [1, 4 * D]), lnp1=kb.din("lnp1", [1, 4 * D]), w_out0=kb.din("w_out0", [2048, D]), w_out1=kb.din("w_out1", [D, D]),
        ng=kb.din("ng", [1, 2 * D]), router_w=kb.din("router_w", [D, 16]), router_b=kb.din("router_b", [1, 16]),
        wg0=kb.din("wg0", [NE, 4, 128, 2048]), wu0=kb.din("wu0", [NE, 4, 128, 2048]), wd0=kb.din("wd0", [NE, 4, 128, 2048]),
        wg1=kb.din("wg1", [NE, 4, 128, 2048]), wu1=kb.din("wu1", [NE, 4, 128, 2048]), wd1=kb.din("wd1", [NE, 4, 128, 2048]),
        at_w_in=kb.din("at_w_in", [D, 1536]), gqk=kb.din("gqk", [1, 1280]), cossin=kb.din("cossin", [tok, 2, 640]),
    )
    I["sel"] = kb.din("sel", [128, 4])
    out = kb.dout("out", [ntown * 128, D])
    groups = [[0, 1, 2, 3], [4, 5, 6, 7]]
    Sx = {n: kb.dscr(n, sh) for n, sh in dict(
        qT_s=[128, T], kT_s=[128, T], ktok_s=[T, 128], v_s=[T, 256], gates_s=[128, NCH * 4], dtr_s=[128, NCH * 8], xbcT_s=[512, T], xpost=[512, T],
        E=[T, D], EG=[4 * T, D], x1a=[tok, D], x0_s=[tok, D], qkv_s=[tok, 1536], kv_own=[ntown * 128, 512], kvg=[4 * ntown * 128, 512],
        attn_s=[tok, D], x1b=[tok, D]).items()}

    with kb.scope():
        emit_S1(kb, dict(xb=I["xb"], cpair=I["cpair"], ada_w=I["ada_w0"], ada_b=I["ada_b0"], w_in_j=I["w_in_j"], cst_d=I["cst_d"], **Sx), NCH)
    if stop < 1:
        return kb.done()
    E3 = Sx["E"].rearrange("(c p) f -> p c f", p=128)
    with kb.scope():
        emit_B(kb, dict(qT=Sx["qT_s"], kT=Sx["kT_s"], ktok=Sx["ktok_s"], v=Sx["v_s"], gates=Sx["gates_s"], xbcT=Sx["xbcT_s"], dtr=Sx["dtr_s"],
                        gb=I["gb"], convw=I["convw"], convb=I["convb"], dtb=I["dtb"], alog=I["alog"], dsk=I["dsk"], cst=I["cst"],
                        E=Sx["E"], xpost=Sx["xpost"]), nlat)
    if stop < 2:
        return kb.done()
    for k in range(T // 256):
        kb.collective("AllGather", Sx["E"][k * 256:(k + 1) * 256, :], Sx["EG"][k * 1024:(k + 1) * 1024, :], groups, ["hm_o", "ys_o", "E_o", "E_z"], "EG")
    kb.S.barrier()
    if stop < 3:
        return kb.done()
    with kb.scope():
        emit_P(kb, 0, dict(xres=I["xown"], cpair=I["cpair"], ada_w=I["ada_w0"], ada_b=I["ada_b0"], ada_b_row=I["ada_b0_row"], lnp=I["lnp0"],
                           w_out=I["w_out0"], cst_d=I["cst_d"], router_w=I["router_w"], router_b=I["router_b"], w_gate=I["wg0"], w_up=I["wu0"],
                           w_down=I["wd0"], ng=I["ng"], EG=Sx["EG"], sel=I["sel"], x1_d=Sx["x1a"],
                           xout_fn=lambda t: Sx["x0_s"][t * 128:(t + 1) * 128, :]), nt, NE)
    if stop < 4:
        return kb.done()
    with kb.scope():
        emit_Q(kb, dict(xin=Sx["x0_s"], cpair=I["cpair"], ada_w=I["ada_w1"], ada_b=I["ada_b1"], w_in=I["at_w_in"], cst_d=I["cst_d"],
                        gqk=I["gqk"], cossin=I["cossin"], qkv=Sx["qkv_s"], kv_own=Sx["kv_own"]), nt)
    for m in range(ntown * 128 // 512):
        kb.collective("AllGather", Sx["kv_own"][m * 512:(m + 1) * 512, :], Sx["kvg"][m * 2048:(m + 1) * 2048, :], groups, ["kv_own"], "kvg")
    kb.S.barrier()
    if stop < 5:
        return kb.done()
    with kb.scope():
        emit_D2(kb, dict(qkv=Sx["qkv_s"], kvg=Sx["kvg"], attn_s=Sx["attn_s"], cst_d=I["cst_d"]), ntown, nlat + 2)
    with kb.scope():
        emit_P(kb, 1, dict(xres=Sx["x0_s"][256:, :], cpair=I["cpair"], ada_w=I["ada_w1"], ada_b=I["ada_b1"], ada_b_row=I["ada_b1_row"], lnp=I["lnp1"],
                           w_out=I["w_out1"], cst_d=I["cst_d"], router_w=I["router_w"], router_b=I["router_b"], w_gate=I["wg1"], w_up=I["wu1"],
                           w_down=I["wd1"], attn=Sx["attn_s"][256:, :], x1_d=Sx["x1b"],
                           xout_fn=lambda t: out[t * 128:(t + 1) * 128, :]), ntown, NE, nctx=0, TB=(512 if ntown % 8 == 0 else 256))
    return kb.done()


def w_in_cols(j):
    g = j // 2
    ar = np.arange
    tm = np.concatenate([1024 + j * 256 + ar(256), 2048 + j * 256 + ar(256), 3088 + j * 256 + ar(256), 512 + j * 128 + ar(128),
                         [3072 + j, 3076 + j, 3080 + j, 3084 + j], [5648 + d * 16 + 4 * j + h for d in range(2) for h in range(4)]])
    fm = np.concatenate([j * 128 + ar(128), 512 + j * 128 + ar(128), 4112 + j * 256 + ar(256), 4112 + 1024 + g * 128 + ar(128), 4112 + 1280 + g * 128 + ar(128)])
    return np.concatenate([tm, fm]).astype(np.int64)


def scan_params(inp, j):
    g = j // 2
    chs = np.concatenate([np.arange(j * 256, (j + 1) * 256), 1024 + np.arange(g * 128, (g + 1) * 128), 1280 + np.arange(g * 128, (g + 1) * 128)])
    cwt = inp["ssm_conv_w"][0][:, chs]
    return {
        "gb": rep(np.concatenate([inp["ml_ig_b"][0][:, j], inp["ml_fg_b"][0][:, j]])),
        "convw": np.ascontiguousarray(cwt.T.reshape(4, 128, 4).transpose(1, 0, 2).reshape(128, 16)),
        "convb": np.ascontiguousarray(inp["ssm_conv_b"][0][chs].reshape(4, 128).T),
        "dtb": rep(inp["ssm_dt_b"][0][:, 4 * j:4 * j + 4].reshape(-1)),
        "alog": rep(inp["ssm_a_log"][0][:, 4 * j:4 * j + 4].reshape(-1)),
        "dsk": rep(inp["ssm_d"][0][4 * j:4 * j + 4]),
        "cst": consts_B(),
    }


def fused_maps(inp, ntown=16):
    nt = ntown + 2
    own = ntown * 128
    T = 256 + 4 * own
    maps = []
    shared = {
        "ada_w0": np.ascontiguousarray(inp["ada_w"][0]), "ada_b0": pp_layout(inp["ada_b"][0]), "ada_b0_row": np.ascontiguousarray(inp["ada_b"][0].reshape(1, -1)),
        "ada_w1": np.ascontiguousarray(inp["ada_w"][1]), "ada_b1": pp_layout(inp["ada_b"][1]), "ada_b1_row": np.ascontiguousarray(inp["ada_b"][1].reshape(1, -1)),
        "cst_d": consts()["ident"],
        "lnp0": np.ascontiguousarray(np.concatenate([inp["ln_g"][0, 0], inp["ln_b"][0, 0], inp["ln_g"][0, 1], inp["ln_b"][0, 1]]).reshape(1, -1)),
        "lnp1": np.ascontiguousarray(np.concatenate([inp["ln_g"][1, 0], inp["ln_b"][1, 0], inp["ln_g"][1, 1], inp["ln_b"][1, 1]]).reshape(1, -1)),
        "w_out0": np.ascontiguousarray(inp["ab_w_out"][0]), "w_out1": np.ascontiguousarray(inp["at_w_out"][0]),
        "ng": np.ascontiguousarray(np.concatenate([inp["ml_norm_g"][0], inp["ssm_norm_g"][0]]).reshape(1, -1)),
        "router_w": np.ascontiguousarray(inp["router_w"]), "router_b": np.ascontiguousarray(inp["router_b"].reshape(1, -1)),
        "wg0": moe_layout("w_gate", inp["moe_w_gate"][0]), "wu0": moe_layout("w_up", inp["moe_w_up"][0]), "wd0": moe_layout("w_down", inp["moe_w_down"][0]),
        "wg1": moe_layout("w_gate", inp["moe_w_gate"][1]), "wu1": moe_layout("w_up", inp["moe_w_up"][1]), "wd1": moe_layout("w_down", inp["moe_w_down"][1]),
        "at_w_in": np.ascontiguousarray(inp["at_w_in"][0]),
        "gqk": np.ascontiguousarray(np.concatenate([np.tile(inp["at_q_g"][0], 8), np.tile(inp["at_k_g"][0], 2)]).reshape(1, -1)),
    }
    for core in range(NCORES):
        b, r = core // 4, core % 4
        xb = np.concatenate([inp["ctx"][b], inp["x"][b, :4 * own]], axis=0)
        xown = np.concatenate([inp["ctx"][b], inp["x"][b, r * own:(r + 1) * own]], axis=0)
        sel = np.zeros((128, 4), np.float32)
        sel[:, r] = 1.0
        m = dict(shared)
        m.update(scan_params(inp, r))
        m.update({"xb": np.ascontiguousarray(xb), "xown": np.ascontiguousarray(xown), "cpair": cpair_pp(inp, b),
                  "w_in_j": np.ascontiguousarray(inp["ab_w_in"][0][:, w_in_cols(r)]), "cossin": cossin_for(core, nt),
                  "sel": sel})
        maps.append(m)
    return maps


def kernel(x, c, ctx, c_ctx, ada_w, ada_b, ln_g, ln_b, ab_w_in, ab_w_out, ml_ig_b, ml_fg_b, ml_norm_g,
           ssm_conv_w, ssm_conv_b, ssm_dt_b, ssm_a_log, ssm_d, ssm_norm_g, at_w_in, at_w_out, at_q_g, at_k_g,
           router_w, router_b, moe_w_gate, moe_w_up, moe_w_down):
    inp = {k: np.asarray(v, dtype=np.float32) for k, v in dict(
        x=x, c=c, ctx=ctx, c_ctx=c_ctx, ada_w=ada_w, ada_b=ada_b, ln_g=ln_g, ln_b=ln_b, ab_w_in=ab_w_in, ab_w_out=ab_w_out,
        ml_ig_b=ml_ig_b, ml_fg_b=ml_fg_b, ml_norm_g=ml_norm_g, ssm_conv_w=ssm_conv_w, ssm_conv_b=ssm_conv_b, ssm_dt_b=ssm_dt_b,
        ssm_a_log=ssm_a_log, ssm_d=ssm_d, ssm_norm_g=ssm_norm_g, at_w_in=at_w_in, at_w_out=at_w_out, at_q_g=at_q_g, at_k_g=at_k_g,
        router_w=router_w, router_b=router_b, moe_w_gate=moe_w_gate, moe_w_up=moe_w_up, moe_w_down=moe_w_down).items()}
    nc = build_fused(16)
    res = run_bass_kernel_spmd(nc, fused_maps(inp, 16), core_ids=list(range(NCORES)))
    out = np.zeros((2, SEQ, D), np.float32)
    for core in range(NCORES):
        b, r = core // 4, core % 4
        out[b, r * TOWN:(r + 1) * TOWN] = res.results[core]["out"]
    return out
```
